# Optimizing a Trainium2 kernel written in Bass

```python
import math
import jax, jax.numpy as jnp
from jax import lax
import numpy as np

D_MODEL = 1024
BATCH = 16
SEQ = 2048
DEPTH = 2

CHUNK = 64
Q_BLOCK = 128
EPS = 1e-6
NEG = -1e30
CONV_WIDTH = 256
CONV_K = 3
SSM_WIDTH = 256
SSM_GROUP = 16
SSM_GROUPS = SSM_WIDTH // SSM_GROUP
SSM_STATE = 64
ATTN_HEADS = 8
HEAD_DIM = 64
IDX_HEADS = 4
IDX_DIM = 64
TOPK_MAX = 256
D_FF = -(-(8 * D_MODEL) // (3 * 256)) * 256
N_BRANCHES = 3
D_IN = (3 * CONV_WIDTH + SSM_WIDTH + ATTN_HEADS * HEAD_DIM + 2 * HEAD_DIM
        + IDX_HEADS * IDX_DIM + IDX_DIM + IDX_HEADS + N_BRANCHES * D_MODEL)

kernel_name = "chunk_causal_hybrid_conv_s5_dsa_adaln"


def _in_split_points():
    sizes = (3 * CONV_WIDTH, SSM_WIDTH, ATTN_HEADS * HEAD_DIM, HEAD_DIM, HEAD_DIM,
             IDX_HEADS * IDX_DIM, IDX_DIM, IDX_HEADS)
    return tuple(int(v) for v in np.cumsum(sizes))


def rms_norm(x, gain):
    xf = x.astype(jnp.float32)
    y = xf * lax.rsqrt(jnp.mean(xf * xf, axis=-1, keepdims=True) + EPS)
    return (y * gain.astype(jnp.float32)).astype(x.dtype)


def modulate(h, shift, scale):
    return h * (1.0 + scale[:, None, :]) + shift[:, None, :]


def short_conv_branch(xin, conv_w, w_out):
    b_g, c_g, val = jnp.split(xin, 3, axis=-1)
    z = c_g * val
    s = z.shape[1]
    zp = jnp.pad(z, ((0, 0), (CONV_K - 1, 0), (0, 0)))
    y = sum(conv_w[k] * zp[:, k:k + s] for k in range(CONV_K))
    return (b_g * y) @ w_out


def _ssm_combine(e1, e2):
    a1r, a1i, b1r, b1i = e1
    a2r, a2i, b2r, b2i = e2
    return (a2r * a1r - a2i * a1i,
            a2r * a1i + a2i * a1r,
            a2r * b1r - a2i * b1i + b2r,
            a2r * b1i + a2i * b1r + b2i)


def s5_branch(u, a_re, a_im, log_dt, b_re, b_im, c_re, c_im, d_skip, w_glu):
    dtype = u.dtype
    bsz, s, _ = u.shape
    f32 = jnp.float32
    uf = u.astype(f32).reshape(bsz, s, SSM_GROUPS, SSM_GROUP)
    ar, ai = a_re.astype(f32), a_im.astype(f32)
    dt = jnp.exp(log_dt.astype(f32))[:, None]
    mag = jnp.exp(dt * ar)
    abar_re = mag * jnp.cos(dt * ai)
    abar_im = mag * jnp.sin(dt * ai)
    den = ar * ar + ai * ai
    nr, ni = abar_re - 1.0, abar_im
    coef_re = (nr * ar + ni * ai) / den
    coef_im = (ni * ar - nr * ai) / den
    br, bi = b_re.astype(f32), b_im.astype(f32)
    bb_re = coef_re[..., None] * br - coef_im[..., None] * bi
    bb_im = coef_re[..., None] * bi + coef_im[..., None] * br
    v_re = jnp.einsum('bsgj,gpj->bsgp', uf, bb_re)
    v_im = jnp.einsum('bsgj,gpj->bsgp', uf, bb_im)
    a_r = jnp.broadcast_to(abar_re, v_re.shape)
    a_i = jnp.broadcast_to(abar_im, v_re.shape)
    _, _, h_re, h_im = lax.associative_scan(_ssm_combine, (a_r, a_i, v_re, v_im), axis=1)
    y = (jnp.einsum('bsgp,gjp->bsgj', h_re, c_re.astype(f32))
         - jnp.einsum('bsgp,gjp->bsgj', h_im, c_im.astype(f32))
         + d_skip.astype(f32).reshape(SSM_GROUPS, SSM_GROUP) * uf)
    z = jax.nn.gelu(y.reshape(bsz, s, SSM_WIDTH)).astype(dtype)
    za, zb = jnp.split(z @ w_glu, 2, axis=-1)
    return za * jax.nn.sigmoid(zb)


def dsa_branch(q, k, v, qi, ki, wi, w_out):
    f32 = jnp.float32
    bsz, s, _ = q.shape
    q = q.reshape(bsz, s, ATTN_HEADS, HEAD_DIM)
    qi = qi.reshape(bsz, s, IDX_HEADS, IDX_DIM)
    k_sel = min(TOPK_MAX, s // 4)
    n_blocks = s // Q_BLOCK
    slopes = 2.0 ** (-(jnp.arange(1, ATTN_HEADS + 1, dtype=f32) * (8.0 / ATTN_HEADS)))
    key_pos = jnp.arange(s, dtype=jnp.int32)
    gather = jax.vmap(lambda tb, ib: tb[ib])

    def block(i):
        start = i * Q_BLOCK
        qb = lax.dynamic_slice_in_dim(q, start, Q_BLOCK, axis=1)
        qib = lax.dynamic_slice_in_dim(qi, start, Q_BLOCK, axis=1)
        wib = lax.dynamic_slice_in_dim(wi, start, Q_BLOCK, axis=1)
        t = start + jnp.arange(Q_BLOCK, dtype=jnp.int32)
        sc = jnp.einsum('bthd,bsd->bths', qib, ki).astype(f32) * (IDX_DIM ** -0.5)
        idx_score = jnp.einsum('bth,bths->bts', wib.astype(f32) * (IDX_HEADS ** -0.5), jax.nn.relu(sc))
        admissible = (key_pos[None, :] // CHUNK) <= (t[:, None] // CHUNK)
        idx_score = jnp.where(admissible[None], idx_score, NEG)
        _, sel = lax.top_k(idx_score, k_sel)
        k_g = gather(k, sel)
        v_g = gather(v, sel)
        valid = (sel // CHUNK) <= (t[None, :, None] // CHUNK)
        dist = jnp.abs(t[None, :, None] - sel).astype(f32)
        logits = (jnp.einsum('bthd,btkd->bthk', qb, k_g).astype(f32) * (HEAD_DIM ** -0.5)
                  - slopes[None, None, :, None] * dist[:, :, None, :])
        logits = jnp.where(valid[:, :, None, :], logits, NEG)
        p = jax.nn.softmax(logits, axis=-1).astype(v.dtype)
        return jnp.einsum('bthk,btkd->bthd', p, v_g)

    out = lax.map(block, jnp.arange(n_blocks))
    out = out.transpose(1, 0, 2, 3, 4).reshape(bsz, s, ATTN_HEADS * HEAD_DIM)
    return out @ w_out


def setup_inputs(seed: int = 0) -> dict:
    key = jax.random.key(seed)
    ks = jax.random.split(key, 32)
    f32 = jnp.float32

    def nrm(k, shape, scale):
        return jax.random.normal(k, shape, f32) * scale

    L, D = DEPTH, D_MODEL
    G, P, J = SSM_GROUPS, SSM_STATE, SSM_GROUP
    n_idx = jnp.arange(P, dtype=f32)
    return {
        "x": nrm(ks[0], (BATCH, SEQ, D), 1.0),
        "c": nrm(ks[1], (BATCH, D), 1.0),
        "w_mod": nrm(ks[2], (L, D, 6 * D), 0.5 * D ** -0.5),
        "b_mod": nrm(ks[3], (L, 6 * D), 0.01),
        "norm1": 1.0 + nrm(ks[4], (L, D), 0.01),
        "w_in": nrm(ks[5], (L, D, D_IN), D ** -0.5),
        "b_gate": nrm(ks[6], (L, N_BRANCHES * D), 0.01),
        "conv_w": nrm(ks[7], (L, CONV_K, CONV_WIDTH), CONV_K ** -0.5),
        "w_conv_out": nrm(ks[8], (L, CONV_WIDTH, D), CONV_WIDTH ** -0.5),
        "ssm_a_re": -0.5 + nrm(ks[9], (L, G, P), 0.01),
        "ssm_a_im": math.pi * n_idx + nrm(ks[10], (L, G, P), 0.01),
        "ssm_log_dt": jax.random.uniform(ks[11], (L, G), f32, math.log(0.001), math.log(0.1)),
        "ssm_b_re": nrm(ks[12], (L, G, P, J), (2 * J) ** -0.5),
        "ssm_b_im": nrm(ks[13], (L, G, P, J), (2 * J) ** -0.5),
        "ssm_c_re": nrm(ks[14], (L, G, J, P), (2 * P) ** -0.5),
        "ssm_c_im": nrm(ks[15], (L, G, J, P), (2 * P) ** -0.5),
        "ssm_d": 1.0 + nrm(ks[16], (L, SSM_WIDTH), 0.1),
        "w_glu": nrm(ks[17], (L, SSM_WIDTH, 2 * D), SSM_WIDTH ** -0.5),
        "w_attn_out": nrm(ks[18], (L, ATTN_HEADS * HEAD_DIM, D), (ATTN_HEADS * HEAD_DIM) ** -0.5),
        "w_o": nrm(ks[19], (L, D, D), D ** -0.5),
        "norm2": 1.0 + nrm(ks[20], (L, D), 0.01),
        "w_ffn_in": nrm(ks[21], (L, D, 2 * D_FF), D ** -0.5),
        "w_ffn_out": nrm(ks[22], (L, D_FF, D), D_FF ** -0.5),
        "norm_f": 1.0 + nrm(ks[23], (D,), 0.01),
    }


def reference(x, c, w_mod, b_mod, norm1, w_in, b_gate, conv_w, w_conv_out,
              ssm_a_re, ssm_a_im, ssm_log_dt, ssm_b_re, ssm_b_im, ssm_c_re, ssm_c_im,
              ssm_d, w_glu, w_attn_out, w_o, norm2, w_ffn_in, w_ffn_out, norm_f):
    split_points = _in_split_points()
    c_act = jax.nn.silu(c)
    for l in range(DEPTH):
        mod = c_act @ w_mod[l] + b_mod[l]
        sh1, sc1, g1, sh2, sc2, g2 = jnp.split(mod, 6, axis=-1)
        h = modulate(rms_norm(x, norm1[l]), sh1, sc1)
        proj = h @ w_in[l]
        p_conv, p_ssm, p_q, p_k, p_v, p_qi, p_ki, p_wi, p_gate = jnp.split(proj, split_points, axis=-1)
        y_a = short_conv_branch(p_conv, conv_w[l], w_conv_out[l])
        y_b = s5_branch(p_ssm, ssm_a_re[l], ssm_a_im[l], ssm_log_dt[l], ssm_b_re[l], ssm_b_im[l],
                        ssm_c_re[l], ssm_c_im[l], ssm_d[l], w_glu[l])
        y_c = dsa_branch(p_q, p_k, p_v, p_qi, p_ki, p_wi, w_attn_out[l])
        g_a, g_b, g_c = jnp.split(jax.nn.sigmoid(p_gate + b_gate[l]), N_BRANCHES, axis=-1)
        mixed = (g_a * y_a + g_b * y_b + g_c * y_c) @ w_o[l]
        x = x + g1[:, None, :] * mixed
        h = modulate(rms_norm(x, norm2[l]), sh2, sc2)
        f_gate, f_up = jnp.split(h @ w_ffn_in[l], 2, axis=-1)
        x = x + g2[:, None, :] * ((jax.nn.silu(f_gate) * f_up) @ w_ffn_out[l])
    return rms_norm(x, norm_f)
```

```python
import numpy as np
import ml_dtypes
from contextlib import ExitStack
import concourse.bass as bass
import concourse.mybir as mybir
from concourse.bass_utils import run_bass_kernel_spmd

F32 = mybir.dt.float32
BF16 = mybir.dt.bfloat16
AF = mybir.ActivationFunctionType
ALU = mybir.AluOpType
AX = mybir.AxisListType

S = 2048
D = 1024
NB = 2
L = 2
DFF = 2816
DIN_P = 5120
NCORES = 8
NEG = -1e30
NDS = 24
BISECT_ITERS = 12
FUSE_NORM = True
SSM_PIPE = True
DBG_TILES = (3, 5, 8, 14)
TWO_PI = 6.283185307179586
MAGIC = 12582912.0


class Buf:
    __slots__ = ("t", "w", "r", "name")

    def __init__(self, t, name=""):
        self.t = t
        self.w = {}
        self.r = {}
        self.name = name

    def __getitem__(self, idx):
        return self.t[idx]


class KB:
    def __init__(self, nc, st):
        self.nc = nc
        self.E = {"pe": nc.tensor, "act": nc.scalar, "dve": nc.vector, "pool": nc.gpsimd, "sp": nc.sync}
        self.semobj = {}
        self.cnt = {e: 0 for e in self.E}
        self.seen = {e: {} for e in self.E}
        for e in self.E:
            self.semobj[e] = st.enter_context(nc.semaphore("s_" + e))
        self.dcnt = [0] * NDS
        self.dnext = 0
        for i in range(NDS):
            self.semobj["d%d" % i] = st.enter_context(nc.semaphore("dq%d" % i))
        self.ninstr = 0

    def _wait(self, eng, sid, val):
        if self.seen[eng].get(sid, 0) >= val:
            return
        self.E[eng].wait_ge(self.semobj[sid], val)
        self.seen[eng][sid] = val

    def _deps(self, eng, reads, writes, merge):
        for b in reads:
            for sid, val in b.w.items():
                if sid == eng and eng in ("pe", "sp"):
                    continue
                self._wait(eng, sid, val)
        strict = eng in ("act", "dve", "pool")
        for b in list(writes) + list(merge):
            if b not in merge:
                for sid, val in b.w.items():
                    if sid != eng or strict:
                        self._wait(eng, sid, val)
            for sid, val in b.r.items():
                if sid != eng or strict:
                    self._wait(eng, sid, val)

    def _reg(self, sid, val, reads, writes, merge):
        for b in reads:
            if b.r.get(sid, 0) < val:
                b.r[sid] = val
        for b in writes:
            b.w = {sid: val}
            b.r = {}
        for b in merge:
            b.w[sid] = val
            b.r = {}

    def op(self, eng, fn, reads=(), writes=(), inc=True, merge=()):
        self._deps(eng, reads, writes, merge)
        ins = fn()
        self.ninstr += 1
        if inc:
            self.cnt[eng] += 1
            ins.then_inc(self.semobj[eng], 1)
            val = self.cnt[eng]
        else:
            val = self.cnt[eng] + 1
        self._reg(eng, val, reads, writes, merge)
        return ins

    def dma(self, q, out, in_, reads=(), writes=(), merge=(), **kw):
        slot = self.dnext
        self.dnext = (self.dnext + 1) % NDS
        sid = "d%d" % slot
        if self.dcnt[slot] > 0:
            self._wait(q, sid, 16 * self.dcnt[slot])
        self._deps(q, reads, writes, merge)
        self.dcnt[slot] += 1
        self.E[q].dma_start(out=out, in_=in_, **kw).then_inc(self.semobj[sid], 16)
        self.ninstr += 1
        self._reg(sid, 16 * self.dcnt[slot], reads, writes, merge)

    def barrier(self, engines=None):
        engines = engines or list(self.E)
        for e in engines:
            for f in self.E:
                if f != e and self.cnt[f] > 0:
                    self._wait(e, f, self.cnt[f])
            for i in range(NDS):
                if self.dcnt[i] > 0:
                    self._wait(e, "d%d" % i, 16 * self.dcnt[i])


class Pool:
    def __init__(self, bufs):
        self.bufs = bufs
        self.i = 0

    def get(self):
        b = self.bufs[self.i]
        self.i = (self.i + 1) % len(self.bufs)
        return b


_UN = [0]


def un(name):
    _UN[0] += 1
    return "%s_u%d" % (name, _UN[0])


def build(nc, stage=99, dbg=None):
    st = ExitStack()
    k = KB(nc, st)
    V, A, P, T = nc.vector, nc.scalar, nc.gpsimd, nc.tensor

    def dram_in(name, shape, dt=F32):
        return nc.dram_tensor(name, list(shape), dt, kind="ExternalInput").ap()

    x_d = dram_in("x", [NB, S, D])
    out_d = nc.dram_tensor("out", [NB, S, D], F32, kind="ExternalOutput").ap()
    cT_d = dram_in("cT", [128, 8, NB])
    wmod_d = dram_in("w_mod", [L, 8, 128, 6 * D])
    bmod_d = dram_in("b_mod2", [L, NB, 6 * D])
    nrm_d = dram_in("nrm", [128, 2 * L, 8])
    normf_d = dram_in("normf_b", [128, D])
    bgate_d = dram_in("b_gate_t", [128, L, 24])
    convw_d = dram_in("conv_w_t", [128, L, 3, 2])
    ssmsc_d = dram_in("ssm_sc", [128, L, 8, 3])
    bpad_d = dram_in("bpad", [128, L, 8, 2, 32])
    cpad_d = dram_in("cpad", [128, L, 8, 2, 128])
    ssmd_d = dram_in("ssm_d_t", [128, L, 2])
    kaug_d = dram_in("kaug_c", [14, S], BF16)
    dtab_d = dram_in("dtab", [128, S], BF16)
    sctab_d = dram_in("sctab", [128, 8])
    qaug_d = dram_in("qaug_c", [16, 6, 1024], BF16)
    cU_d = dram_in("constU", [128, 128], BF16)
    cDneg_d = dram_in("constDneg", [128, 1024], BF16)
    cIrep_d = dram_in("constIrep", [128, 1024], BF16)
    ident_d = dram_in("ident", [128, 128])
    diagm_d = dram_in("diagmask", [128, 128])
    iota_d = dram_in("iota512", [128, 512])
    sel_d = dram_in("sel65", [65, 64])

    wspecs = {
        "w_in": (D, DIN_P),
        "w_conv_out": (256, D),
        "w_glu": (256, 2 * D),
        "w_attn_out": (512, D),
        "w_o": (D, D),
        "w_ffn_in": (D, 2 * DFF),
        "w_ffn_out": (DFF, D),
    }
    wf32 = {}
    wbf = {}
    wbuf = {}
    for name, (kk, nn) in wspecs.items():
        wf32[name] = dram_in(name, [L, kk, nn])
        wbf[name] = nc.dram_tensor(name + "_b", [L, kk, nn], BF16, kind="Internal").ap()
        for l in range(L):
            wbuf[(name, l)] = Buf(None, name + str(l))

    dbg_out = {}

    def dbg_tensor(name, shape, dt=F32):
        t = nc.dram_tensor("dbg_" + name, list(shape), dt, kind="ExternalOutput").ap()
        dbg_out[name] = t
        return t

    def sb(name, shape, dt=F32):
        return Buf(st.enter_context(nc.sbuf_tensor(un("S_" + name), list(shape), dt)), name)

    def ps(name, shape, dt=F32):
        return Buf(st.enter_context(nc.psum_tensor(un("P_" + name), list(shape), dt)), name)

    def prepass(l, name):
        kk, nn = wspecs[name]
        rows = kk * nn // 2048
        src = wf32[name][l].rearrange("k n -> (k n)").rearrange("(r c) -> r c", c=2048)
        dst = wbf[name][l].rearrange("k n -> (k n)").rearrange("(r c) -> r c", c=2048)
        sid = "pp_%s_%d" % (name, l)
        k.semobj[sid] = st.enter_context(nc.semaphore(sid))
        n_ = 0
        for r0 in range(0, rows, 1024):
            r1 = min(rows, r0 + 1024)
            P.dma_start(out=dst[r0:r1, :], in_=src[r0:r1, :]).then_inc(k.semobj[sid], 16)
            n_ += 1
        wbuf[(name, l)].w = {sid: 16 * n_}

    prepass(0, "w_in")

    xT = sb("xT", [128, 8, S])
    hT = sb("hT", [128, 8, S], BF16)
    hTb = [Buf(hT.t, "hT%d" % i) for i in range(4)]
    xTb = [Buf(xT.t, "xT%d" % i) for i in range(4)]

    def HB(sl):
        return hTb[sl.start // 512]

    def XB(sl):
        return xTb[sl.start // 512]
    ident = sb("ident", [128, 128])
    identb = sb("identb", [128, 128], BF16)
    onesb = sb("onesb", [128, 128], BF16)
    nrm = sb("nrm_s", [128, 2 * L, 8])
    modT = sb("modT", [128, L, 48, NB])
    esc = sb("esc", [128, L, 2, NB, 8])
    bgate = sb("bgate", [128, L, 24])
    convw = sb("convw", [128, L, 3, 2])
    ssmd = sb("ssmd", [128, L, 2])
    eps_t = sb("eps_t", [128, 1])
    halfpi = sb("halfpi", [128, 1])
    zero_t = sb("zero_t", [128, 1])

    for dst, src in ((ident, ident_d), (nrm, nrm_d), (bgate, bgate_d),
                     (convw, convw_d), (ssmd, ssmd_d)):
        k.dma("sp", dst[:], src, writes=[dst])
    k.op("dve", lambda: V.tensor_copy(out=identb[:], in_=ident[:]), [ident], [identb])
    k.op("dve", lambda: V.memset(onesb[:], 1.0 / D), [], [onesb])
    k.op("dve", lambda: V.memset(eps_t[:], 1e-6), [], [eps_t])
    k.op("dve", lambda: V.memset(halfpi[:], TWO_PI / 4), [], [halfpi])
    k.op("dve", lambda: V.memset(zero_t[:], 0.0), [], [zero_t])

    psb = [ps("psb%d" % i, [128, 512]) for i in range(8)]
    pp = Pool(psb)

    with ExitStack() as ph:
        def sbp(name, shape, dt=F32):
            return Buf(ph.enter_context(nc.sbuf_tensor(un(name), list(shape), dt)), name)
        cT = sbp("cT_s", [128, 8, NB])
        cact = sbp("cact", [128, 8, NB])
        modtok = sbp("modtok", [NB, 6 * D])
        bmod = sbp("bmod", [NB, 6 * D])
        wm = Pool([sbp("wm%d" % i, [128, 8, 512]) for i in range(2)])
        k.dma("sp", cT[:], cT_d, writes=[cT])
        k.op("act", lambda: A.activation(out=cact[:], in_=cT[:], func=AF.Silu), [cT], [cact])
        for l in range(L):
            k.dma("sp", bmod[:], bmod_d[l], writes=[bmod])
            for nb in range(12):
                w = wm.get()
                k.dma("sp", w[:], wmod_d[l, :, :, nb * 512:(nb + 1) * 512].rearrange("kc p n -> p kc n"),
                      writes=[w])
                pt = pp.get()
                for kc in range(8):
                    k.op("pe", lambda: T.matmul(pt[0:NB, :], lhsT=cact[:, kc, :], rhs=w[:, kc, :],
                                                start=(kc == 0), stop=(kc == 7)),
                         [cact, w], [pt], inc=(kc == 7))
                k.op("dve", lambda: V.tensor_tensor(out=modtok[:, nb * 512:(nb + 1) * 512], in0=pt[0:NB, :],
                                                    in1=bmod[:, nb * 512:(nb + 1) * 512], op=ALU.add),
                     [pt, bmod], [modtok])
            pt = pp.get()
            for c in range(48):
                k.op("pe", lambda: T.transpose(out=pt[:, c * NB:(c + 1) * NB], in_=modtok[:, c * 128:(c + 1) * 128],
                                               identity=ident[0:NB, 0:NB]),
                     [modtok, ident], [pt], inc=(c == 47))
            k.op("dve", lambda: V.tensor_copy(out=modT[:, l, :, :].rearrange("p c b -> p (c b)"),
                                              in_=pt[:, 0:48 * NB]), [pt], [modT])
            for j, (sci, ni) in enumerate(((1, 2 * l), (4, 2 * l + 1))):
                for b in range(NB):
                    k.op("dve", lambda: V.scalar_tensor_tensor(
                        out=esc[:, l, j, b, :], in0=modT[:, l, sci * 8:(sci + 1) * 8, b], scalar=1.0,
                        in1=nrm[:, ni, :], op0=ALU.add, op1=ALU.mult), [modT, nrm], [esc])
        k.barrier()

    for l in range(L):
        for name in wspecs:
            if not (l == 0 and name == "w_in"):
                prepass(l, name)
    if dbg is not None and "modT" in dbg:
        t = dbg_tensor("modT", [128, L * 48 * NB])
        k.dma("act", t, modT[:].rearrange("p l c b -> p (l c b)"), reads=[modT])
        t = dbg_tensor("esc", [128, L * 2 * NB * 8])
        k.dma("act", t, esc[:].rearrange("p l j b c -> p (l j b c)"), reads=[esc])

    def load_x(b):
        with ExitStack() as ph:
            xin = Pool([Buf(ph.enter_context(nc.sbuf_tensor(un("xin%d" % i), [128, D], F32))) for i in range(2)])
            for ti in range(16):
                xt = xin.get()
                k.dma("sp", xt[:], x_d[b, ti * 128:(ti + 1) * 128, :], writes=[xt])
                for half in range(2):
                    pt = pp.get()
                    for j in range(4):
                        kc = half * 4 + j
                        k.op("pe", lambda: T.transpose(out=pt[:, j * 128:(j + 1) * 128],
                                                       in_=xt[:, kc * 128:(kc + 1) * 128], identity=ident[:]),
                             [xt, ident], [pt], inc=(j == 3))
                    eng = "act" if half == 0 else "dve"
                    o = xT[:, half * 4:(half + 1) * 4, ti * 128:(ti + 1) * 128]
                    i_ = pt[:, :].rearrange("p (c t) -> p c t", c=4)
                    if eng == "act":
                        k.op("act", lambda: A.activation(out=o, in_=i_, func=AF.Copy), [pt], [xTb[ti // 4]])
                    else:
                        k.op("dve", lambda: V.tensor_copy(out=o, in_=i_), [pt], [xTb[ti // 4]])
            k.barrier()

    def norm_pools(ph):
        sqp = Pool([Buf(ph.enter_context(nc.sbuf_tensor(un("sq%d" % i), [128, 512], BF16))) for i in range(4)])
        rsp = Pool([Buf(ph.enter_context(nc.sbuf_tensor(un("rstd%d" % i), [128, 512], F32))) for i in range(1)])
        tmp = Pool([Buf(ph.enter_context(nc.sbuf_tensor(un("nmt%d" % i), [128, 512], F32))) for i in range(2)])
        return sqp, rsp, tmp

    def norm_tb(l, j, b, tb, pools):
        sqp, rsp, tmp = pools
        shi = 0 if j == 0 else 3
        cols = slice(tb * 512, (tb + 1) * 512)
        rstd = rsp.get()
        pt = pp.get()
        for kc in range(8):
            sq = sqp.get()
            k.op("act", lambda: A.activation(out=sq[:], in_=xT[:, kc, cols], func=AF.Square), [xTb[tb]], [sq])
            k.op("pe", lambda: T.matmul(pt[:, :], lhsT=onesb[:], rhs=sq[:], start=(kc == 0), stop=(kc == 7)),
                 [onesb, sq], [pt], inc=True)
        k.op("act", lambda: A.activation(out=rstd[:], in_=pt[:, :], func=AF.Sqrt, bias=eps_t[:], scale=1.0),
             [pt, eps_t], [rstd])
        k.op("dve", lambda: V.reciprocal(out=rstd[:], in_=rstd[:]), [rstd], [rstd])
        for kc in range(8):
            t_ = tmp.get()
            k.op("dve", lambda: V.tensor_tensor(out=t_[:], in0=xT[:, kc, cols], in1=rstd[:], op=ALU.mult),
                 [xTb[tb], rstd], [t_])
            k.op("act", lambda: A.activation(out=hT[:, kc, cols], in_=t_[:], func=AF.Identity,
                                             bias=modT[:, l, shi * 8 + kc, b:b + 1],
                                             scale=esc[:, l, j, b, kc:kc + 1]),
                 [t_, modT, esc], [hTb[tb]])

    def norm_mod(l, j, b, ph):
        pools = norm_pools(ph)
        for tb in range(4):
            norm_tb(l, j, b, tb, pools)

    def final_store(b):
        with ExitStack() as ph:
            normf_b = Buf(ph.enter_context(nc.sbuf_tensor(un("normf_bs"), [128, D], F32)))
            k.dma("sp", normf_b[:], normf_d, writes=[normf_b])
            ot = Pool([Buf(ph.enter_context(nc.sbuf_tensor(un("ot%d" % i), [128, D], F32))) for i in range(2)])
            junk = Buf(ph.enter_context(nc.sbuf_tensor(un("fjunk"), [128, D], BF16)))
            ss = Pool([Buf(ph.enter_context(nc.sbuf_tensor(un("ss%d" % i), [128, 2], F32))) for i in range(2)])
            rs = Pool([Buf(ph.enter_context(nc.sbuf_tensor(un("rs%d" % i), [128, 1], F32))) for i in range(2)])
            for ti in range(16):
                pts = []
                s_ = ss.get()
                for half in range(2):
                    pt = pp.get()
                    for j in range(4):
                        kc = half * 4 + j
                        k.op("pe", lambda: T.transpose(out=pt[:, j * 128:(j + 1) * 128],
                                                       in_=xT[:, kc, ti * 128:(ti + 1) * 128], identity=ident[:]),
                             [xTb[ti // 4], ident], [pt], inc=(j == 3))
                    k.op("act", lambda: A.activation(out=junk[:, half * 512:(half + 1) * 512], in_=pt[:, :],
                                                     func=AF.Square, accum_out=s_[:, half:half + 1]),
                         [pt], [junk, s_])
                    pts.append(pt)
                r_ = rs.get()
                k.op("dve", lambda: V.tensor_tensor(out=r_[:], in0=s_[:, 0:1], in1=s_[:, 1:2], op=ALU.add),
                     [s_], [r_])
                k.op("act", lambda: A.activation(out=r_[:], in_=r_[:], func=AF.Sqrt, bias=eps_t[:], scale=1.0 / D),
                     [r_, eps_t], [r_])
                k.op("dve", lambda: V.reciprocal(out=r_[:], in_=r_[:]), [r_], [r_])
                o = ot.get()
                for half in range(2):
                    k.op("dve", lambda: V.scalar_tensor_tensor(
                        out=o[:, half * 512:(half + 1) * 512], in0=pts[half][:, :], scalar=r_[:, 0:1],
                        in1=normf_b[:, half * 512:(half + 1) * 512], op0=ALU.mult, op1=ALU.mult),
                        [pts[half], r_, normf_b], [o])
                k.dma("act", out_d[b, ti * 128:(ti + 1) * 128, :], o[:], reads=[o])
            k.barrier()

    thrall = sb("thrall", [128, 1])
    k.op("dve", lambda: V.memset(thrall[:], -1e29), [], [thrall])

    theta = sb("theta", [128, L * 8])
    rdec = sb("rdec", [128, L * 8])
    coefr = sb("coefr", [128, L * 8])
    coefi = sb("coefi", [128, L * 8])
    with ExitStack() as ph:
        def sbp(name, shape, dt=F32):
            return Buf(ph.enter_context(nc.sbuf_tensor(un(name), list(shape), dt)), name)
        sc = sbp("ssc", [128, L * 8, 3])
        k.dma("sp", sc[:], ssmsc_d.rearrange("p l k c -> p (l k) c"), writes=[sc])
        ar, ai, ldt = sc[:, :, 0], sc[:, :, 1], sc[:, :, 2]
        tt = [sbp("sst%d" % i, [128, L * 8]) for i in range(10)]
        dt_, dtar, t1, kk, thr_, sn, ab, cs, abr, abi = tt
        k.op("act", lambda: A.activation(out=dt_[:], in_=ldt, func=AF.Exp), [sc], [dt_])
        k.op("dve", lambda: V.tensor_tensor(out=dtar[:], in0=dt_[:], in1=ar, op=ALU.mult), [dt_, sc], [dtar])
        k.op("dve", lambda: V.tensor_tensor(out=theta[:], in0=dt_[:], in1=ai, op=ALU.mult), [dt_, sc], [theta])
        k.op("act", lambda: A.activation(out=rdec[:], in_=dtar[:], func=AF.Exp), [dtar], [rdec])
        k.op("dve", lambda: V.tensor_scalar(out=t1[:], in0=theta[:], scalar1=1.0 / TWO_PI, scalar2=MAGIC,
                                            op0=ALU.mult, op1=ALU.add), [theta], [t1])
        k.op("dve", lambda: V.tensor_scalar(out=kk[:], in0=t1[:], scalar1=MAGIC, scalar2=-TWO_PI,
                                            op0=ALU.subtract, op1=ALU.mult), [t1], [kk])
        k.op("dve", lambda: V.tensor_tensor(out=thr_[:], in0=theta[:], in1=kk[:], op=ALU.add), [theta, kk], [thr_])
        k.op("dve", lambda: V.tensor_scalar(out=thr_[:], in0=thr_[:], scalar1=-3.141592, scalar2=3.141592,
                                            op0=ALU.max, op1=ALU.min), [thr_], [thr_])
        k.op("act", lambda: A.activation(out=sn[:], in_=thr_[:], func=AF.Sin), [thr_], [sn])
        k.op("dve", lambda: V.tensor_scalar(out=ab[:], in0=thr_[:], scalar1=TWO_PI / 4, scalar2=-TWO_PI,
                                            op0=ALU.is_gt, op1=ALU.mult), [thr_], [ab])
        k.op("dve", lambda: V.scalar_tensor_tensor(out=ab[:], in0=thr_[:], scalar=TWO_PI / 4, in1=ab[:],
                                                   op0=ALU.add, op1=ALU.add), [thr_, ab], [ab])
        k.op("dve", lambda: V.tensor_scalar(out=ab[:], in0=ab[:], scalar1=-3.141592, scalar2=3.141592,
                                            op0=ALU.max, op1=ALU.min), [ab], [ab])
        k.op("act", lambda: A.activation(out=cs[:], in_=ab[:], func=AF.Sin), [ab], [cs])
        k.op("dve", lambda: V.tensor_tensor(out=abr[:], in0=rdec[:], in1=cs[:], op=ALU.mult), [rdec, cs], [abr])
        k.op("dve", lambda: V.tensor_tensor(out=abi[:], in0=rdec[:], in1=sn[:], op=ALU.mult), [rdec, sn], [abi])
        u1, u2, den, nr = dt_, dtar, t1, kk
        k.op("dve", lambda: V.tensor_scalar(out=nr[:], in0=abr[:], scalar1=-1.0, scalar2=None, op0=ALU.add),
             [abr], [nr])
        k.op("dve", lambda: V.tensor_tensor(out=u1[:], in0=ar, in1=ar, op=ALU.mult), [sc], [u1])
        k.op("dve", lambda: V.tensor_tensor(out=u2[:], in0=ai, in1=ai, op=ALU.mult), [sc], [u2])
        k.op("dve", lambda: V.tensor_tensor(out=den[:], in0=u1[:], in1=u2[:], op=ALU.add), [u1, u2], [den])
        k.op("dve", lambda: V.reciprocal(out=den[:], in_=den[:]), [den], [den])
        k.op("dve", lambda: V.tensor_tensor(out=u1[:], in0=nr[:], in1=ar, op=ALU.mult), [nr, sc], [u1])
        k.op("dve", lambda: V.tensor_tensor(out=u2[:], in0=abi[:], in1=ai, op=ALU.mult), [abi, sc], [u2])
        k.op("dve", lambda: V.tensor_tensor(out=u1[:], in0=u1[:], in1=u2[:], op=ALU.add), [u1, u2], [u1])
        k.op("dve", lambda: V.tensor_tensor(out=coefr[:], in0=u1[:], in1=den[:], op=ALU.mult), [u1, den], [coefr])
        k.op("dve", lambda: V.tensor_tensor(out=u1[:], in0=abi[:], in1=ar, op=ALU.mult), [abi, sc], [u1])
        k.op("dve", lambda: V.tensor_tensor(out=u2[:], in0=nr[:], in1=ai, op=ALU.mult), [nr, sc], [u2])
        k.op("dve", lambda: V.tensor_tensor(out=u1[:], in0=u1[:], in1=u2[:], op=ALU.subtract), [u1, u2], [u1])
        k.op("dve", lambda: V.tensor_tensor(out=coefi[:], in0=u1[:], in1=den[:], op=ALU.mult), [u1, den], [coefi])
        k.barrier()
    if dbg is not None and "ssmsetup" in dbg:
        for nme, tl in (("theta", theta), ("rdec", rdec), ("coefr", coefr), ("coefi", coefi)):
            t = dbg_tensor(nme, [128, L * 8])
            k.dma("act", t, tl[:], reads=[tl])

    def wload(dst, name, l, c0, c1, kc=8, p=128, k0=0):
        src = wbf[name][l, k0:k0 + kc * p, c0:c1].rearrange("(kc p) n -> p kc n", p=p)
        k.dma("sp", dst[0:p, 0:kc, 0:c1 - c0], src, reads=[wbuf[(name, l)]], writes=[dst])

    def attn_phase(l, b, oT):
        with ExitStack() as ph:
            def sbp(name, shape, dt=F32):
                return Buf(ph.enter_context(nc.sbuf_tensor(un(name), list(shape), dt)), name)
            Lp = Pool(psb[0:4])
            Ob = [psb[4], psb[5]]
            Mp = Pool(psb[6:8])
            wQI = sbp("wQI", [128, 8, 256], BF16)
            wq = sbp("wq", [128, 8, 512], BF16)
            kaug = sbp("kaug", [128, S], BF16)
            kiT = sbp("kiT", [64, S], BF16)
            Vp = sbp("Vp", [128, 16, 65], BF16)
            wiT = sbp("wiT", [128, 16, 4])
            qps = Pool([sbp("qp%d" % i, [128, 1024], BF16) for i in range(3)])
            qis = Pool([sbp("qi%d" % i, [64, 512], BF16) for i in range(2)])
            Rts = Pool([sbp("Rt%d" % i, [128, 512]) for i in range(2)])
            pts = Pool([sbp("pt%d" % i, [128, 512], BF16) for i in range(3)])
            accs = Pool([sbp("accS%d" % i, [65, 512]) for i in range(1)])
            recs = Pool([sbp("rec%d" % i, [64, 512]) for i in range(1)])
            sm = Pool([sbp("sm%d" % i, [128, 8]) for i in range(8)])
            pw = sbp("pw", [128, 32])
            hss = Pool([sbp("hs%d" % i, [128, 64]) for i in range(2)])
            for it in range(BISECT_ITERS):
                k.op("dve", lambda: V.memset(pw[:, it:it + 1], 2.0 ** (-(it + 1))), [], [pw])
            cU = sbp("cU", [128, 128], BF16)
            cDneg = sbp("cDneg", [128, 1024], BF16)
            cIrep = sbp("cIrep", [128, 1024], BF16)
            diagm = sbp("diagm", [128, 128])
            sel65 = sbp("sel65", [65, 64])
            dtab = sbp("dtab", [128, S], BF16)
            sctab = sbp("sctab", [128, 8])
            dsh = sbp("dsh", [128, 128])
            ones8 = sbp("ones8", [128, 8])
            dm8s = Pool([sbp("dm8%d" % i, [128, 32]) for i in range(4)])
            ones32 = sbp("ones32", [128, 32])
            k.op("dve", lambda: V.memset(ones32[:], 1.0), [], [ones32])
            k.op("dve", lambda: V.memset(ones8[:], 1.0), [], [ones8])
            for dst, src in ((cU, cU_d), (cDneg, cDneg_d), (cIrep, cIrep_d), (diagm, diagm_d), (sel65, sel_d),
                             (dtab, dtab_d), (sctab, sctab_d)):
                k.dma("sp", dst[:], src, writes=[dst])
            wstg = ExitStack()
            wD = Buf(wstg.enter_context(nc.sbuf_tensor(un("wD"), [128, 8, 452], BF16)), "wD")
            wload(wD, "w_in", l, 1536, 1988)
            wload(wQI, "w_in", l, 1664, 1920)
            wload(wq, "w_in", l, 1024, 1536)
            k.dma("sp", kaug[64:78, :], kaug_d, merge=[kaug])
            k.op("dve", lambda: V.memset(Vp[:, :, 64:65], 1.0), [], [Vp])
            for tb in range(4):
                cols = slice(tb * 512, (tb + 1) * 512)
                for (c0, dst) in ((0, kaug), (384, kiT)):
                    pt = Mp.get()
                    for kc in range(8):
                        k.op("pe", lambda: T.matmul(pt[0:64, :], lhsT=wD[:, kc, c0:c0 + 64], rhs=hT[:, kc, cols],
                                                    start=(kc == 0), stop=(kc == 7)), [wD, HB(cols)], [pt], inc=(kc == 7))
                    k.op("act", lambda: A.activation(out=dst[0:64, cols], in_=pt[0:64, :], func=AF.Copy),
                         [pt], [], merge=[dst])
            for ti in range(16):
                tc_ = slice(ti * 128, (ti + 1) * 128)
                pt = Mp.get()
                for kc in range(8):
                    k.op("pe", lambda: T.matmul(pt[:, 0:64], lhsT=hT[:, kc, tc_], rhs=wD[:, kc, 64:128],
                                                start=(kc == 0), stop=(kc == 7)), [wD, HB(tc_)], [pt], inc=(kc == 7))
                k.op("dve", lambda: V.tensor_copy(out=Vp[:, ti, 0:64], in_=pt[:, 0:64]), [pt, Vp], [], merge=[Vp])
                pt2 = Mp.get()
                for kc in range(8):
                    k.op("pe", lambda: T.matmul(pt2[:, 0:4], lhsT=hT[:, kc, tc_], rhs=wD[:, kc, 448:452],
                                                start=(kc == 0), stop=(kc == 7)), [wD, HB(tc_)], [pt2], inc=(kc == 7))
                k.op("act", lambda: A.activation(out=wiT[:, ti, :], in_=pt2[:, 0:4], func=AF.Copy, scale=1.0 / 16),
                     [pt2], [wiT])

            k.barrier()
            wstg.close()
            idxs = Pool([sbp("idx%d" % i, [128, S]) for i in range(3)])
            mbs = Pool([sbp("mb%d" % i, [128, S], BF16) for i in range(2)])
            junk = sbp("junk", [128, S], mybir.dt.uint8)
            rkb = sbp("rkb", [128, S], mybir.dt.float16)
            if dbg is not None and "attn" in dbg and b == 0 and l == 0:
                t = dbg_tensor("wiT", [128, 64])
                k.dma("act", t, wiT[:].rearrange("p a c -> p (a c)"), reads=[wiT])
            F16 = mybir.dt.float16

            def stage_P(i):
                tc_ = slice(i * 128, (i + 1) * 128)
                nk = 128 * (i + 1)
                qp = qps.get()
                k.dma("sp", qp[72:78, :], qaug_d[i], merge=[qp])
                for half in range(2):
                    pt = Lp.get()
                    for hh in range(4):
                        h = half * 4 + hh
                        for kc in range(8):
                            k.op("pe", lambda: T.matmul(pt[0:64, hh * 128:(hh + 1) * 128],
                                                        lhsT=wq[:, kc, h * 64:(h + 1) * 64], rhs=hT[:, kc, tc_],
                                                        start=(kc == 0), stop=(kc == 7)),
                                 [wq, HB(tc_)], [pt], inc=(hh == 3 and kc == 7))
                    k.op("act", lambda: A.activation(out=qp[0:64, half * 512:(half + 1) * 512], in_=pt[0:64, :],
                                                     func=AF.Copy, scale=0.125), [pt], [], merge=[qp])
                qi = qis.get()
                pt = Lp.get()
                for hh in range(4):
                    for kc in range(8):
                        k.op("pe", lambda: T.matmul(pt[0:64, hh * 128:(hh + 1) * 128],
                                                    lhsT=wQI[:, kc, hh * 64:(hh + 1) * 64],
                                                    rhs=hT[:, kc, tc_], start=(kc == 0), stop=(kc == 7)),
                             [wQI, HB(tc_)], [pt], inc=(hh == 3 and kc == 7))
                k.op("act", lambda: A.activation(out=qi[:], in_=pt[0:64, :], func=AF.Copy), [pt], [qi])
                idx = idxs.get()
                for kb2 in range((nk + 511) // 512):
                    w_ = min(512, nk - kb2 * 512)
                    kc_ = slice(kb2 * 512, kb2 * 512 + w_)
                    for hh in range(4):
                        pt = Mp.get()
                        k.op("pe", lambda: T.matmul(pt[:, 0:w_], lhsT=qi[:, hh * 128:(hh + 1) * 128],
                                                    rhs=kiT[0:64, kc_], start=True, stop=True), [qi, kiT], [pt])
                        rt = Rts.get()
                        k.op("act", lambda: A.activation(out=rt[:, 0:w_], in_=pt[:, 0:w_], func=AF.Relu), [pt], [rt])
                        if hh == 0:
                            k.op("pool", lambda: P.tensor_scalar(out=idx[:, kc_], in0=rt[:, 0:w_],
                                                                 scalar1=wiT[:, i, 0:1], scalar2=0.0, op0=ALU.mult,
                                                                 op1=ALU.add), [rt, wiT], [idx])
                        else:
                            k.op("pool", lambda: P.tensor_scalar(out=rt[:, 0:w_], in0=rt[:, 0:w_],
                                                                 scalar1=wiT[:, i, hh:hh + 1], scalar2=0.0,
                                                                 op0=ALU.mult, op1=ALU.add), [rt, wiT], [rt])
                            k.op("pool", lambda: P.tensor_tensor(out=idx[:, kc_], in0=idx[:, kc_], in1=rt[:, 0:w_],
                                                                 op=ALU.add), [rt, idx], [idx])
                s_ = sm.get()
                return dict(i=i, nk=nk, qp=qp, idx=idx, s_=s_)

            def gen_A(c):
                i, nk, idx, s_ = c["i"], c["nk"], c["idx"], c["s_"]
                hi0, thr, cnt, e_ = s_[:, 0:1], s_[:, 1:2], s_[:, 2:3], s_[:, 3:4]
                if i >= 2:
                    k.op("dve", lambda: V.tensor_reduce(out=hi0, in_=idx[:, 0:nk], axis=AX.X, op=ALU.max,
                                                        apply_absolute_value=True), [idx], [s_])
                    yield
                    k.op("dve", lambda: V.tensor_scalar(out=hi0, in0=hi0, scalar1=1.01, scalar2=1e-6, op0=ALU.mult,
                                                        op1=ALU.add), [s_], [s_])
                    yield
                    k.op("dve", lambda: V.memset(thr, 0.0), [s_], [s_])
                    yield
                k.op("dve", lambda: V.tensor_tensor(out=idx[:, nk - 128:nk], in0=idx[:, nk - 128:nk], in1=diagm[:],
                                                    op=ALU.add), [idx, diagm], [idx])
                yield
                if i < 2:
                    return
                hs = hss.get()
                k.op("dve", lambda: V.tensor_scalar(out=hs[:, 0:BISECT_ITERS], in0=pw[:, 0:BISECT_ITERS],
                                                    scalar1=hi0, scalar2=None, op0=ALU.mult), [pw, s_], [hs])
                yield
                k.op("dve", lambda: V.tensor_scalar(out=hs[:, 32:32 + BISECT_ITERS], in0=pw[:, 0:BISECT_ITERS],
                                                    scalar1=hi0, scalar2=2.0, op0=ALU.mult, op1=ALU.mult),
                     [pw, s_], [hs])
                yield
                for it in range(BISECT_ITERS):
                    k.op("dve", lambda: V.tensor_scalar(out=junk[:, 0:nk], in0=idx[:, 0:nk], scalar1=thr,
                                                        scalar2=0.0, op0=ALU.is_ge, op1=ALU.add, accum_out=cnt),
                         [idx, s_], [junk, s_])
                    yield
                    k.op("dve", lambda: V.tensor_scalar(out=e_, in0=cnt, scalar1=255.5,
                                                        scalar2=hs[:, 32 + it:33 + it],
                                                        op0=ALU.is_ge, op1=ALU.mult), [s_, hs], [s_])
                    yield
                    k.op("dve", lambda: V.scalar_tensor_tensor(out=thr, in0=e_, scalar=hs[:, it:it + 1], in1=thr,
                                                               op0=ALU.subtract, op1=ALU.add), [s_, hs], [s_])
                    yield

            def gen_B(c):
                i, nk, idx, s_, qp = c["i"], c["nk"], c["idx"], c["s_"], c["qp"]
                mb = mbs.get()
                c["mb"] = mb
                if i >= 2:
                    hi0, thr = s_[:, 0:1], s_[:, 1:2]
                    lob, hib, chi, need = s_[:, 4:5], s_[:, 5:6], s_[:, 6:7], s_[:, 7:8]
                    cl = 2.0 ** (-BISECT_ITERS)
                    k.op("dve", lambda: V.scalar_tensor_tensor(out=lob, in0=hi0, scalar=-cl, in1=thr,
                                                               op0=ALU.mult, op1=ALU.add), [s_], [s_])
                    yield
                    k.op("dve", lambda: V.scalar_tensor_tensor(out=hib, in0=hi0, scalar=cl * 1.001, in1=thr,
                                                               op0=ALU.mult, op1=ALU.add), [s_], [s_])
                    yield
                    k.op("dve", lambda: V.tensor_scalar(out=hib, in0=hib, scalar1=1e-30, scalar2=None,
                                                        op0=ALU.add), [s_], [s_])
                    yield
                    k.op("dve", lambda: V.tensor_scalar(out=junk[:, 0:nk], in0=idx[:, 0:nk], scalar1=hib,
                                                        scalar2=0.0, op0=ALU.is_ge, op1=ALU.add, accum_out=chi),
                         [idx, s_], [junk, s_])
                    yield
                    k.op("dve", lambda: V.tensor_scalar(out=need, in0=chi, scalar1=-1.0, scalar2=256.0,
                                                        op0=ALU.mult, op1=ALU.add), [s_], [s_])
                    yield
                    k.op("dve", lambda: V.tensor_scalar(out=mb[:, 0:nk], in0=idx[:, 0:nk], scalar1=lob,
                                                        scalar2=None, op0=ALU.is_ge), [idx, s_], [mb])
                    yield
                    k.op("dve", lambda: V.scalar_tensor_tensor(out=mb[:, 0:nk], in0=idx[:, 0:nk], scalar=hib,
                                                               in1=mb[:, 0:nk], op0=ALU.is_lt, op1=ALU.mult),
                         [idx, s_, mb], [mb])
                    yield
                    k.op("dve", lambda: V.tensor_tensor_scan(out=rkb[:, 0:nk],
                                                             data0=ones8[:, 0:1].to_broadcast([128, nk]),
                                                             data1=mb[:, 0:nk], initial=0.0, op0=ALU.mult,
                                                             op1=ALU.add), [mb, ones8], [rkb])
                    yield
                    k.op("dve", lambda: V.scalar_tensor_tensor(out=rkb[:, 0:nk], in0=rkb[:, 0:nk], scalar=need,
                                                               in1=mb[:, 0:nk], op0=ALU.is_le, op1=ALU.mult),
                         [rkb, mb, s_], [rkb])
                    yield
                    k.op("dve", lambda: V.scalar_tensor_tensor(out=rkb[:, 0:nk], in0=idx[:, 0:nk], scalar=hib,
                                                               in1=rkb[:, 0:nk], op0=ALU.is_ge, op1=ALU.add),
                         [idx, s_, rkb], [rkb])
                    yield
                    k.op("dve", lambda: V.tensor_scalar(out=mb[:, 0:nk], in0=rkb[:, 0:nk], scalar1=-1.0,
                                                        scalar2=32768.0, op0=ALU.add, op1=ALU.mult), [rkb], [mb])
                    yield
                else:
                    k.op("dve", lambda: V.tensor_scalar(out=mb[:, 0:nk], in0=idx[:, 0:nk], scalar1=thrall[:, 0:1],
                                                        scalar2=-32768.0, op0=ALU.is_lt, op1=ALU.mult),
                         [idx, thrall], [mb])
                    yield
                s2 = sm.get()
                k.op("dve", lambda: V.scalar_tensor_tensor(out=rkb[:, 0:nk], in0=mb[:, 0:nk], scalar=-1.0,
                                                           in1=dtab[:, 1920 - 128 * i:2048], op0=ALU.mult,
                                                           op1=ALU.add), [mb, dtab], [rkb])
                yield
                k.op("dve", lambda: V.tensor_reduce(out=s2[:, 0:1], in_=rkb[:, 0:nk], axis=AX.X, op=ALU.min),
                     [rkb], [s2])
                yield
                dm32, tr32 = dm8s.get(), dm8s.get()
                k.op("dve", lambda: V.tensor_scalar(out=dm32[:], in0=ones32[:], scalar1=s2[:, 0:1], scalar2=None,
                                                    op0=ALU.mult), [ones32, s2], [dm32])
                yield
                k.op("dve", lambda: V.transpose(out=tr32[:], in_=dm32[:]), [dm32], [tr32])
                yield
                for bq in range(4):
                    k.op("dve", lambda: V.tensor_copy(out=dsh[64:72, bq * 32:(bq + 1) * 32],
                                                      in_=tr32[bq * 32:bq * 32 + 8, 0:32]), [tr32], [dsh])
                    yield
                for h in range(8):
                    k.op("dve", lambda: V.tensor_scalar(out=qp[64:72, h * 128:(h + 1) * 128], in0=dsh[64:72, :],
                                                        scalar1=sctab[64:72, h:h + 1], scalar2=None, op0=ALU.mult),
                         [dsh, sctab], [], merge=[qp])
                    yield
                if dbg is not None and "attn" in dbg and b == 0 and l == 0 and i in DBG_TILES:
                    t = dbg_tensor("idx%d" % i, [128, S])
                    k.dma("act", t, idx[:], reads=[idx])
                    t = dbg_tensor("mb%d" % i, [128, S], BF16)
                    k.dma("act", t, mb[:], reads=[mb])

            def stage_C(c):
                i, nk, qp, mb = c["i"], c["nk"], c["qp"], c["mb"]
                tc_ = slice(i * 128, (i + 1) * 128)
                pairs = [(j, half) for j in range(i + 1) for half in range(2)]
                Lts = {}

                def emit_L(n):
                    j, half = pairs[n]
                    kj = slice(j * 128, (j + 1) * 128)
                    hc = slice(half * 512, (half + 1) * 512)
                    Lt = Lp.get()
                    Lts[n] = Lt
                    k.op("pe", lambda: T.matmul(Lt[:, :], lhsT=kaug[0:78, kj], rhs=qp[0:78, hc],
                                                start=True, stop=False), [kaug, qp], [Lt], inc=False)
                    k.op("pe", lambda: T.matmul(Lt[:, :], lhsT=mb[:, kj], rhs=cIrep[:, hc],
                                                start=False, stop=(j != i)), [mb, cIrep], [Lt], inc=(j != i))
                    if j == i:
                        k.op("pe", lambda: T.matmul(Lt[:, :], lhsT=cU[:], rhs=cDneg[:, hc],
                                                    start=False, stop=True), [cU, cDneg], [Lt])

                def emit_exp_pv(n):
                    j, half = pairs[n]
                    Lt = Lts.pop(n)
                    p_ = pts.get()
                    k.op("act", lambda: A.activation(out=p_[:], in_=Lt[:, :], func=AF.Exp), [Lt], [p_])
                    k.op("pe", lambda: T.matmul(Ob[half][0:65, :], lhsT=Vp[:, j, 0:65], rhs=p_[:],
                                                start=(j == 0), stop=(j == i)), [Vp, p_], [Ob[half]],
                         inc=(j == i))

                for n in range(min(2, len(pairs))):
                    emit_L(n)
                for n in range(len(pairs)):
                    if n + 2 < len(pairs):
                        emit_L(n + 2)
                    emit_exp_pv(n)
                for half in range(2):
                    acc = accs.get()
                    k.op("act", lambda: A.activation(out=acc[0:65, :], in_=Ob[half][0:65, :], func=AF.Copy),
                         [Ob[half]], [acc])
                    pt = Mp.get()
                    k.op("pe", lambda: T.matmul(pt[0:64, :], lhsT=sel65[0:65, :], rhs=acc[0:65, :],
                                                start=True, stop=True), [sel65, acc], [pt])
                    rec = recs.get()
                    k.op("act", lambda: A.activation(out=rec[:], in_=pt[0:64, :], func=AF.Ln), [pt], [rec])
                    k.op("act", lambda: A.activation(out=rec[:], in_=rec[:], func=AF.Exp, scale=-1.0), [rec], [rec])
                    for par in range(2):
                        av = acc[0:64, :].rearrange("p (a q t) -> p a q t", a=2, q=2)[:, :, par, :]
                        rv = rec[:, :].rearrange("p (a q t) -> p a q t", a=2, q=2)[:, :, par, :]
                        k.op("pool", lambda: P.tensor_tensor(
                            out=oT[par * 64:(par + 1) * 64, half * 2:half * 2 + 2, tc_], in0=av, in1=rv,
                            op=ALU.mult), [acc, rec], [oT])

            def run(g):
                for _ in g:
                    pass

            def interleave(g1, g2):
                live = [g1, g2]
                while live:
                    for g in list(live):
                        try:
                            next(g)
                        except StopIteration:
                            live.remove(g)

            ctxs = {0: stage_P(0), 1: stage_P(1)}
            run(gen_A(ctxs[0]))
            for s_i in range(16):
                if s_i + 2 < 16:
                    ctxs[s_i + 2] = stage_P(s_i + 2)
                if s_i + 1 < 16:
                    interleave(gen_A(ctxs[s_i + 1]), gen_B(ctxs[s_i]))
                else:
                    run(gen_B(ctxs[s_i]))
                stage_C(ctxs[s_i])
            k.barrier()

    def conv_phase(l, b, uA):
        with ExitStack() as ph:
            def sbp(name, shape, dt=F32):
                return Buf(ph.enter_context(nc.sbuf_tensor(un(name), list(shape), dt)), name)
            wA = sbp("wA", [128, 8, 768], BF16)
            zt = sbp("zt", [128, S + 2])
            cgt = Pool([sbp("cgt%d" % i, [128, 512]) for i in range(2)])
            bgt = sbp("bgt", [128, S])
            yt = sbp("yt", [128, S])
            wload(wA, "w_in", l, 0, 768)
            k.op("dve", lambda: V.memset(zt[:, 0:2], 0.0), [], [zt])
            for j in range(2):
                for tb in range(4):
                    cols = slice(tb * 512, (tb + 1) * 512)
                    pc, pv, pb = pp.get(), pp.get(), pp.get()
                    for (pt_, c0) in ((pc, 256 + 128 * j), (pv, 512 + 128 * j), (pb, 128 * j)):
                        for kc in range(8):
                            k.op("pe", lambda: T.matmul(pt_[:, :], lhsT=wA[:, kc, c0:c0 + 128], rhs=hT[:, kc, cols],
                                                        start=(kc == 0), stop=(kc == 7)), [wA, HB(cols)], [pt_],
                                 inc=(kc == 7))
                    cg = cgt.get()
                    k.op("act", lambda: A.activation(out=cg[:], in_=pc[:, :], func=AF.Copy), [pc], [cg])
                    k.op("dve", lambda: V.tensor_tensor(out=zt[:, 2 + tb * 512:2 + (tb + 1) * 512], in0=pv[:, :],
                                                        in1=cg[:], op=ALU.mult), [pv, cg], [zt])
                    k.op("act", lambda: A.activation(out=bgt[:, cols], in_=pb[:, :], func=AF.Copy), [pb], [bgt])
                k.op("dve", lambda: V.tensor_scalar(out=yt[:], in0=zt[:, 2:S + 2], scalar1=convw[:, l, 2, j:j + 1],
                                                    scalar2=None, op0=ALU.mult), [zt, convw], [yt])
                k.op("dve", lambda: V.scalar_tensor_tensor(out=yt[:], in0=zt[:, 1:S + 1],
                                                           scalar=convw[:, l, 1, j:j + 1], in1=yt[:],
                                                           op0=ALU.mult, op1=ALU.add), [zt, convw, yt], [yt])
                k.op("dve", lambda: V.scalar_tensor_tensor(out=yt[:], in0=zt[:, 0:S],
                                                           scalar=convw[:, l, 0, j:j + 1], in1=yt[:],
                                                           op0=ALU.mult, op1=ALU.add), [zt, convw, yt], [yt])
                k.op("dve", lambda: V.tensor_tensor(out=uA[:, j, :], in0=bgt[:], in1=yt[:], op=ALU.mult),
                     [bgt, yt], [uA])
            k.barrier()

    def ssm_phase(l, b, zT):
        with ExitStack() as ph:
            def sbp(name, shape, dt=F32):
                return Buf(ph.enter_context(nc.sbuf_tensor(un(name), list(shape), dt)), name)
            vP = Pool(psb[0:4])
            yP = Pool(psb[4:6])
            mP = Pool(psb[6:8])
            wB = sbp("wB", [128, 8, 256], BF16)
            ubf = sbp("ubf", [128, 2, S], BF16)
            BbT = sbp("BbT", [128, 8, 2, 128], BF16)
            k.op("dve", lambda: V.memset(BbT[:], 0.0), [], [BbT])
            Cre = sbp("Cre", [128, 8, 128], BF16)
            nCre = sbp("nCre", [128, 8, 128], BF16)
            nCim = sbp("nCim", [128, 8, 128], BF16)
            gst = sbp("gst", [128, 8, 2])
            iota = sbp("iota", [128, 512])
            k.dma("sp", iota[:], iota_d, writes=[iota])
            stg = ExitStack()
            bp = Buf(stg.enter_context(nc.sbuf_tensor(un("bp"), [128, 8, 2, 32], F32)), "bp")
            cp = Buf(stg.enter_context(nc.sbuf_tensor(un("cp"), [128, 8, 2, 128], F32)), "cp")
            bbt = Pool([Buf(stg.enter_context(nc.sbuf_tensor(un("bbt"), [128, 32], F32)), "bbt") for i in range(3)])
            wload(wB, "w_in", l, 768, 1024)
            k.dma("sp", bp[:], bpad_d[:, l], writes=[bp])
            k.dma("sp", cp[:], cpad_d[:, l], writes=[cp])
            k.op("act", lambda: A.activation(out=Cre[:], in_=cp[:, :, 0, :], func=AF.Copy), [cp], [Cre])
            k.op("act", lambda: A.activation(out=nCre[:], in_=cp[:, :, 0, :], func=AF.Copy, scale=-1.0), [cp], [nCre])
            k.op("act", lambda: A.activation(out=nCim[:], in_=cp[:, :, 1, :], func=AF.Copy, scale=-1.0), [cp], [nCim])
            for kk_ in range(8):
                ci = l * 8 + kk_
                for ri in range(2):
                    t0, t1_ = bbt.get(), bbt.get()
                    if ri == 0:
                        k.op("dve", lambda: V.tensor_scalar(out=t0[:], in0=bp[:, kk_, 1, :], scalar1=coefi[:, ci:ci + 1],
                                                            scalar2=None, op0=ALU.mult), [bp, coefi], [t0])
                        k.op("dve", lambda: V.scalar_tensor_tensor(out=t1_[:], in0=bp[:, kk_, 0, :],
                                                                   scalar=coefr[:, ci:ci + 1], in1=t0[:],
                                                                   op0=ALU.mult, op1=ALU.subtract),
                             [bp, coefr, t0], [t1_])
                    else:
                        k.op("dve", lambda: V.tensor_scalar(out=t0[:], in0=bp[:, kk_, 0, :], scalar1=coefi[:, ci:ci + 1],
                                                            scalar2=None, op0=ALU.mult), [bp, coefi], [t0])
                        k.op("dve", lambda: V.scalar_tensor_tensor(out=t1_[:], in0=bp[:, kk_, 1, :],
                                                                   scalar=coefr[:, ci:ci + 1], in1=t0[:],
                                                                   op0=ALU.mult, op1=ALU.add),
                             [bp, coefr, t0], [t1_])
                    pt = mP.get()
                    k.op("pe", lambda: T.transpose(out=pt[0:32, 0:128], in_=t1_[:, :], identity=ident[:]),
                         [t1_, ident], [pt])
                    r0 = 32 * (kk_ % 4)
                    k.op("act", lambda: A.activation(out=BbT[r0:r0 + 32, kk_, ri, :], in_=pt[0:32, 0:128],
                                                     func=AF.Copy), [pt, BbT], [], merge=[BbT])
            k.barrier()
            stg.close()
            phs = Pool([sbp("phs%d" % i, [128, 512]) for i in range(2)])
            Ct = Pool([sbp("Ct%d" % i, [128, 512]) for i in range(2)])
            St = Pool([sbp("St%d" % i, [128, 512]) for i in range(2)])
            ms = Pool([sbp("ms%d" % i, [128, 512]) for i in range(8)])
            ws = Pool([sbp("ws%d" % i, [128, 512]) for i in range(4)])
            gs = Pool([sbp("gs%d" % i, [128, 512]) for i in range(2)])
            prs = Pool([sbp("pr%d" % i, [128, 512], BF16) for i in range(4)])
            yfs = Pool([sbp("yf%d" % i, [128, 512]) for i in range(1)])
            gt = Pool([sbp("gt%d" % i, [128, 512]) for i in range(2)])
            for kb in range(2):
                for tb in range(4):
                    cols = slice(tb * 512, (tb + 1) * 512)
                    pt = mP.get()
                    for kc in range(8):
                        k.op("pe", lambda: T.matmul(pt[:, :], lhsT=wB[:, kc, kb * 128:(kb + 1) * 128],
                                                    rhs=hT[:, kc, cols], start=(kc == 0), stop=(kc == 7)),
                             [wB, HB(cols)], [pt], inc=(kc == 7))
                    k.op("act", lambda: A.activation(out=ubf[:, kb, cols], in_=pt[:, :], func=AF.Copy), [pt], [ubf])
            ystate = {}

            def ssm_S1(tb, kk_):
                cols = slice(tb * 512, (tb + 1) * 512)
                ci = l * 8 + kk_
                kb = kk_ // 4
                p0, p1 = phs.get(), phs.get()
                k.op("dve", lambda: V.tensor_scalar(out=p0[:], in0=iota[:], scalar1=float(tb * 512),
                                                    scalar2=theta[:, ci:ci + 1], op0=ALU.add, op1=ALU.mult),
                     [iota, theta], [p0])
                k.op("dve", lambda: V.tensor_scalar(out=p1[:], in0=p0[:], scalar1=1.0 / TWO_PI, scalar2=MAGIC,
                                                    op0=ALU.mult, op1=ALU.add), [p0], [p1])
                k.op("dve", lambda: V.tensor_scalar(out=p1[:], in0=p1[:], scalar1=MAGIC, scalar2=-TWO_PI,
                                                    op0=ALU.subtract, op1=ALU.mult), [p1], [p1])
                k.op("dve", lambda: V.tensor_tensor(out=p0[:], in0=p0[:], in1=p1[:], op=ALU.add), [p0, p1], [p0])
                k.op("dve", lambda: V.tensor_scalar(out=p0[:], in0=p0[:], scalar1=-3.141592, scalar2=3.141592,
                                                    op0=ALU.max, op1=ALU.min), [p0], [p0])
                Sn, Cs = St.get(), Ct.get()
                k.op("act", lambda: A.activation(out=Sn[:], in_=p0[:], func=AF.Sin), [p0], [Sn])
                k.op("dve", lambda: V.tensor_scalar(out=p1[:], in0=p0[:], scalar1=TWO_PI / 4, scalar2=-TWO_PI,
                                                    op0=ALU.is_gt, op1=ALU.mult), [p0], [p1])
                k.op("dve", lambda: V.scalar_tensor_tensor(out=p1[:], in0=p0[:], scalar=TWO_PI / 4, in1=p1[:],
                                                           op0=ALU.add, op1=ALU.add), [p0, p1], [p1])
                k.op("dve", lambda: V.tensor_scalar(out=p1[:], in0=p1[:], scalar1=-3.141592, scalar2=3.141592,
                                                    op0=ALU.max, op1=ALU.min), [p1], [p1])
                k.op("act", lambda: A.activation(out=Cs[:], in_=p1[:], func=AF.Sin), [p1], [Cs])
                vr, vi = vP.get(), vP.get()
                for (vt, ri) in ((vr, 0), (vi, 1)):
                    k.op("pe", lambda: T.matmul(vt[:, :], lhsT=BbT[:, kk_, ri, :],
                                                rhs=ubf[:, kb, cols], start=True, stop=True),
                         [BbT, ubf], [vt])
                m1, m2, m3, m4 = ms.get(), ms.get(), ms.get(), ms.get()
                k.op("dve", lambda: V.tensor_tensor(out=m1[:], in0=vr[:, :], in1=Cs[:], op=ALU.mult), [vr, Cs], [m1])
                k.op("dve", lambda: V.tensor_tensor(out=m2[:], in0=vi[:, :], in1=Sn[:], op=ALU.mult), [vi, Sn], [m2])
                k.op("dve", lambda: V.tensor_tensor(out=m3[:], in0=vi[:, :], in1=Cs[:], op=ALU.mult), [vi, Cs], [m3])
                k.op("dve", lambda: V.tensor_tensor(out=m4[:], in0=vr[:, :], in1=Sn[:], op=ALU.mult), [vr, Sn], [m4])
                wr, wi_ = ws.get(), ws.get()
                k.op("pool", lambda: P.tensor_tensor(out=wr[:], in0=m1[:], in1=m2[:], op=ALU.add), [m1, m2], [wr])
                k.op("pool", lambda: P.tensor_tensor(out=wi_[:], in0=m3[:], in1=m4[:], op=ALU.subtract),
                     [m3, m4], [wi_])
                return dict(tb=tb, kk_=kk_, Sn=Sn, Cs=Cs, wr=wr, wi_=wi_)

            def ssm_S2(c):
                tb, kk_, Sn, Cs, wr, wi_ = c["tb"], c["kk_"], c["Sn"], c["Cs"], c["wr"], c["wi_"]
                cols = slice(tb * 512, (tb + 1) * 512)
                ci = l * 8 + kk_
                kb = kk_ // 4
                gr, gi = gs.get(), gs.get()
                dec = rdec[:, ci:ci + 1].to_broadcast([128, 512])
                for (g_, w__, ri) in ((gr, wr, 0), (gi, wi_, 1)):
                    init = 0.0 if tb == 0 else gst[:, kk_, ri:ri + 1]
                    k.op("dve", lambda: V.tensor_tensor_scan(out=g_[:], data0=dec, data1=w__[:], initial=init,
                                                             op0=ALU.mult, op1=ALU.add),
                         [rdec, w__, gst], [g_])
                    if tb < 3:
                        k.op("dve", lambda: V.tensor_copy(out=gst[:, kk_, ri:ri + 1], in_=g_[:, 511:512]),
                             [g_], [gst])
                if kk_ % 4 == 0:
                    ystate["ypt"] = yP.get()
                ypt = ystate["ypt"]
                combos = ((gr, Cs, Cre), (gi, Sn, nCre), (gi, Cs, nCim), (gr, Sn, nCim))
                for n_, (g_, tb_, cm) in enumerate(combos):
                    pr = prs.get()
                    k.op("pool", lambda: P.tensor_tensor(out=pr[:], in0=g_[:], in1=tb_[:], op=ALU.mult),
                         [g_, tb_], [pr])
                    k.op("pe", lambda: T.matmul(ypt[:, :], lhsT=cm[:, kk_, :], rhs=pr[:],
                                                start=(kk_ % 4 == 0 and n_ == 0), stop=(kk_ % 4 == 3 and n_ == 3)),
                         [cm, pr], [ypt], inc=(n_ == 3))
                if kk_ % 4 == 3:
                    yf = yfs.get()
                    k.op("dve", lambda: V.scalar_tensor_tensor(out=yf[:], in0=ubf[:, kb, cols],
                                                               scalar=ssmd[:, l, kb:kb + 1], in1=ypt[:, :],
                                                               op0=ALU.mult, op1=ALU.add), [ubf, ssmd, ypt], [yf])
                    if dbg is not None and "ssm" in dbg and b == 0 and l == 0:
                        t = dbg_tensor("y_%d_%d" % (tb, kb), [128, 512])
                        k.dma("act", t, yf[:], reads=[yf])
                    g1_, g2_ = gt.get(), gt.get()
                    k.op("dve", lambda: V.tensor_tensor(out=g1_[:], in0=yf[:], in1=yf[:], op=ALU.mult), [yf], [g1_])
                    k.op("dve", lambda: V.tensor_scalar(out=g1_[:], in0=g1_[:], scalar1=0.044715, scalar2=1.0,
                                                        op0=ALU.mult, op1=ALU.add), [g1_], [g1_])
                    k.op("dve", lambda: V.tensor_tensor(out=g1_[:], in0=g1_[:], in1=yf[:], op=ALU.mult),
                         [g1_, yf], [g1_])
                    k.op("act", lambda: A.activation(out=g2_[:], in_=g1_[:], func=AF.Sigmoid,
                                                     scale=1.5957691216057308), [g1_], [g2_])
                    k.op("dve", lambda: V.tensor_tensor(out=zT[:, kb, cols], in0=yf[:], in1=g2_[:], op=ALU.mult),
                         [yf, g2_], [zT])

            its = [(tb, kk_) for tb in range(4) for kk_ in range(8)]
            if SSM_PIPE:
                cur = ssm_S1(*its[0])
                for n_it in range(len(its)):
                    nxt = ssm_S1(*its[n_it + 1]) if n_it + 1 < len(its) else None
                    ssm_S2(cur)
                    cur = nxt
            else:
                for it_ in its:
                    ssm_S2(ssm_S1(*it_))
            k.barrier()

    def mix_phase(l, b, uA, zT, oT):
        with ExitStack() as ph:
            def sbp(name, shape, dt=F32):
                return Buf(ph.enter_context(nc.sbuf_tensor(un(name), list(shape), dt)), name)
            gseg = Pool([sbp("gseg%d" % i, [128, 8, 256], BF16) for i in range(6)])
            wcos = Pool([sbp("wco%d" % i, [128, 2, 256], BF16) for i in range(2)])
            wgas = Pool([sbp("wga%d" % i, [128, 2, 256], BF16) for i in range(2)])
            wgbs = Pool([sbp("wgb%d" % i, [128, 2, 256], BF16) for i in range(2)])
            waos = Pool([sbp("wao%d" % i, [128, 4, 256], BF16) for i in range(2)])
            wos = Pool([sbp("wo%d" % i, [128, 8, 512], BF16) for i in range(1)])
            mixs = Pool([sbp("mix%d" % i, [128, 8, 512], BF16) for i in range(1)])
            sg = Pool([sbp("sg%d" % i, [128, 512]) for i in range(4)])
            tm = Pool([sbp("tm%d" % i, [128, 512]) for i in range(3)])
            npools = norm_pools(ph) if FUSE_NORM else None
            for tb in range(4):
                cols = slice(tb * 512, (tb + 1) * 512)
                mix = mixs.get()
                for cg in range(4):
                    if cg == 2 and tb > 0 and npools is not None:
                        norm_tb(l, 1, b, tb - 1, npools)
                    segs = []
                    for br in range(3):
                        sgm = gseg.get()
                        c0 = 2048 + br * 1024 + cg * 256
                        wload(sgm, "w_in", l, c0, c0 + 256)
                        segs.append(sgm)
                    wco, wga, wgb, wao = wcos.get(), wgas.get(), wgbs.get(), waos.get()
                    wload(wco, "w_conv_out", l, cg * 256, (cg + 1) * 256, kc=2)
                    wload(wga, "w_glu", l, cg * 256, (cg + 1) * 256, kc=2)
                    wload(wgb, "w_glu", l, D + cg * 256, D + (cg + 1) * 256, kc=2)
                    wload(wao, "w_attn_out", l, cg * 256, (cg + 1) * 256, kc=4)
                    for cc in range(2):
                        c = cg * 2 + cc
                        mc = slice(cc * 128, (cc + 1) * 128)
                        gp = []
                        for br in range(3):
                            pt = pp.get()
                            for kc in range(8):
                                k.op("pe", lambda: T.matmul(pt[:, :], lhsT=segs[br][:, kc, mc], rhs=hT[:, kc, cols],
                                                            start=(kc == 0), stop=(kc == 7)), [segs[br], HB(cols)], [pt],
                                     inc=(kc == 7))
                            gp.append(pt)
                        sgs = []
                        for br in range(3):
                            s_ = sg.get()
                            k.op("act", lambda: A.activation(out=s_[:], in_=gp[br][:, :], func=AF.Sigmoid,
                                                             bias=bgate[:, l, br * 8 + c:br * 8 + c + 1], scale=1.0),
                                 [gp[br], bgate], [s_])
                            sgs.append(s_)
                        pya, pza, pzb, pyc = pp.get(), pp.get(), pp.get(), pp.get()
                        for kc in range(2):
                            k.op("pe", lambda: T.matmul(pya[:, :], lhsT=wco[:, kc, mc], rhs=uA[:, kc, cols],
                                                        start=(kc == 0), stop=(kc == 1)), [wco, uA], [pya], inc=(kc == 1))
                        for kc in range(2):
                            k.op("pe", lambda: T.matmul(pza[:, :], lhsT=wga[:, kc, mc], rhs=zT[:, kc, cols],
                                                        start=(kc == 0), stop=(kc == 1)), [wga, zT], [pza], inc=(kc == 1))
                        for kc in range(2):
                            k.op("pe", lambda: T.matmul(pzb[:, :], lhsT=wgb[:, kc, mc], rhs=zT[:, kc, cols],
                                                        start=(kc == 0), stop=(kc == 1)), [wgb, zT], [pzb], inc=(kc == 1))
                        for kc in range(4):
                            k.op("pe", lambda: T.matmul(pyc[:, :], lhsT=wao[:, kc, mc], rhs=oT[:, kc, cols],
                                                        start=(kc == 0), stop=(kc == 3)), [wao, oT], [pyc], inc=(kc == 3))
                        sz = sg.get()
                        k.op("act", lambda: A.activation(out=sz[:], in_=pzb[:, :], func=AF.Sigmoid), [pzb], [sz])
                        t1_, t2_, t3_ = tm.get(), tm.get(), tm.get()
                        k.op("dve", lambda: V.tensor_tensor(out=t1_[:], in0=pya[:, :], in1=sgs[0][:], op=ALU.mult),
                             [pya, sgs[0]], [t1_])
                        k.op("dve", lambda: V.tensor_tensor(out=t2_[:], in0=pza[:, :], in1=sz[:], op=ALU.mult),
                             [pza, sz], [t2_])
                        k.op("pool", lambda: P.tensor_tensor(out=t2_[:], in0=t2_[:], in1=sgs[1][:], op=ALU.mult),
                             [t2_, sgs[1]], [t2_])
                        k.op("dve", lambda: V.tensor_tensor(out=t3_[:], in0=pyc[:, :], in1=sgs[2][:], op=ALU.mult),
                             [pyc, sgs[2]], [t3_])
                        k.op("pool", lambda: P.tensor_tensor(out=t1_[:], in0=t1_[:], in1=t2_[:], op=ALU.add),
                             [t1_, t2_], [t1_])
                        k.op("pool", lambda: P.tensor_tensor(out=mix[:, c, :], in0=t1_[:], in1=t3_[:], op=ALU.add),
                             [t1_, t3_], [mix])
                for nh in range(2):
                    wo = wos.get()
                    wload(wo, "w_o", l, nh * 512, (nh + 1) * 512)
                    for nn in range(4):
                        n = nh * 4 + nn
                        pt = pp.get()
                        for c in range(8):
                            k.op("pe", lambda: T.matmul(pt[:, :], lhsT=wo[:, c, nn * 128:(nn + 1) * 128], rhs=mix[:, c, :],
                                                        start=(c == 0), stop=(c == 7)), [wo, mix], [pt], inc=(c == 7))
                        k.op("dve", lambda: V.scalar_tensor_tensor(out=xT[:, n, cols], in0=pt[:, :],
                                                                   scalar=modT[:, l, 2 * 8 + n, b:b + 1],
                                                                   in1=xT[:, n, cols],
                                                                   op0=ALU.mult, op1=ALU.add), [pt, modT, xTb[tb]], [xTb[tb]])
            if npools is not None:
                norm_tb(l, 1, b, 3, npools)
            k.barrier()

    def ffn_phase(l, b):
        with ExitStack() as ph:
            def sbp(name, shape, dt=F32):
                return Buf(ph.enter_context(nc.sbuf_tensor(un(name), list(shape), dt)), name)
            if not FUSE_NORM:
                with ExitStack() as ph2:
                    norm_mod(l, 1, b, ph2)
                    k.barrier()
            npools = norm_pools(ph) if FUSE_NORM else None
            fseg = Pool([sbp("fseg%d" % i, [128, 8, 512], BF16) for i in range(2)])
            wfo = sbp("wfo", [128, 22, D], BF16)
            acts = Pool([sbp("act%d" % i, [128, 22, 512], BF16) for i in range(1)])
            sg = Pool([sbp("fsg%d" % i, [128, 512]) for i in range(2)])
            wload(wfo, "w_ffn_out", l, 0, D, kc=22)
            for tb in range(4):
                cols = slice(tb * 512, (tb + 1) * 512)
                act = acts.get()
                for fg in range(11):
                    if fg == 5 and tb > 0 and npools is not None and l + 1 < L:
                        norm_tb(l + 1, 0, b, tb - 1, npools)
                    seg = fseg.get()
                    wload(seg, "w_ffn_in", l, fg * 512, (fg + 1) * 512)
                    for fc in range(2):
                        f = fg * 2 + fc
                        pg, pu = pp.get(), pp.get()
                        for (pt_, c0) in ((pg, fc * 128), (pu, 256 + fc * 128)):
                            for kc in range(8):
                                k.op("pe", lambda: T.matmul(pt_[:, :], lhsT=seg[:, kc, c0:c0 + 128], rhs=hT[:, kc, cols],
                                                            start=(kc == 0), stop=(kc == 7)), [seg, HB(cols)], [pt_],
                                     inc=(kc == 7))
                        s_ = sg.get()
                        k.op("act", lambda: A.activation(out=s_[:], in_=pg[:, :], func=AF.Silu), [pg], [s_])
                        k.op("dve", lambda: V.tensor_tensor(out=act[:, f, :], in0=pu[:, :], in1=s_[:], op=ALU.mult),
                             [pu, s_], [act])
                for n in range(8):
                    pt = pp.get()
                    for f in range(22):
                        k.op("pe", lambda: T.matmul(pt[:, :], lhsT=wfo[:, f, n * 128:(n + 1) * 128], rhs=act[:, f, :],
                                                    start=(f == 0), stop=(f == 21)), [wfo, act], [pt], inc=(f == 21))
                    k.op("dve", lambda: V.scalar_tensor_tensor(out=xT[:, n, cols], in0=pt[:, :],
                                                               scalar=modT[:, l, 5 * 8 + n, b:b + 1], in1=xT[:, n, cols],
                                                               op0=ALU.mult, op1=ALU.add), [pt, modT, xTb[tb]], [xTb[tb]])
            if npools is not None and l + 1 < L:
                norm_tb(l + 1, 0, b, 3, npools)
            k.barrier()

    def dump(name, buf, shape, dt, b, l):
        if dbg is not None and name in dbg and b == 0 and l == 0:
            t = dbg_tensor(name, shape, dt)
            pat = {2: "p a -> p a", 3: "p a s -> p (a s)"}[len(buf.t.shape)]
            k.dma("act", t, buf[:].rearrange(pat) if len(buf.t.shape) == 3 else buf[:], reads=[buf])

    for b in range(NB):
        load_x(b)
        for l in range(L if stage >= 2 else 0):
            if l == 0 or not FUSE_NORM or stage < 99:
                with ExitStack() as ph:
                    norm_mod(l, 0, b, ph)
                    k.barrier()
            dump("hT", hT, [128, 8 * S], BF16, b, l)
            if stage >= 3:
                with ExitStack() as lay:
                    def sbl(name, shape, dt=F32):
                        return Buf(lay.enter_context(nc.sbuf_tensor(un(name), list(shape), dt)), name)
                    oT = sbl("oT", [128, 4, S], BF16)
                    attn_phase(l, b, oT)
                    dump("oT", oT, [128, 4 * S], BF16, b, l)
                    if stage >= 4:
                        uA = sbl("uA", [128, 2, S], BF16)
                        conv_phase(l, b, uA)
                        dump("uA", uA, [128, 2 * S], BF16, b, l)
                    if stage >= 5:
                        zT = sbl("zT", [128, 2, S], BF16)
                        ssm_phase(l, b, zT)
                        dump("zT", zT, [128, 2 * S], BF16, b, l)
                    if stage >= 6:
                        mix_phase(l, b, uA, zT, oT)
                        dump("x1", xT, [128, 8 * S], F32, b, l)
                    k.barrier()
            if stage >= 7:
                ffn_phase(l, b)
                dump("x2", xT, [128, 8 * S], F32, b, l)
            if stage < 99 and l == 0:
                break
        final_store(b)
        if stage < 99:
            break

    k.barrier()
    st.close()
    return k, dbg_out


def _host_consts():
    c = {}
    s = np.arange(S)
    kaug = np.zeros((14, S), np.float32)
    kaug[0:8] = 1
    kaug[8] = 1
    kaug[9] = 1
    kaug[10] = ((s % 128) // 64) * 64
    kaug[11] = s % 64
    kaug[12] = 1
    kaug[13] = s // 128
    tq = np.arange(128)[:, None]
    uu = np.arange(S)[None, :]
    c["dtab"] = np.abs(tq - uu + 1920).astype(np.float32).astype(ml_dtypes.bfloat16)
    sct = np.zeros((128, 8), np.float32)
    sct[64:72] = np.diag(2.0 ** (-(np.arange(1, 9))))
    sct[0:8] = np.diag(2.0 ** (-(np.arange(1, 9))))
    c["sctab"] = sct
    c["kaug_c"] = kaug.astype(ml_dtypes.bfloat16)
    slopes = 2.0 ** (-(np.arange(1, 9)))
    tp = np.arange(128)
    qaug = np.zeros((16, 6, 8, 128), np.float32)
    for i in range(16):
        for h in range(8):
            qaug[i, 0, h] = -slopes[h] * ((tp // 64) * 64)
            qaug[i, 1, h] = -slopes[h] * (tp % 64)
            qaug[i, 2, h] = slopes[h]
            qaug[i, 3, h] = slopes[h]
            qaug[i, 4, h] = -slopes[h] * 128 * i
            qaug[i, 5, h] = slopes[h] * 128
    c["qaug_c"] = qaug.reshape(16, 6, 1024).astype(ml_dtypes.bfloat16)
    tt = tp[:, None]
    ss = tp[None, :]
    c["constU"] = np.maximum(ss - tt, 0).astype(np.float32).astype(ml_dtypes.bfloat16)
    dneg = np.zeros((128, 8, 128), np.float32)
    irep = np.zeros((128, 8, 128), np.float32)
    for h in range(8):
        dneg[:, h, :] = -2 * slopes[h] * np.eye(128)
        irep[:, h, :] = np.eye(128)
    c["constDneg"] = dneg.reshape(128, 1024).astype(ml_dtypes.bfloat16)
    c["constIrep"] = irep.reshape(128, 1024).astype(ml_dtypes.bfloat16)
    c["ident"] = np.eye(128, dtype=np.float32)
    dm = np.zeros((128, 128), np.float32)
    dm[:64, 64:] = NEG
    c["diagmask"] = dm
    c["iota512"] = np.broadcast_to(np.arange(512, dtype=np.float32), (128, 512)).copy()
    sel = np.zeros((65, 64), np.float32)
    sel[64, :] = 1.0
    c["sel65"] = sel
    return c


def _layout_common(inp):
    f = np.float32
    m = {}
    m["w_mod"] = np.ascontiguousarray(inp["w_mod"], f).reshape(L, 8, 128, 6 * D)
    nrm = np.stack([inp["norm1"][0], inp["norm2"][0], inp["norm1"][1], inp["norm2"][1]], 0)
    m["nrm"] = np.ascontiguousarray(nrm.reshape(2 * L, 8, 128).transpose(2, 0, 1), f)
    m["normf_b"] = np.ascontiguousarray(np.broadcast_to(inp["norm_f"][None, :], (128, D)), f)
    w_in = np.zeros((L, D, DIN_P), f)
    w_in[:, :, 0:1988] = inp["w_in"][:, :, 0:1988]
    w_in[:, :, 2048:5120] = inp["w_in"][:, :, 1988:5060]
    m["w_in"] = w_in
    m["b_gate_t"] = np.ascontiguousarray(inp["b_gate"].reshape(L, 24, 128).transpose(2, 0, 1), f)
    m["conv_w_t"] = np.ascontiguousarray(inp["conv_w"].reshape(L, 3, 2, 128).transpose(3, 0, 1, 2), f)
    for nme in ("w_conv_out", "w_glu", "w_attn_out", "w_o", "w_ffn_out"):
        m[nme] = np.ascontiguousarray(inp[nme], f)
    wf = inp["w_ffn_in"]
    wfr = np.zeros((L, D, 2 * DFF), f)
    for g in range(11):
        wfr[:, :, g * 512:g * 512 + 256] = wf[:, :, g * 256:(g + 1) * 256]
        wfr[:, :, g * 512 + 256:(g + 1) * 512] = wf[:, :, DFF + g * 256:DFF + (g + 1) * 256]
    m["w_ffn_in"] = wfr
    sc = np.zeros((128, L, 8, 3), f)
    for l in range(L):
        for g in range(16):
            kk, gl = g // 2, g % 2
            sc[gl * 64:(gl + 1) * 64, l, kk, 0] = inp["ssm_a_re"][l, g]
            sc[gl * 64:(gl + 1) * 64, l, kk, 1] = inp["ssm_a_im"][l, g]
            sc[gl * 64:(gl + 1) * 64, l, kk, 2] = inp["ssm_log_dt"][l, g]
    m["ssm_sc"] = sc
    bpad = np.zeros((128, L, 8, 2, 32), f)
    cpad = np.zeros((128, L, 8, 2, 128), f)
    for l in range(L):
        for g in range(16):
            kk, gl = g // 2, g % 2
            rows = slice(gl * 64, (gl + 1) * 64)
            bpad[rows, l, kk, 0, gl * 16:(gl + 1) * 16] = inp["ssm_b_re"][l, g]
            bpad[rows, l, kk, 1, gl * 16:(gl + 1) * 16] = inp["ssm_b_im"][l, g]
            c0 = 32 * (kk % 4) + gl * 16
            cpad[rows, l, kk, 0, c0:c0 + 16] = inp["ssm_c_re"][l, g].T
            cpad[rows, l, kk, 1, c0:c0 + 16] = inp["ssm_c_im"][l, g].T
    m["bpad"] = bpad
    m["cpad"] = cpad
    m["ssm_d_t"] = np.ascontiguousarray(inp["ssm_d"].reshape(L, 2, 128).transpose(2, 0, 1), f)
    m.update(_host_consts())
    return m


def _layout_core(inp, core):
    f = np.float32
    rows = slice(core * NB, (core + 1) * NB)
    m = {}
    m["x"] = np.ascontiguousarray(inp["x"][rows], f)
    m["cT"] = np.ascontiguousarray(inp["c"][rows].reshape(NB, 8, 128).transpose(2, 1, 0), f)
    m["b_mod2"] = np.ascontiguousarray(np.broadcast_to(inp["b_mod"][:, None, :], (L, NB, 6 * D)), f)
    return m


_CACHE = {}


def kernel(**inputs):
    inp = {k_: np.asarray(v) for k_, v in inputs.items()}
    if "nc" not in _CACHE:
        nc = bass.Bass("TRN2", target_bir_lowering=False)
        build(nc)
        _CACHE["nc"] = nc
    nc = _CACHE["nc"]
    common = _layout_common(inp)
    in_maps = []
    for core in range(NCORES):
        m = dict(common)
        m.update(_layout_core(inp, core))
        in_maps.append(m)
    res = run_bass_kernel_spmd(nc, in_maps, core_ids=list(range(NCORES)))
    out = np.concatenate([np.asarray(r["out"], np.float32) for r in res.results], axis=0)
    return out
```

```python
import numpy as np
import ml_dtypes
from contextlib import ExitStack
import concourse.bass as bass
import concourse.mybir as mybir
from concourse.bass_utils import run_bass_kernel_spmd

F32 = mybir.dt.float32
BF16 = mybir.dt.bfloat16
AF = mybir.ActivationFunctionType
ALU = mybir.AluOpType
AX = mybir.AxisListType

S = 2048
D = 1024
NB = 2
L = 2
DFF = 2816
DIN_P = 5120
NCORES = 8
NEG = -1e30
NDS = 24
BISECT_ITERS = 12
FUSE_NORM = True
SSM_PIPE = True
DBG_TILES = (3, 5, 8, 14)
TWO_PI = 6.283185307179586
MAGIC = 12582912.0


class Buf:
    __slots__ = ("t", "w", "r", "name")

    def __init__(self, t, name=""):
        self.t = t
        self.w = {}
        self.r = {}
        self.name = name

    def __getitem__(self, idx):
        return self.t[idx]


class KB:
    def __init__(self, nc, st):
        self.nc = nc
        self.E = {"pe": nc.tensor, "act": nc.scalar, "dve": nc.vector, "pool": nc.gpsimd, "sp": nc.sync}
        self.semobj = {}
        self.cnt = {e: 0 for e in self.E}
        self.seen = {e: {} for e in self.E}
        for e in self.E:
            self.semobj[e] = st.enter_context(nc.semaphore("s_" + e))
        self.dcnt = [0] * NDS
        self.dnext = 0
        for i in range(NDS):
            self.semobj["d%d" % i] = st.enter_context(nc.semaphore("dq%d" % i))
        self.ninstr = 0

    def _wait(self, eng, sid, val):
        if self.seen[eng].get(sid, 0) >= val:
            return
        self.E[eng].wait_ge(self.semobj[sid], val)
        self.seen[eng][sid] = val

    def _deps(self, eng, reads, writes, merge):
        for b in reads:
            for sid, val in b.w.items():
                if sid == eng and eng in ("pe", "sp"):
                    continue
                self._wait(eng, sid, val)
        strict = eng in ("act", "dve", "pool")
        for b in list(writes) + list(merge):
            if b not in merge:
                for sid, val in b.w.items():
                    if sid != eng or strict:
                        self._wait(eng, sid, val)
            for sid, val in b.r.items():
                if sid != eng or strict:
                    self._wait(eng, sid, val)

    def _reg(self, sid, val, reads, writes, merge):
        for b in reads:
            if b.r.get(sid, 0) < val:
                b.r[sid] = val
        for b in writes:
            b.w = {sid: val}
            b.r = {}
        for b in merge:
            b.w[sid] = val
            b.r = {}

    def op(self, eng, fn, reads=(), writes=(), inc=True, merge=()):
        self._deps(eng, reads, writes, merge)
        ins = fn()
        self.ninstr += 1
        if inc:
            self.cnt[eng] += 1
            ins.then_inc(self.semobj[eng], 1)
            val = self.cnt[eng]
        else:
            val = self.cnt[eng] + 1
        self._reg(eng, val, reads, writes, merge)
        return ins

    def dma(self, q, out, in_, reads=(), writes=(), merge=(), **kw):
        slot = self.dnext
        self.dnext = (self.dnext + 1) % NDS
        sid = "d%d" % slot
        if self.dcnt[slot] > 0:
            self._wait(q, sid, 16 * self.dcnt[slot])
        self._deps(q, reads, writes, merge)
        self.dcnt[slot] += 1
        self.E[q].dma_start(out=out, in_=in_, **kw).then_inc(self.semobj[sid], 16)
        self.ninstr += 1
        self._reg(sid, 16 * self.dcnt[slot], reads, writes, merge)

    def barrier(self, engines=None):
        engines = engines or list(self.E)
        for e in engines:
            for f in self.E:
                if f != e and self.cnt[f] > 0:
                    self._wait(e, f, self.cnt[f])
            for i in range(NDS):
                if self.dcnt[i] > 0:
                    self._wait(e, "d%d" % i, 16 * self.dcnt[i])


class Pool:
    def __init__(self, bufs):
        self.bufs = bufs
        self.i = 0

    def get(self):
        b = self.bufs[self.i]
        self.i = (self.i + 1) % len(self.bufs)
        return b


_UN = [0]


def un(name):
    _UN[0] += 1
    return "%s_u%d" % (name, _UN[0])


def build(nc, stage=99, dbg=None):
    st = ExitStack()
    k = KB(nc, st)
    V, A, P, T = nc.vector, nc.scalar, nc.gpsimd, nc.tensor

    def dram_in(name, shape, dt=F32):
        return nc.dram_tensor(name, list(shape), dt, kind="ExternalInput").ap()

    x_d = dram_in("x", [NB, S, D])
    out_d = nc.dram_tensor("out", [NB, S, D], F32, kind="ExternalOutput").ap()
    cT_d = dram_in("cT", [128, 8, NB])
    wmod_d = dram_in("w_mod", [L, 8, 128, 6 * D])
    bmod_d = dram_in("b_mod2", [L, NB, 6 * D])
    nrm_d = dram_in("nrm", [128, 2 * L, 8])
    normf_d = dram_in("normf_b", [128, D])
    bgate_d = dram_in("b_gate_t", [128, L, 24])
    convw_d = dram_in("conv_w_t", [128, L, 3, 2])
    ssmsc_d = dram_in("ssm_sc", [128, L, 8, 3])
    bpad_d = dram_in("bpad", [128, L, 8, 2, 32])
    cpad_d = dram_in("cpad", [128, L, 8, 2, 128])
    ssmd_d = dram_in("ssm_d_t", [128, L, 2])
    kaug_d = dram_in("kaug_c", [14, S], BF16)
    dtab_d = dram_in("dtab", [128, S], BF16)
    sctab_d = dram_in("sctab", [128, 8])
    qaug_d = dram_in("qaug_c", [16, 6, 1024], BF16)
    cU_d = dram_in("constU", [128, 128], BF16)
    cDneg_d = dram_in("constDneg", [128, 1024], BF16)
    cIrep_d = dram_in("constIrep", [128, 1024], BF16)
    ident_d = dram_in("ident", [128, 128])
    diagm_d = dram_in("diagmask", [128, 128])
    iota_d = dram_in("iota512", [128, 512])
    sel_d = dram_in("sel65", [65, 64])

    wspecs = {
        "w_in": (D, DIN_P),
        "w_conv_out": (256, D),
        "w_glu": (256, 2 * D),
        "w_attn_out": (512, D),
        "w_o": (D, D),
        "w_ffn_in": (D, 2 * DFF),
        "w_ffn_out": (DFF, D),
    }
    wf32 = {}
    wbf = {}
    wbuf = {}
    for name, (kk, nn) in wspecs.items():
        wf32[name] = dram_in(name, [L, kk, nn])
        wbf[name] = nc.dram_tensor(name + "_b", [L, kk, nn], BF16, kind="Internal").ap()
        for l in range(L):
            wbuf[(name, l)] = Buf(None, name + str(l))

    dbg_out = {}

    def dbg_tensor(name, shape, dt=F32):
        t = nc.dram_tensor("dbg_" + name, list(shape), dt, kind="ExternalOutput").ap()
        dbg_out[name] = t
        return t

    def sb(name, shape, dt=F32):
        return Buf(st.enter_context(nc.sbuf_tensor(un("S_" + name), list(shape), dt)), name)

    def ps(name, shape, dt=F32):
        return Buf(st.enter_context(nc.psum_tensor(un("P_" + name), list(shape), dt)), name)

    def prepass(l, name):
        kk, nn = wspecs[name]
        rows = kk * nn // 2048
        src = wf32[name][l].rearrange("k n -> (k n)").rearrange("(r c) -> r c", c=2048)
        dst = wbf[name][l].rearrange("k n -> (k n)").rearrange("(r c) -> r c", c=2048)
        sid = "pp_%s_%d" % (name, l)
        k.semobj[sid] = st.enter_context(nc.semaphore(sid))
        n_ = 0
        for r0 in range(0, rows, 1024):
            r1 = min(rows, r0 + 1024)
            P.dma_start(out=dst[r0:r1, :], in_=src[r0:r1, :]).then_inc(k.semobj[sid], 16)
            n_ += 1
        wbuf[(name, l)].w = {sid: 16 * n_}

    prepass(0, "w_in")

    xT = sb("xT", [128, 8, S])
    hT = sb("hT", [128, 8, S], BF16)
    hTb = [Buf(hT.t, "hT%d" % i) for i in range(4)]
    xTb = [Buf(xT.t, "xT%d" % i) for i in range(4)]

    def HB(sl):
        return hTb[sl.start // 512]

    def XB(sl):
        return xTb[sl.start // 512]
    ident = sb("ident", [128, 128])
    identb = sb("identb", [128, 128], BF16)
    onesb = sb("onesb", [128, 128], BF16)
    nrm = sb("nrm_s", [128, 2 * L, 8])
    modT = sb("modT", [128, L, 48, NB])
    esc = sb("esc", [128, L, 2, NB, 8])
    bgate = sb("bgate", [128, L, 24])
    convw = sb("convw", [128, L, 3, 2])
    ssmd = sb("ssmd", [128, L, 2])
    eps_t = sb("eps_t", [128, 1])
    halfpi = sb("halfpi", [128, 1])
    zero_t = sb("zero_t", [128, 1])

    for dst, src in ((ident, ident_d), (nrm, nrm_d), (bgate, bgate_d),
                     (convw, convw_d), (ssmd, ssmd_d)):
        k.dma("sp", dst[:], src, writes=[dst])
    k.op("dve", lambda: V.tensor_copy(out=identb[:], in_=ident[:]), [ident], [identb])
    k.op("dve", lambda: V.memset(onesb[:], 1.0 / D), [], [onesb])
    k.op("dve", lambda: V.memset(eps_t[:], 1e-6), [], [eps_t])
    k.op("dve", lambda: V.memset(halfpi[:], TWO_PI / 4), [], [halfpi])
    k.op("dve", lambda: V.memset(zero_t[:], 0.0), [], [zero_t])

    psb = [ps("psb%d" % i, [128, 512]) for i in range(8)]
    pp = Pool(psb)

    def adaln_and_prepass():
        with ExitStack() as ph:
            def sbp(name, shape, dt=F32):
                return Buf(ph.enter_context(nc.sbuf_tensor(un(name), list(shape), dt)), name)
            cT = sbp("cT_s", [128, 8, NB])
            cact = sbp("cact", [128, 8, NB])
            modtok = sbp("modtok", [NB, 6 * D])
            bmod = sbp("bmod", [NB, 6 * D])
            wm = Pool([sbp("wm%d" % i, [128, 8, 512]) for i in range(2)])
            k.dma("sp", cT[:], cT_d, writes=[cT])
            k.op("act", lambda: A.activation(out=cact[:], in_=cT[:], func=AF.Silu), [cT], [cact])
            for l in range(L):
                k.dma("sp", bmod[:], bmod_d[l], writes=[bmod])
                for nb in range(12):
                    w = wm.get()
                    k.dma("sp", w[:], wmod_d[l, :, :, nb * 512:(nb + 1) * 512].rearrange("kc p n -> p kc n"),
                          writes=[w])
                    pt = pp.get()
                    for kc in range(8):
                        k.op("pe", lambda: T.matmul(pt[0:NB, :], lhsT=cact[:, kc, :], rhs=w[:, kc, :],
                                                    start=(kc == 0), stop=(kc == 7)),
                             [cact, w], [pt], inc=(kc == 7))
                    k.op("dve", lambda: V.tensor_tensor(out=modtok[:, nb * 512:(nb + 1) * 512], in0=pt[0:NB, :],
                                                        in1=bmod[:, nb * 512:(nb + 1) * 512], op=ALU.add),
                         [pt, bmod], [modtok])
                pt = pp.get()
                for c in range(48):
                    k.op("pe", lambda: T.transpose(out=pt[:, c * NB:(c + 1) * NB], in_=modtok[:, c * 128:(c + 1) * 128],
                                                   identity=ident[0:NB, 0:NB]),
                         [modtok, ident], [pt], inc=(c == 47))
                k.op("dve", lambda: V.tensor_copy(out=modT[:, l, :, :].rearrange("p c b -> p (c b)"),
                                                  in_=pt[:, 0:48 * NB]), [pt], [modT])
                for j, (sci, ni) in enumerate(((1, 2 * l), (4, 2 * l + 1))):
                    for b in range(NB):
                        k.op("dve", lambda: V.scalar_tensor_tensor(
                            out=esc[:, l, j, b, :], in0=modT[:, l, sci * 8:(sci + 1) * 8, b], scalar=1.0,
                            in1=nrm[:, ni, :], op0=ALU.add, op1=ALU.mult), [modT, nrm], [esc])
            k.barrier()

        for l in range(L):
            for name in wspecs:
                if not (l == 0 and name == "w_in"):
                    prepass(l, name)
        if dbg is not None and "modT" in dbg:
            t = dbg_tensor("modT", [128, L * 48 * NB])
            k.dma("act", t, modT[:].rearrange("p l c b -> p (l c b)"), reads=[modT])
            t = dbg_tensor("esc", [128, L * 2 * NB * 8])
            k.dma("act", t, esc[:].rearrange("p l j b c -> p (l j b c)"), reads=[esc])


    def load_x(b):
        with ExitStack() as ph:
            xin = Pool([Buf(ph.enter_context(nc.sbuf_tensor(un("xin%d" % i), [128, D], F32))) for i in range(2)])
            for ti in range(16):
                xt = xin.get()
                k.dma("sp", xt[:], x_d[b, ti * 128:(ti + 1) * 128, :], writes=[xt])
                for half in range(2):
                    pt = pp.get()
                    for j in range(4):
                        kc = half * 4 + j
                        k.op("pe", lambda: T.transpose(out=pt[:, j * 128:(j + 1) * 128],
                                                       in_=xt[:, kc * 128:(kc + 1) * 128], identity=ident[:]),
                             [xt, ident], [pt], inc=(j == 3))
                    eng = "act" if half == 0 else "dve"
                    o = xT[:, half * 4:(half + 1) * 4, ti * 128:(ti + 1) * 128]
                    i_ = pt[:, :].rearrange("p (c t) -> p c t", c=4)
                    if eng == "act":
                        k.op("act", lambda: A.activation(out=o, in_=i_, func=AF.Copy), [pt], [xTb[ti // 4]])
                    else:
                        k.op("dve", lambda: V.tensor_copy(out=o, in_=i_), [pt], [xTb[ti // 4]])
            k.barrier()

    def norm_pools(ph):
        sqp = Pool([Buf(ph.enter_context(nc.sbuf_tensor(un("sq%d" % i), [128, 512], BF16))) for i in range(4)])
        rsp = Pool([Buf(ph.enter_context(nc.sbuf_tensor(un("rstd%d" % i), [128, 512], F32))) for i in range(1)])
        tmp = Pool([Buf(ph.enter_context(nc.sbuf_tensor(un("nmt%d" % i), [128, 512], F32))) for i in range(2)])
        return sqp, rsp, tmp

    def norm_tb(l, j, b, tb, pools):
        sqp, rsp, tmp = pools
        shi = 0 if j == 0 else 3
        cols = slice(tb * 512, (tb + 1) * 512)
        rstd = rsp.get()
        pt = pp.get()
        for kc in range(8):
            sq = sqp.get()
            k.op("act", lambda: A.activation(out=sq[:], in_=xT[:, kc, cols], func=AF.Square), [xTb[tb]], [sq])
            k.op("pe", lambda: T.matmul(pt[:, :], lhsT=onesb[:], rhs=sq[:], start=(kc == 0), stop=(kc == 7)),
                 [onesb, sq], [pt], inc=True)
        k.op("act", lambda: A.activation(out=rstd[:], in_=pt[:, :], func=AF.Sqrt, bias=eps_t[:], scale=1.0),
             [pt, eps_t], [rstd])
        k.op("dve", lambda: V.reciprocal(out=rstd[:], in_=rstd[:]), [rstd], [rstd])
        for kc in range(8):
            t_ = tmp.get()
            k.op("dve", lambda: V.tensor_tensor(out=t_[:], in0=xT[:, kc, cols], in1=rstd[:], op=ALU.mult),
                 [xTb[tb], rstd], [t_])
            k.op("act", lambda: A.activation(out=hT[:, kc, cols], in_=t_[:], func=AF.Identity,
                                             bias=modT[:, l, shi * 8 + kc, b:b + 1],
                                             scale=esc[:, l, j, b, kc:kc + 1]),
                 [t_, modT, esc], [hTb[tb]])

    def norm_mod(l, j, b, ph):
        pools = norm_pools(ph)
        for tb in range(4):
            norm_tb(l, j, b, tb, pools)

    def final_store(b):
        with ExitStack() as ph:
            normf_b = Buf(ph.enter_context(nc.sbuf_tensor(un("normf_bs"), [128, D], F32)))
            k.dma("sp", normf_b[:], normf_d, writes=[normf_b])
            ot = Pool([Buf(ph.enter_context(nc.sbuf_tensor(un("ot%d" % i), [128, D], F32))) for i in range(2)])
            junk = Buf(ph.enter_context(nc.sbuf_tensor(un("fjunk"), [128, D], BF16)))
            ss = Pool([Buf(ph.enter_context(nc.sbuf_tensor(un("ss%d" % i), [128, 2], F32))) for i in range(2)])
            rs = Pool([Buf(ph.enter_context(nc.sbuf_tensor(un("rs%d" % i), [128, 1], F32))) for i in range(2)])
            for ti in range(16):
                pts = []
                s_ = ss.get()
                for half in range(2):
                    pt = pp.get()
                    for j in range(4):
                        kc = half * 4 + j
                        k.op("pe", lambda: T.transpose(out=pt[:, j * 128:(j + 1) * 128],
                                                       in_=xT[:, kc, ti * 128:(ti + 1) * 128], identity=ident[:]),
                             [xTb[ti // 4], ident], [pt], inc=(j == 3))
                    k.op("act", lambda: A.activation(out=junk[:, half * 512:(half + 1) * 512], in_=pt[:, :],
                                                     func=AF.Square, accum_out=s_[:, half:half + 1]),
                         [pt], [junk, s_])
                    pts.append(pt)
                r_ = rs.get()
                k.op("dve", lambda: V.tensor_tensor(out=r_[:], in0=s_[:, 0:1], in1=s_[:, 1:2], op=ALU.add),
                     [s_], [r_])
                k.op("act", lambda: A.activation(out=r_[:], in_=r_[:], func=AF.Sqrt, bias=eps_t[:], scale=1.0 / D),
                     [r_, eps_t], [r_])
                k.op("dve", lambda: V.reciprocal(out=r_[:], in_=r_[:]), [r_], [r_])
                o = ot.get()
                for half in range(2):
                    k.op("dve", lambda: V.scalar_tensor_tensor(
                        out=o[:, half * 512:(half + 1) * 512], in0=pts[half][:, :], scalar=r_[:, 0:1],
                        in1=normf_b[:, half * 512:(half + 1) * 512], op0=ALU.mult, op1=ALU.mult),
                        [pts[half], r_, normf_b], [o])
                k.dma("act", out_d[b, ti * 128:(ti + 1) * 128, :], o[:], reads=[o])
            k.barrier()

    thrall = sb("thrall", [128, 1])
    k.op("dve", lambda: V.memset(thrall[:], -1e29), [], [thrall])

    theta = sb("theta", [128, L * 8])
    rdec = sb("rdec", [128, L * 8])
    coefr = sb("coefr", [128, L * 8])
    coefi = sb("coefi", [128, L * 8])
    with ExitStack() as ph:
        def sbp(name, shape, dt=F32):
            return Buf(ph.enter_context(nc.sbuf_tensor(un(name), list(shape), dt)), name)
        sc = sbp("ssc", [128, L * 8, 3])
        k.dma("sp", sc[:], ssmsc_d.rearrange("p l k c -> p (l k) c"), writes=[sc])
        ar, ai, ldt = sc[:, :, 0], sc[:, :, 1], sc[:, :, 2]
        tt = [sbp("sst%d" % i, [128, L * 8]) for i in range(10)]
        dt_, dtar, t1, kk, thr_, sn, ab, cs, abr, abi = tt
        k.op("act", lambda: A.activation(out=dt_[:], in_=ldt, func=AF.Exp), [sc], [dt_])
        k.op("dve", lambda: V.tensor_tensor(out=dtar[:], in0=dt_[:], in1=ar, op=ALU.mult), [dt_, sc], [dtar])
        k.op("dve", lambda: V.tensor_tensor(out=theta[:], in0=dt_[:], in1=ai, op=ALU.mult), [dt_, sc], [theta])
        k.op("act", lambda: A.activation(out=rdec[:], in_=dtar[:], func=AF.Exp), [dtar], [rdec])
        k.op("dve", lambda: V.tensor_scalar(out=t1[:], in0=theta[:], scalar1=1.0 / TWO_PI, scalar2=MAGIC,
                                            op0=ALU.mult, op1=ALU.add), [theta], [t1])
        k.op("dve", lambda: V.tensor_scalar(out=kk[:], in0=t1[:], scalar1=MAGIC, scalar2=-TWO_PI,
                                            op0=ALU.subtract, op1=ALU.mult), [t1], [kk])
        k.op("dve", lambda: V.tensor_tensor(out=thr_[:], in0=theta[:], in1=kk[:], op=ALU.add), [theta, kk], [thr_])
        k.op("dve", lambda: V.tensor_scalar(out=thr_[:], in0=thr_[:], scalar1=-3.141592, scalar2=3.141592,
                                            op0=ALU.max, op1=ALU.min), [thr_], [thr_])
        k.op("act", lambda: A.activation(out=sn[:], in_=thr_[:], func=AF.Sin), [thr_], [sn])
        k.op("dve", lambda: V.tensor_scalar(out=ab[:], in0=thr_[:], scalar1=TWO_PI / 4, scalar2=-TWO_PI,
                                            op0=ALU.is_gt, op1=ALU.mult), [thr_], [ab])
        k.op("dve", lambda: V.scalar_tensor_tensor(out=ab[:], in0=thr_[:], scalar=TWO_PI / 4, in1=ab[:],
                                                   op0=ALU.add, op1=ALU.add), [thr_, ab], [ab])
        k.op("dve", lambda: V.tensor_scalar(out=ab[:], in0=ab[:], scalar1=-3.141592, scalar2=3.141592,
                                            op0=ALU.max, op1=ALU.min), [ab], [ab])
        k.op("act", lambda: A.activation(out=cs[:], in_=ab[:], func=AF.Sin), [ab], [cs])
        k.op("dve", lambda: V.tensor_tensor(out=abr[:], in0=rdec[:], in1=cs[:], op=ALU.mult), [rdec, cs], [abr])
        k.op("dve", lambda: V.tensor_tensor(out=abi[:], in0=rdec[:], in1=sn[:], op=ALU.mult), [rdec, sn], [abi])
        u1, u2, den, nr = dt_, dtar, t1, kk
        k.op("dve", lambda: V.tensor_scalar(out=nr[:], in0=abr[:], scalar1=-1.0, scalar2=None, op0=ALU.add),
             [abr], [nr])
        k.op("dve", lambda: V.tensor_tensor(out=u1[:], in0=ar, in1=ar, op=ALU.mult), [sc], [u1])
        k.op("dve", lambda: V.tensor_tensor(out=u2[:], in0=ai, in1=ai, op=ALU.mult), [sc], [u2])
        k.op("dve", lambda: V.tensor_tensor(out=den[:], in0=u1[:], in1=u2[:], op=ALU.add), [u1, u2], [den])
        k.op("dve", lambda: V.reciprocal(out=den[:], in_=den[:]), [den], [den])
        k.op("dve", lambda: V.tensor_tensor(out=u1[:], in0=nr[:], in1=ar, op=ALU.mult), [nr, sc], [u1])
        k.op("dve", lambda: V.tensor_tensor(out=u2[:], in0=abi[:], in1=ai, op=ALU.mult), [abi, sc], [u2])
        k.op("dve", lambda: V.tensor_tensor(out=u1[:], in0=u1[:], in1=u2[:], op=ALU.add), [u1, u2], [u1])
        k.op("dve", lambda: V.tensor_tensor(out=coefr[:], in0=u1[:], in1=den[:], op=ALU.mult), [u1, den], [coefr])
        k.op("dve", lambda: V.tensor_tensor(out=u1[:], in0=abi[:], in1=ar, op=ALU.mult), [abi, sc], [u1])
        k.op("dve", lambda: V.tensor_tensor(out=u2[:], in0=nr[:], in1=ai, op=ALU.mult), [nr, sc], [u2])
        k.op("dve", lambda: V.tensor_tensor(out=u1[:], in0=u1[:], in1=u2[:], op=ALU.subtract), [u1, u2], [u1])
        k.op("dve", lambda: V.tensor_tensor(out=coefi[:], in0=u1[:], in1=den[:], op=ALU.mult), [u1, den], [coefi])
        k.barrier()
    if dbg is not None and "ssmsetup" in dbg:
        for nme, tl in (("theta", theta), ("rdec", rdec), ("coefr", coefr), ("coefi", coefi)):
            t = dbg_tensor(nme, [128, L * 8])
            k.dma("act", t, tl[:], reads=[tl])

    def wload(dst, name, l, c0, c1, kc=8, p=128, k0=0):
        src = wbf[name][l, k0:k0 + kc * p, c0:c1].rearrange("(kc p) n -> p kc n", p=p)
        k.dma("sp", dst[0:p, 0:kc, 0:c1 - c0], src, reads=[wbuf[(name, l)]], writes=[dst])

    def attn_phase(l, b, oT):
        with ExitStack() as ph:
            def sbp(name, shape, dt=F32):
                return Buf(ph.enter_context(nc.sbuf_tensor(un(name), list(shape), dt)), name)
            Lp = Pool(psb[0:4])
            Ob = [psb[4], psb[5]]
            Mp = Pool(psb[6:8])
            wQI = sbp("wQI", [128, 8, 256], BF16)
            wq = sbp("wq", [128, 8, 512], BF16)
            kaug = sbp("kaug", [128, S], BF16)
            kiT = sbp("kiT", [64, S], BF16)
            Vp = sbp("Vp", [128, 16, 65], BF16)
            wiT = sbp("wiT", [128, 16, 4])
            qps = Pool([sbp("qp%d" % i, [128, 1024], BF16) for i in range(3)])
            qis = Pool([sbp("qi%d" % i, [64, 512], BF16) for i in range(2)])
            Rts = Pool([sbp("Rt%d" % i, [128, 512]) for i in range(2)])
            pts = Pool([sbp("pt%d" % i, [128, 512], BF16) for i in range(3)])
            accs = Pool([sbp("accS%d" % i, [65, 512]) for i in range(1)])
            recs = Pool([sbp("rec%d" % i, [64, 512]) for i in range(1)])
            sm = Pool([sbp("sm%d" % i, [128, 8]) for i in range(8)])
            pw = sbp("pw", [128, 32])
            hss = Pool([sbp("hs%d" % i, [128, 64]) for i in range(2)])
            for it in range(BISECT_ITERS):
                k.op("dve", lambda: V.memset(pw[:, it:it + 1], 2.0 ** (-(it + 1))), [], [pw])
            cU = sbp("cU", [128, 128], BF16)
            cDneg = sbp("cDneg", [128, 1024], BF16)
            cIrep = sbp("cIrep", [128, 1024], BF16)
            diagm = sbp("diagm", [128, 128])
            sel65 = sbp("sel65", [65, 64])
            dtab = sbp("dtab", [128, S], BF16)
            sctab = sbp("sctab", [128, 8])
            dsh = sbp("dsh", [128, 128])
            ones8 = sbp("ones8", [128, 8])
            dm8s = Pool([sbp("dm8%d" % i, [128, 32]) for i in range(4)])
            ones32 = sbp("ones32", [128, 32])
            k.op("dve", lambda: V.memset(ones32[:], 1.0), [], [ones32])
            k.op("dve", lambda: V.memset(ones8[:], 1.0), [], [ones8])
            for dst, src in ((cU, cU_d), (cDneg, cDneg_d), (cIrep, cIrep_d), (diagm, diagm_d), (sel65, sel_d),
                             (dtab, dtab_d), (sctab, sctab_d)):
                k.dma("sp", dst[:], src, writes=[dst])
            wstg = ExitStack()
            wD = Buf(wstg.enter_context(nc.sbuf_tensor(un("wD"), [128, 8, 452], BF16)), "wD")
            wload(wD, "w_in", l, 1536, 1988)
            wload(wQI, "w_in", l, 1664, 1920)
            wload(wq, "w_in", l, 1024, 1536)
            k.dma("sp", kaug[64:78, :], kaug_d, merge=[kaug])
            k.op("dve", lambda: V.memset(Vp[:, :, 64:65], 1.0), [], [Vp])
            for tb in range(4):
                cols = slice(tb * 512, (tb + 1) * 512)
                for (c0, dst) in ((0, kaug), (384, kiT)):
                    pt = Mp.get()
                    for kc in range(8):
                        k.op("pe", lambda: T.matmul(pt[0:64, :], lhsT=wD[:, kc, c0:c0 + 64], rhs=hT[:, kc, cols],
                                                    start=(kc == 0), stop=(kc == 7)), [wD, HB(cols)], [pt], inc=(kc == 7))
                    k.op("act", lambda: A.activation(out=dst[0:64, cols], in_=pt[0:64, :], func=AF.Copy),
                         [pt], [], merge=[dst])
            for ti in range(16):
                tc_ = slice(ti * 128, (ti + 1) * 128)
                pt = Mp.get()
                for kc in range(8):
                    k.op("pe", lambda: T.matmul(pt[:, 0:64], lhsT=hT[:, kc, tc_], rhs=wD[:, kc, 64:128],
                                                start=(kc == 0), stop=(kc == 7)), [wD, HB(tc_)], [pt], inc=(kc == 7))
                k.op("dve", lambda: V.tensor_copy(out=Vp[:, ti, 0:64], in_=pt[:, 0:64]), [pt, Vp], [], merge=[Vp])
                pt2 = Mp.get()
                for kc in range(8):
                    k.op("pe", lambda: T.matmul(pt2[:, 0:4], lhsT=hT[:, kc, tc_], rhs=wD[:, kc, 448:452],
                                                start=(kc == 0), stop=(kc == 7)), [wD, HB(tc_)], [pt2], inc=(kc == 7))
                k.op("act", lambda: A.activation(out=wiT[:, ti, :], in_=pt2[:, 0:4], func=AF.Copy, scale=1.0 / 16),
                     [pt2], [wiT])

            k.barrier()
            wstg.close()
            idxs = Pool([sbp("idx%d" % i, [128, S]) for i in range(3)])
            mbs = Pool([sbp("mb%d" % i, [128, S], BF16) for i in range(2)])
            junk = sbp("junk", [128, S], mybir.dt.uint8)
            rkb = sbp("rkb", [128, S], mybir.dt.float16)
            if dbg is not None and "attn" in dbg and b == 0 and l == 0:
                t = dbg_tensor("wiT", [128, 64])
                k.dma("act", t, wiT[:].rearrange("p a c -> p (a c)"), reads=[wiT])
            F16 = mybir.dt.float16

            def stage_P(i):
                tc_ = slice(i * 128, (i + 1) * 128)
                nk = 128 * (i + 1)
                qp = qps.get()
                k.dma("sp", qp[72:78, :], qaug_d[i], merge=[qp])
                for half in range(2):
                    pt = Lp.get()
                    for hh in range(4):
                        h = half * 4 + hh
                        for kc in range(8):
                            k.op("pe", lambda: T.matmul(pt[0:64, hh * 128:(hh + 1) * 128],
                                                        lhsT=wq[:, kc, h * 64:(h + 1) * 64], rhs=hT[:, kc, tc_],
                                                        start=(kc == 0), stop=(kc == 7)),
                                 [wq, HB(tc_)], [pt], inc=(hh == 3 and kc == 7))
                    k.op("act", lambda: A.activation(out=qp[0:64, half * 512:(half + 1) * 512], in_=pt[0:64, :],
                                                     func=AF.Copy, scale=0.125), [pt], [], merge=[qp])
                qi = qis.get()
                pt = Lp.get()
                for hh in range(4):
                    for kc in range(8):
                        k.op("pe", lambda: T.matmul(pt[0:64, hh * 128:(hh + 1) * 128],
                                                    lhsT=wQI[:, kc, hh * 64:(hh + 1) * 64],
                                                    rhs=hT[:, kc, tc_], start=(kc == 0), stop=(kc == 7)),
                             [wQI, HB(tc_)], [pt], inc=(hh == 3 and kc == 7))
                k.op("act", lambda: A.activation(out=qi[:], in_=pt[0:64, :], func=AF.Copy), [pt], [qi])
                idx = idxs.get()
                for kb2 in range((nk + 511) // 512):
                    w_ = min(512, nk - kb2 * 512)
                    kc_ = slice(kb2 * 512, kb2 * 512 + w_)
                    for hh in range(4):
                        pt = Mp.get()
                        k.op("pe", lambda: T.matmul(pt[:, 0:w_], lhsT=qi[:, hh * 128:(hh + 1) * 128],
                                                    rhs=kiT[0:64, kc_], start=True, stop=True), [qi, kiT], [pt])
                        rt = Rts.get()
                        k.op("act", lambda: A.activation(out=rt[:, 0:w_], in_=pt[:, 0:w_], func=AF.Relu), [pt], [rt])
                        if hh == 0:
                            k.op("pool", lambda: P.tensor_scalar(out=idx[:, kc_], in0=rt[:, 0:w_],
                                                                 scalar1=wiT[:, i, 0:1], scalar2=0.0, op0=ALU.mult,
                                                                 op1=ALU.add), [rt, wiT], [idx])
                        else:
                            k.op("pool", lambda: P.tensor_scalar(out=rt[:, 0:w_], in0=rt[:, 0:w_],
                                                                 scalar1=wiT[:, i, hh:hh + 1], scalar2=0.0,
                                                                 op0=ALU.mult, op1=ALU.add), [rt, wiT], [rt])
                            k.op("pool", lambda: P.tensor_tensor(out=idx[:, kc_], in0=idx[:, kc_], in1=rt[:, 0:w_],
                                                                 op=ALU.add), [rt, idx], [idx])
                s_ = sm.get()
                return dict(i=i, nk=nk, qp=qp, idx=idx, s_=s_)

            def gen_A(c):
                i, nk, idx, s_ = c["i"], c["nk"], c["idx"], c["s_"]
                hi0, thr, cnt, e_ = s_[:, 0:1], s_[:, 1:2], s_[:, 2:3], s_[:, 3:4]
                if i >= 2:
                    k.op("dve", lambda: V.tensor_reduce(out=hi0, in_=idx[:, 0:nk], axis=AX.X, op=ALU.max,
                                                        apply_absolute_value=True), [idx], [s_])
                    yield
                    k.op("dve", lambda: V.tensor_scalar(out=hi0, in0=hi0, scalar1=1.01, scalar2=1e-6, op0=ALU.mult,
                                                        op1=ALU.add), [s_], [s_])
                    yield
                    k.op("dve", lambda: V.memset(thr, 0.0), [s_], [s_])
                    yield
                k.op("dve", lambda: V.tensor_tensor(out=idx[:, nk - 128:nk], in0=idx[:, nk - 128:nk], in1=diagm[:],
                                                    op=ALU.add), [idx, diagm], [idx])
                yield
                if i < 2:
                    return
                hs = hss.get()
                k.op("dve", lambda: V.tensor_scalar(out=hs[:, 0:BISECT_ITERS], in0=pw[:, 0:BISECT_ITERS],
                                                    scalar1=hi0, scalar2=None, op0=ALU.mult), [pw, s_], [hs])
                yield
                k.op("dve", lambda: V.tensor_scalar(out=hs[:, 32:32 + BISECT_ITERS], in0=pw[:, 0:BISECT_ITERS],
                                                    scalar1=hi0, scalar2=2.0, op0=ALU.mult, op1=ALU.mult),
                     [pw, s_], [hs])
                yield
                for it in range(BISECT_ITERS):
                    k.op("dve", lambda: V.tensor_scalar(out=junk[:, 0:nk], in0=idx[:, 0:nk], scalar1=thr,
                                                        scalar2=0.0, op0=ALU.is_ge, op1=ALU.add, accum_out=cnt),
                         [idx, s_], [junk, s_])
                    yield
                    k.op("dve", lambda: V.tensor_scalar(out=e_, in0=cnt, scalar1=255.5,
                                                        scalar2=hs[:, 32 + it:33 + it],
                                                        op0=ALU.is_ge, op1=ALU.mult), [s_, hs], [s_])
                    yield
                    k.op("dve", lambda: V.scalar_tensor_tensor(out=thr, in0=e_, scalar=hs[:, it:it + 1], in1=thr,
                                                               op0=ALU.subtract, op1=ALU.add), [s_, hs], [s_])
                    yield

            def gen_B(c):
                i, nk, idx, s_, qp = c["i"], c["nk"], c["idx"], c["s_"], c["qp"]
                mb = mbs.get()
                c["mb"] = mb
                if i >= 2:
                    hi0, thr = s_[:, 0:1], s_[:, 1:2]
                    lob, hib, chi, need = s_[:, 4:5], s_[:, 5:6], s_[:, 6:7], s_[:, 7:8]
                    cl = 2.0 ** (-BISECT_ITERS)
                    k.op("dve", lambda: V.scalar_tensor_tensor(out=lob, in0=hi0, scalar=-cl, in1=thr,
                                                               op0=ALU.mult, op1=ALU.add), [s_], [s_])
                    yield
                    k.op("dve", lambda: V.scalar_tensor_tensor(out=hib, in0=hi0, scalar=cl * 1.001, in1=thr,
                                                               op0=ALU.mult, op1=ALU.add), [s_], [s_])
                    yield
                    k.op("dve", lambda: V.tensor_scalar(out=hib, in0=hib, scalar1=1e-30, scalar2=None,
                                                        op0=ALU.add), [s_], [s_])
                    yield
                    k.op("dve", lambda: V.tensor_scalar(out=junk[:, 0:nk], in0=idx[:, 0:nk], scalar1=hib,
                                                        scalar2=0.0, op0=ALU.is_ge, op1=ALU.add, accum_out=chi),
                         [idx, s_], [junk, s_])
                    yield
                    k.op("dve", lambda: V.tensor_scalar(out=need, in0=chi, scalar1=-1.0, scalar2=256.0,
                                                        op0=ALU.mult, op1=ALU.add), [s_], [s_])
                    yield
                    k.op("dve", lambda: V.tensor_scalar(out=mb[:, 0:nk], in0=idx[:, 0:nk], scalar1=lob,
                                                        scalar2=None, op0=ALU.is_ge), [idx, s_], [mb])
                    yield
                    k.op("dve", lambda: V.scalar_tensor_tensor(out=mb[:, 0:nk], in0=idx[:, 0:nk], scalar=hib,
                                                               in1=mb[:, 0:nk], op0=ALU.is_lt, op1=ALU.mult),
                         [idx, s_, mb], [mb])
                    yield
                    k.op("dve", lambda: V.tensor_tensor_scan(out=rkb[:, 0:nk],
                                                             data0=ones8[:, 0:1].to_broadcast([128, nk]),
                                                             data1=mb[:, 0:nk], initial=0.0, op0=ALU.mult,
                                                             op1=ALU.add), [mb, ones8], [rkb])
                    yield
                    k.op("dve", lambda: V.scalar_tensor_tensor(out=rkb[:, 0:nk], in0=rkb[:, 0:nk], scalar=need,
                                                               in1=mb[:, 0:nk], op0=ALU.is_le, op1=ALU.mult),
                         [rkb, mb, s_], [rkb])
                    yield
                    k.op("dve", lambda: V.scalar_tensor_tensor(out=rkb[:, 0:nk], in0=idx[:, 0:nk], scalar=hib,
                                                               in1=rkb[:, 0:nk], op0=ALU.is_ge, op1=ALU.add),
                         [idx, s_, rkb], [rkb])
                    yield
                    k.op("dve", lambda: V.tensor_scalar(out=mb[:, 0:nk], in0=rkb[:, 0:nk], scalar1=-1.0,
                                                        scalar2=32768.0, op0=ALU.add, op1=ALU.mult), [rkb], [mb])
                    yield
                else:
                    k.op("dve", lambda: V.tensor_scalar(out=mb[:, 0:nk], in0=idx[:, 0:nk], scalar1=thrall[:, 0:1],
                                                        scalar2=-32768.0, op0=ALU.is_lt, op1=ALU.mult),
                         [idx, thrall], [mb])
                    yield
                s2 = sm.get()
                k.op("dve", lambda: V.scalar_tensor_tensor(out=rkb[:, 0:nk], in0=mb[:, 0:nk], scalar=-1.0,
                                                           in1=dtab[:, 1920 - 128 * i:2048], op0=ALU.mult,
                                                           op1=ALU.add), [mb, dtab], [rkb])
                yield
                k.op("dve", lambda: V.tensor_reduce(out=s2[:, 0:1], in_=rkb[:, 0:nk], axis=AX.X, op=ALU.min),
                     [rkb], [s2])
                yield
                dm32, tr32 = dm8s.get(), dm8s.get()
                k.op("dve", lambda: V.tensor_scalar(out=dm32[:], in0=ones32[:], scalar1=s2[:, 0:1], scalar2=None,
                                                    op0=ALU.mult), [ones32, s2], [dm32])
                yield
                k.op("dve", lambda: V.transpose(out=tr32[:], in_=dm32[:]), [dm32], [tr32])
                yield
                for bq in range(4):
                    k.op("dve", lambda: V.tensor_copy(out=dsh[64:72, bq * 32:(bq + 1) * 32],
                                                      in_=tr32[bq * 32:bq * 32 + 8, 0:32]), [tr32], [dsh])
                    yield
                for h in range(8):
                    k.op("dve", lambda: V.tensor_scalar(out=qp[64:72, h * 128:(h + 1) * 128], in0=dsh[64:72, :],
                                                        scalar1=sctab[64:72, h:h + 1], scalar2=None, op0=ALU.mult),
                         [dsh, sctab], [], merge=[qp])
                    yield
                if dbg is not None and "attn" in dbg and b == 0 and l == 0 and i in DBG_TILES:
                    t = dbg_tensor("idx%d" % i, [128, S])
                    k.dma("act", t, idx[:], reads=[idx])
                    t = dbg_tensor("mb%d" % i, [128, S], BF16)
                    k.dma("act", t, mb[:], reads=[mb])

            def stage_C(c):
                i, nk, qp, mb = c["i"], c["nk"], c["qp"], c["mb"]
                tc_ = slice(i * 128, (i + 1) * 128)
                pairs = [(j, half) for j in range(i + 1) for half in range(2)]
                Lts = {}

                def emit_L(n):
                    j, half = pairs[n]
                    kj = slice(j * 128, (j + 1) * 128)
                    hc = slice(half * 512, (half + 1) * 512)
                    Lt = Lp.get()
                    Lts[n] = Lt
                    k.op("pe", lambda: T.matmul(Lt[:, :], lhsT=kaug[0:78, kj], rhs=qp[0:78, hc],
                                                start=True, stop=False), [kaug, qp], [Lt], inc=False)
                    k.op("pe", lambda: T.matmul(Lt[:, :], lhsT=mb[:, kj], rhs=cIrep[:, hc],
                                                start=False, stop=(j != i)), [mb, cIrep], [Lt], inc=(j != i))
                    if j == i:
                        k.op("pe", lambda: T.matmul(Lt[:, :], lhsT=cU[:], rhs=cDneg[:, hc],
                                                    start=False, stop=True), [cU, cDneg], [Lt])

                def emit_exp_pv(n):
                    j, half = pairs[n]
                    Lt = Lts.pop(n)
                    p_ = pts.get()
                    k.op("act", lambda: A.activation(out=p_[:], in_=Lt[:, :], func=AF.Exp), [Lt], [p_])
                    k.op("pe", lambda: T.matmul(Ob[half][0:65, :], lhsT=Vp[:, j, 0:65], rhs=p_[:],
                                                start=(j == 0), stop=(j == i)), [Vp, p_], [Ob[half]],
                         inc=(j == i))

                for n in range(min(2, len(pairs))):
                    emit_L(n)
                for n in range(len(pairs)):
                    if n + 2 < len(pairs):
                        emit_L(n + 2)
                    emit_exp_pv(n)
                for half in range(2):
                    acc = accs.get()
                    k.op("act", lambda: A.activation(out=acc[0:65, :], in_=Ob[half][0:65, :], func=AF.Copy),
                         [Ob[half]], [acc])
                    pt = Mp.get()
                    k.op("pe", lambda: T.matmul(pt[0:64, :], lhsT=sel65[0:65, :], rhs=acc[0:65, :],
                                                start=True, stop=True), [sel65, acc], [pt])
                    rec = recs.get()
                    k.op("act", lambda: A.activation(out=rec[:], in_=pt[0:64, :], func=AF.Ln), [pt], [rec])
                    k.op("act", lambda: A.activation(out=rec[:], in_=rec[:], func=AF.Exp, scale=-1.0), [rec], [rec])
                    for par in range(2):
                        av = acc[0:64, :].rearrange("p (a q t) -> p a q t", a=2, q=2)[:, :, par, :]
                        rv = rec[:, :].rearrange("p (a q t) -> p a q t", a=2, q=2)[:, :, par, :]
                        k.op("pool", lambda: P.tensor_tensor(
                            out=oT[par * 64:(par + 1) * 64, half * 2:half * 2 + 2, tc_], in0=av, in1=rv,
                            op=ALU.mult), [acc, rec], [oT])

            def run(g):
                for _ in g:
                    pass

            def interleave(g1, g2):
                live = [g1, g2]
                while live:
                    for g in list(live):
                        try:
                            next(g)
                        except StopIteration:
                            live.remove(g)

            ctxs = {0: stage_P(0), 1: stage_P(1)}
            run(gen_A(ctxs[0]))
            for s_i in range(16):
                if s_i + 2 < 16:
                    ctxs[s_i + 2] = stage_P(s_i + 2)
                if s_i + 1 < 16:
                    interleave(gen_A(ctxs[s_i + 1]), gen_B(ctxs[s_i]))
                else:
                    run(gen_B(ctxs[s_i]))
                stage_C(ctxs[s_i])
            k.barrier()

    def conv_phase(l, b, uA):
        with ExitStack() as ph:
            def sbp(name, shape, dt=F32):
                return Buf(ph.enter_context(nc.sbuf_tensor(un(name), list(shape), dt)), name)
            wA = sbp("wA", [128, 8, 768], BF16)
            zt = sbp("zt", [128, S + 2])
            cgt = Pool([sbp("cgt%d" % i, [128, 512]) for i in range(2)])
            bgt = sbp("bgt", [128, S])
            yt = sbp("yt", [128, S])
            wload(wA, "w_in", l, 0, 768)
            k.op("dve", lambda: V.memset(zt[:, 0:2], 0.0), [], [zt])
            for j in range(2):
                for tb in range(4):
                    cols = slice(tb * 512, (tb + 1) * 512)
                    pc, pv, pb = pp.get(), pp.get(), pp.get()
                    for (pt_, c0) in ((pc, 256 + 128 * j), (pv, 512 + 128 * j), (pb, 128 * j)):
                        for kc in range(8):
                            k.op("pe", lambda: T.matmul(pt_[:, :], lhsT=wA[:, kc, c0:c0 + 128], rhs=hT[:, kc, cols],
                                                        start=(kc == 0), stop=(kc == 7)), [wA, HB(cols)], [pt_],
                                 inc=(kc == 7))
                    cg = cgt.get()
                    k.op("act", lambda: A.activation(out=cg[:], in_=pc[:, :], func=AF.Copy), [pc], [cg])
                    k.op("dve", lambda: V.tensor_tensor(out=zt[:, 2 + tb * 512:2 + (tb + 1) * 512], in0=pv[:, :],
                                                        in1=cg[:], op=ALU.mult), [pv, cg], [zt])
                    k.op("act", lambda: A.activation(out=bgt[:, cols], in_=pb[:, :], func=AF.Copy), [pb], [bgt])
                k.op("dve", lambda: V.tensor_scalar(out=yt[:], in0=zt[:, 2:S + 2], scalar1=convw[:, l, 2, j:j + 1],
                                                    scalar2=None, op0=ALU.mult), [zt, convw], [yt])
                k.op("dve", lambda: V.scalar_tensor_tensor(out=yt[:], in0=zt[:, 1:S + 1],
                                                           scalar=convw[:, l, 1, j:j + 1], in1=yt[:],
                                                           op0=ALU.mult, op1=ALU.add), [zt, convw, yt], [yt])
                k.op("dve", lambda: V.scalar_tensor_tensor(out=yt[:], in0=zt[:, 0:S],
                                                           scalar=convw[:, l, 0, j:j + 1], in1=yt[:],
                                                           op0=ALU.mult, op1=ALU.add), [zt, convw, yt], [yt])
                k.op("dve", lambda: V.tensor_tensor(out=uA[:, j, :], in0=bgt[:], in1=yt[:], op=ALU.mult),
                     [bgt, yt], [uA])
            k.barrier()

    def ssm_phase(l, b, zT):
        with ExitStack() as ph:
            def sbp(name, shape, dt=F32):
                return Buf(ph.enter_context(nc.sbuf_tensor(un(name), list(shape), dt)), name)
            vP = Pool(psb[0:4])
            yP = Pool(psb[4:6])
            mP = Pool(psb[6:8])
            wB = sbp("wB", [128, 8, 256], BF16)
            ubf = sbp("ubf", [128, 2, S], BF16)
            BbT = sbp("BbT", [128, 8, 2, 128], BF16)
            k.op("dve", lambda: V.memset(BbT[:], 0.0), [], [BbT])
            Cre = sbp("Cre", [128, 8, 128], BF16)
            nCre = sbp("nCre", [128, 8, 128], BF16)
            nCim = sbp("nCim", [128, 8, 128], BF16)
            gst = sbp("gst", [128, 8, 2])
            iota = sbp("iota", [128, 512])
            k.dma("sp", iota[:], iota_d, writes=[iota])
            stg = ExitStack()
            bp = Buf(stg.enter_context(nc.sbuf_tensor(un("bp"), [128, 8, 2, 32], F32)), "bp")
            cp = Buf(stg.enter_context(nc.sbuf_tensor(un("cp"), [128, 8, 2, 128], F32)), "cp")
            bbt = Pool([Buf(stg.enter_context(nc.sbuf_tensor(un("bbt"), [128, 32], F32)), "bbt") for i in range(3)])
            wload(wB, "w_in", l, 768, 1024)
            k.dma("sp", bp[:], bpad_d[:, l], writes=[bp])
            k.dma("sp", cp[:], cpad_d[:, l], writes=[cp])
            k.op("act", lambda: A.activation(out=Cre[:], in_=cp[:, :, 0, :], func=AF.Copy), [cp], [Cre])
            k.op("act", lambda: A.activation(out=nCre[:], in_=cp[:, :, 0, :], func=AF.Copy, scale=-1.0), [cp], [nCre])
            k.op("act", lambda: A.activation(out=nCim[:], in_=cp[:, :, 1, :], func=AF.Copy, scale=-1.0), [cp], [nCim])
            for kk_ in range(8):
                ci = l * 8 + kk_
                for ri in range(2):
                    t0, t1_ = bbt.get(), bbt.get()
                    if ri == 0:
                        k.op("dve", lambda: V.tensor_scalar(out=t0[:], in0=bp[:, kk_, 1, :], scalar1=coefi[:, ci:ci + 1],
                                                            scalar2=None, op0=ALU.mult), [bp, coefi], [t0])
                        k.op("dve", lambda: V.scalar_tensor_tensor(out=t1_[:], in0=bp[:, kk_, 0, :],
                                                                   scalar=coefr[:, ci:ci + 1], in1=t0[:],
                                                                   op0=ALU.mult, op1=ALU.subtract),
                             [bp, coefr, t0], [t1_])
                    else:
                        k.op("dve", lambda: V.tensor_scalar(out=t0[:], in0=bp[:, kk_, 0, :], scalar1=coefi[:, ci:ci + 1],
                                                            scalar2=None, op0=ALU.mult), [bp, coefi], [t0])
                        k.op("dve", lambda: V.scalar_tensor_tensor(out=t1_[:], in0=bp[:, kk_, 1, :],
                                                                   scalar=coefr[:, ci:ci + 1], in1=t0[:],
                                                                   op0=ALU.mult, op1=ALU.add),
                             [bp, coefr, t0], [t1_])
                    pt = mP.get()
                    k.op("pe", lambda: T.transpose(out=pt[0:32, 0:128], in_=t1_[:, :], identity=ident[:]),
                         [t1_, ident], [pt])
                    r0 = 32 * (kk_ % 4)
                    k.op("act", lambda: A.activation(out=BbT[r0:r0 + 32, kk_, ri, :], in_=pt[0:32, 0:128],
                                                     func=AF.Copy), [pt, BbT], [], merge=[BbT])
            k.barrier()
            stg.close()
            phs = Pool([sbp("phs%d" % i, [128, 512]) for i in range(2)])
            Ct = Pool([sbp("Ct%d" % i, [128, 512]) for i in range(2)])
            St = Pool([sbp("St%d" % i, [128, 512]) for i in range(2)])
            ms = Pool([sbp("ms%d" % i, [128, 512]) for i in range(8)])
            ws = Pool([sbp("ws%d" % i, [128, 512]) for i in range(4)])
            gs = Pool([sbp("gs%d" % i, [128, 512]) for i in range(2)])
            prs = Pool([sbp("pr%d" % i, [128, 512], BF16) for i in range(4)])
            yfs = Pool([sbp("yf%d" % i, [128, 512]) for i in range(1)])
            gt = Pool([sbp("gt%d" % i, [128, 512]) for i in range(2)])
            for kb in range(2):
                for tb in range(4):
                    cols = slice(tb * 512, (tb + 1) * 512)
                    pt = mP.get()
                    for kc in range(8):
                        k.op("pe", lambda: T.matmul(pt[:, :], lhsT=wB[:, kc, kb * 128:(kb + 1) * 128],
                                                    rhs=hT[:, kc, cols], start=(kc == 0), stop=(kc == 7)),
                             [wB, HB(cols)], [pt], inc=(kc == 7))
                    k.op("act", lambda: A.activation(out=ubf[:, kb, cols], in_=pt[:, :], func=AF.Copy), [pt], [ubf])
            ystate = {}

            def ssm_S1(tb, kk_):
                cols = slice(tb * 512, (tb + 1) * 512)
                ci = l * 8 + kk_
                kb = kk_ // 4
                p0, p1 = phs.get(), phs.get()
                k.op("dve", lambda: V.tensor_scalar(out=p0[:], in0=iota[:], scalar1=float(tb * 512),
                                                    scalar2=theta[:, ci:ci + 1], op0=ALU.add, op1=ALU.mult),
                     [iota, theta], [p0])
                k.op("dve", lambda: V.tensor_scalar(out=p1[:], in0=p0[:], scalar1=1.0 / TWO_PI, scalar2=MAGIC,
                                                    op0=ALU.mult, op1=ALU.add), [p0], [p1])
                k.op("dve", lambda: V.tensor_scalar(out=p1[:], in0=p1[:], scalar1=MAGIC, scalar2=-TWO_PI,
                                                    op0=ALU.subtract, op1=ALU.mult), [p1], [p1])
                k.op("dve", lambda: V.tensor_tensor(out=p0[:], in0=p0[:], in1=p1[:], op=ALU.add), [p0, p1], [p0])
                k.op("dve", lambda: V.tensor_scalar(out=p0[:], in0=p0[:], scalar1=-3.141592, scalar2=3.141592,
                                                    op0=ALU.max, op1=ALU.min), [p0], [p0])
                Sn, Cs = St.get(), Ct.get()
                k.op("act", lambda: A.activation(out=Sn[:], in_=p0[:], func=AF.Sin), [p0], [Sn])
                k.op("dve", lambda: V.tensor_scalar(out=p1[:], in0=p0[:], scalar1=TWO_PI / 4, scalar2=-TWO_PI,
                                                    op0=ALU.is_gt, op1=ALU.mult), [p0], [p1])
                k.op("dve", lambda: V.scalar_tensor_tensor(out=p1[:], in0=p0[:], scalar=TWO_PI / 4, in1=p1[:],
                                                           op0=ALU.add, op1=ALU.add), [p0, p1], [p1])
                k.op("dve", lambda: V.tensor_scalar(out=p1[:], in0=p1[:], scalar1=-3.141592, scalar2=3.141592,
                                                    op0=ALU.max, op1=ALU.min), [p1], [p1])
                k.op("act", lambda: A.activation(out=Cs[:], in_=p1[:], func=AF.Sin), [p1], [Cs])
                vr, vi = vP.get(), vP.get()
                for (vt, ri) in ((vr, 0), (vi, 1)):
                    k.op("pe", lambda: T.matmul(vt[:, :], lhsT=BbT[:, kk_, ri, :],
                                                rhs=ubf[:, kb, cols], start=True, stop=True),
                         [BbT, ubf], [vt])
                m1, m2, m3, m4 = ms.get(), ms.get(), ms.get(), ms.get()
                k.op("dve", lambda: V.tensor_tensor(out=m1[:], in0=vr[:, :], in1=Cs[:], op=ALU.mult), [vr, Cs], [m1])
                k.op("dve", lambda: V.tensor_tensor(out=m2[:], in0=vi[:, :], in1=Sn[:], op=ALU.mult), [vi, Sn], [m2])
                k.op("dve", lambda: V.tensor_tensor(out=m3[:], in0=vi[:, :], in1=Cs[:], op=ALU.mult), [vi, Cs], [m3])
                k.op("dve", lambda: V.tensor_tensor(out=m4[:], in0=vr[:, :], in1=Sn[:], op=ALU.mult), [vr, Sn], [m4])
                wr, wi_ = ws.get(), ws.get()
                k.op("pool", lambda: P.tensor_tensor(out=wr[:], in0=m1[:], in1=m2[:], op=ALU.add), [m1, m2], [wr])
                k.op("pool", lambda: P.tensor_tensor(out=wi_[:], in0=m3[:], in1=m4[:], op=ALU.subtract),
                     [m3, m4], [wi_])
                return dict(tb=tb, kk_=kk_, Sn=Sn, Cs=Cs, wr=wr, wi_=wi_)

            def ssm_S2(c):
                tb, kk_, Sn, Cs, wr, wi_ = c["tb"], c["kk_"], c["Sn"], c["Cs"], c["wr"], c["wi_"]
                cols = slice(tb * 512, (tb + 1) * 512)
                ci = l * 8 + kk_
                kb = kk_ // 4
                gr, gi = gs.get(), gs.get()
                dec = rdec[:, ci:ci + 1].to_broadcast([128, 512])
                for (g_, w__, ri) in ((gr, wr, 0), (gi, wi_, 1)):
                    init = 0.0 if tb == 0 else gst[:, kk_, ri:ri + 1]
                    k.op("dve", lambda: V.tensor_tensor_scan(out=g_[:], data0=dec, data1=w__[:], initial=init,
                                                             op0=ALU.mult, op1=ALU.add),
                         [rdec, w__, gst], [g_])
                    if tb < 3:
                        k.op("dve", lambda: V.tensor_copy(out=gst[:, kk_, ri:ri + 1], in_=g_[:, 511:512]),
                             [g_], [gst])
                if kk_ % 4 == 0:
                    ystate["ypt"] = yP.get()
                ypt = ystate["ypt"]
                combos = ((gr, Cs, Cre), (gi, Sn, nCre), (gi, Cs, nCim), (gr, Sn, nCim))
                for n_, (g_, tb_, cm) in enumerate(combos):
                    pr = prs.get()
                    k.op("pool", lambda: P.tensor_tensor(out=pr[:], in0=g_[:], in1=tb_[:], op=ALU.mult),
                         [g_, tb_], [pr])
                    k.op("pe", lambda: T.matmul(ypt[:, :], lhsT=cm[:, kk_, :], rhs=pr[:],
                                                start=(kk_ % 4 == 0 and n_ == 0), stop=(kk_ % 4 == 3 and n_ == 3)),
                         [cm, pr], [ypt], inc=(n_ == 3))
                if kk_ % 4 == 3:
                    yf = yfs.get()
                    k.op("dve", lambda: V.scalar_tensor_tensor(out=yf[:], in0=ubf[:, kb, cols],
                                                               scalar=ssmd[:, l, kb:kb + 1], in1=ypt[:, :],
                                                               op0=ALU.mult, op1=ALU.add), [ubf, ssmd, ypt], [yf])
                    if dbg is not None and "ssm" in dbg and b == 0 and l == 0:
                        t = dbg_tensor("y_%d_%d" % (tb, kb), [128, 512])
                        k.dma("act", t, yf[:], reads=[yf])
                    g1_, g2_ = gt.get(), gt.get()
                    k.op("dve", lambda: V.tensor_tensor(out=g1_[:], in0=yf[:], in1=yf[:], op=ALU.mult), [yf], [g1_])
                    k.op("dve", lambda: V.tensor_scalar(out=g1_[:], in0=g1_[:], scalar1=0.044715, scalar2=1.0,
                                                        op0=ALU.mult, op1=ALU.add), [g1_], [g1_])
                    k.op("dve", lambda: V.tensor_tensor(out=g1_[:], in0=g1_[:], in1=yf[:], op=ALU.mult),
                         [g1_, yf], [g1_])
                    k.op("act", lambda: A.activation(out=g2_[:], in_=g1_[:], func=AF.Sigmoid,
                                                     scale=1.5957691216057308), [g1_], [g2_])
                    k.op("dve", lambda: V.tensor_tensor(out=zT[:, kb, cols], in0=yf[:], in1=g2_[:], op=ALU.mult),
                         [yf, g2_], [zT])

            its = [(tb, kk_) for tb in range(4) for kk_ in range(8)]
            if SSM_PIPE:
                cur = ssm_S1(*its[0])
                for n_it in range(len(its)):
                    nxt = ssm_S1(*its[n_it + 1]) if n_it + 1 < len(its) else None
                    ssm_S2(cur)
                    cur = nxt
            else:
                for it_ in its:
                    ssm_S2(ssm_S1(*it_))
            k.barrier()

    def mix_phase(l, b, uA, zT, oT):
        with ExitStack() as ph:
            def sbp(name, shape, dt=F32):
                return Buf(ph.enter_context(nc.sbuf_tensor(un(name), list(shape), dt)), name)
            gseg = Pool([sbp("gseg%d" % i, [128, 8, 256], BF16) for i in range(6)])
            wcos = Pool([sbp("wco%d" % i, [128, 2, 256], BF16) for i in range(2)])
            wgas = Pool([sbp("wga%d" % i, [128, 2, 256], BF16) for i in range(2)])
            wgbs = Pool([sbp("wgb%d" % i, [128, 2, 256], BF16) for i in range(2)])
            waos = Pool([sbp("wao%d" % i, [128, 4, 256], BF16) for i in range(2)])
            wos = Pool([sbp("wo%d" % i, [128, 8, 512], BF16) for i in range(1)])
            mixs = Pool([sbp("mix%d" % i, [128, 8, 512], BF16) for i in range(1)])
            sg = Pool([sbp("sg%d" % i, [128, 512]) for i in range(4)])
            tm = Pool([sbp("tm%d" % i, [128, 512]) for i in range(3)])
            npools = norm_pools(ph) if FUSE_NORM else None
            for tb in range(4):
                cols = slice(tb * 512, (tb + 1) * 512)
                mix = mixs.get()
                for cg in range(4):
                    if cg == 2 and tb > 0 and npools is not None:
                        norm_tb(l, 1, b, tb - 1, npools)
                    segs = []
                    for br in range(3):
                        sgm = gseg.get()
                        c0 = 2048 + br * 1024 + cg * 256
                        wload(sgm, "w_in", l, c0, c0 + 256)
                        segs.append(sgm)
                    wco, wga, wgb, wao = wcos.get(), wgas.get(), wgbs.get(), waos.get()
                    wload(wco, "w_conv_out", l, cg * 256, (cg + 1) * 256, kc=2)
                    wload(wga, "w_glu", l, cg * 256, (cg + 1) * 256, kc=2)
                    wload(wgb, "w_glu", l, D + cg * 256, D + (cg + 1) * 256, kc=2)
                    wload(wao, "w_attn_out", l, cg * 256, (cg + 1) * 256, kc=4)
                    for cc in range(2):
                        c = cg * 2 + cc
                        mc = slice(cc * 128, (cc + 1) * 128)
                        gp = []
                        for br in range(3):
                            pt = pp.get()
                            for kc in range(8):
                                k.op("pe", lambda: T.matmul(pt[:, :], lhsT=segs[br][:, kc, mc], rhs=hT[:, kc, cols],
                                                            start=(kc == 0), stop=(kc == 7)), [segs[br], HB(cols)], [pt],
                                     inc=(kc == 7))
                            gp.append(pt)
                        sgs = []
                        for br in range(3):
                            s_ = sg.get()
                            k.op("act", lambda: A.activation(out=s_[:], in_=gp[br][:, :], func=AF.Sigmoid,
                                                             bias=bgate[:, l, br * 8 + c:br * 8 + c + 1], scale=1.0),
                                 [gp[br], bgate], [s_])
                            sgs.append(s_)
                        pya, pza, pzb, pyc = pp.get(), pp.get(), pp.get(), pp.get()
                        for kc in range(2):
                            k.op("pe", lambda: T.matmul(pya[:, :], lhsT=wco[:, kc, mc], rhs=uA[:, kc, cols],
                                                        start=(kc == 0), stop=(kc == 1)), [wco, uA], [pya], inc=(kc == 1))
                        for kc in range(2):
                            k.op("pe", lambda: T.matmul(pza[:, :], lhsT=wga[:, kc, mc], rhs=zT[:, kc, cols],
                                                        start=(kc == 0), stop=(kc == 1)), [wga, zT], [pza], inc=(kc == 1))
                        for kc in range(2):
                            k.op("pe", lambda: T.matmul(pzb[:, :], lhsT=wgb[:, kc, mc], rhs=zT[:, kc, cols],
                                                        start=(kc == 0), stop=(kc == 1)), [wgb, zT], [pzb], inc=(kc == 1))
                        for kc in range(4):
                            k.op("pe", lambda: T.matmul(pyc[:, :], lhsT=wao[:, kc, mc], rhs=oT[:, kc, cols],
                                                        start=(kc == 0), stop=(kc == 3)), [wao, oT], [pyc], inc=(kc == 3))
                        sz = sg.get()
                        k.op("act", lambda: A.activation(out=sz[:], in_=pzb[:, :], func=AF.Sigmoid), [pzb], [sz])
                        t1_, t2_, t3_ = tm.get(), tm.get(), tm.get()
                        k.op("dve", lambda: V.tensor_tensor(out=t1_[:], in0=pya[:, :], in1=sgs[0][:], op=ALU.mult),
                             [pya, sgs[0]], [t1_])
                        k.op("dve", lambda: V.tensor_tensor(out=t2_[:], in0=pza[:, :], in1=sz[:], op=ALU.mult),
                             [pza, sz], [t2_])
                        k.op("pool", lambda: P.tensor_tensor(out=t2_[:], in0=t2_[:], in1=sgs[1][:], op=ALU.mult),
                             [t2_, sgs[1]], [t2_])
                        k.op("dve", lambda: V.tensor_tensor(out=t3_[:], in0=pyc[:, :], in1=sgs[2][:], op=ALU.mult),
                             [pyc, sgs[2]], [t3_])
                        k.op("pool", lambda: P.tensor_tensor(out=t1_[:], in0=t1_[:], in1=t2_[:], op=ALU.add),
                             [t1_, t2_], [t1_])
                        k.op("pool", lambda: P.tensor_tensor(out=mix[:, c, :], in0=t1_[:], in1=t3_[:], op=ALU.add),
                             [t1_, t3_], [mix])
                for nh in range(2):
                    wo = wos.get()
                    wload(wo, "w_o", l, nh * 512, (nh + 1) * 512)
                    for nn in range(4):
                        n = nh * 4 + nn
                        pt = pp.get()
                        for c in range(8):
                            k.op("pe", lambda: T.matmul(pt[:, :], lhsT=wo[:, c, nn * 128:(nn + 1) * 128], rhs=mix[:, c, :],
                                                        start=(c == 0), stop=(c == 7)), [wo, mix], [pt], inc=(c == 7))
                        k.op("dve", lambda: V.scalar_tensor_tensor(out=xT[:, n, cols], in0=pt[:, :],
                                                                   scalar=modT[:, l, 2 * 8 + n, b:b + 1],
                                                                   in1=xT[:, n, cols],
                                                                   op0=ALU.mult, op1=ALU.add), [pt, modT, xTb[tb]], [xTb[tb]])
            if npools is not None:
                norm_tb(l, 1, b, 3, npools)
            k.barrier()

    def ffn_phase(l, b):
        with ExitStack() as ph:
            def sbp(name, shape, dt=F32):
                return Buf(ph.enter_context(nc.sbuf_tensor(un(name), list(shape), dt)), name)
            if not FUSE_NORM:
                with ExitStack() as ph2:
                    norm_mod(l, 1, b, ph2)
                    k.barrier()
            npools = norm_pools(ph) if FUSE_NORM else None
            fseg = Pool([sbp("fseg%d" % i, [128, 8, 512], BF16) for i in range(2)])
            wfo = sbp("wfo", [128, 22, D], BF16)
            acts = Pool([sbp("act%d" % i, [128, 22, 512], BF16) for i in range(1)])
            sg = Pool([sbp("fsg%d" % i, [128, 512]) for i in range(2)])
            wload(wfo, "w_ffn_out", l, 0, D, kc=22)
            for tb in range(4):
                cols = slice(tb * 512, (tb + 1) * 512)
                act = acts.get()
                for fg in range(11):
                    if fg == 5 and tb > 0 and npools is not None and l + 1 < L:
                        norm_tb(l + 1, 0, b, tb - 1, npools)
                    seg = fseg.get()
                    wload(seg, "w_ffn_in", l, fg * 512, (fg + 1) * 512)
                    for fc in range(2):
                        f = fg * 2 + fc
                        pg, pu = pp.get(), pp.get()
                        for (pt_, c0) in ((pg, fc * 128), (pu, 256 + fc * 128)):
                            for kc in range(8):
                                k.op("pe", lambda: T.matmul(pt_[:, :], lhsT=seg[:, kc, c0:c0 + 128], rhs=hT[:, kc, cols],
                                                            start=(kc == 0), stop=(kc == 7)), [seg, HB(cols)], [pt_],
                                     inc=(kc == 7))
                        s_ = sg.get()
                        k.op("act", lambda: A.activation(out=s_[:], in_=pg[:, :], func=AF.Silu), [pg], [s_])
                        k.op("dve", lambda: V.tensor_tensor(out=act[:, f, :], in0=pu[:, :], in1=s_[:], op=ALU.mult),
                             [pu, s_], [act])
                for n in range(8):
                    pt = pp.get()
                    for f in range(22):
                        k.op("pe", lambda: T.matmul(pt[:, :], lhsT=wfo[:, f, n * 128:(n + 1) * 128], rhs=act[:, f, :],
                                                    start=(f == 0), stop=(f == 21)), [wfo, act], [pt], inc=(f == 21))
                    k.op("dve", lambda: V.scalar_tensor_tensor(out=xT[:, n, cols], in0=pt[:, :],
                                                               scalar=modT[:, l, 5 * 8 + n, b:b + 1], in1=xT[:, n, cols],
                                                               op0=ALU.mult, op1=ALU.add), [pt, modT, xTb[tb]], [xTb[tb]])
            if npools is not None and l + 1 < L:
                norm_tb(l + 1, 0, b, 3, npools)
            k.barrier()

    def dump(name, buf, shape, dt, b, l):
        if dbg is not None and name in dbg and b == 0 and l == 0:
            t = dbg_tensor(name, shape, dt)
            pat = {2: "p a -> p a", 3: "p a s -> p (a s)"}[len(buf.t.shape)]
            k.dma("act", t, buf[:].rearrange(pat) if len(buf.t.shape) == 3 else buf[:], reads=[buf])

    for b in range(NB):
        load_x(b)
        if b == 0:
            adaln_and_prepass()
        for l in range(L if stage >= 2 else 0):
            if l == 0 or not FUSE_NORM or stage < 99:
                with ExitStack() as ph:
                    norm_mod(l, 0, b, ph)
                    k.barrier()
            dump("hT", hT, [128, 8 * S], BF16, b, l)
            if stage >= 3:
                with ExitStack() as lay:
                    def sbl(name, shape, dt=F32):
                        return Buf(lay.enter_context(nc.sbuf_tensor(un(name), list(shape), dt)), name)
                    oT = sbl("oT", [128, 4, S], BF16)
                    attn_phase(l, b, oT)
                    dump("oT", oT, [128, 4 * S], BF16, b, l)
                    if stage >= 4:
                        uA = sbl("uA", [128, 2, S], BF16)
                        conv_phase(l, b, uA)
                        dump("uA", uA, [128, 2 * S], BF16, b, l)
                    if stage >= 5:
                        zT = sbl("zT", [128, 2, S], BF16)
                        ssm_phase(l, b, zT)
                        dump("zT", zT, [128, 2 * S], BF16, b, l)
                    if stage >= 6:
                        mix_phase(l, b, uA, zT, oT)
                        dump("x1", xT, [128, 8 * S], F32, b, l)
                    k.barrier()
            if stage >= 7:
                ffn_phase(l, b)
                dump("x2", xT, [128, 8 * S], F32, b, l)
            if stage < 99 and l == 0:
                break
        final_store(b)
        if stage < 99:
            break

    k.barrier()
    st.close()
    return k, dbg_out


def _host_consts():
    c = {}
    s = np.arange(S)
    kaug = np.zeros((14, S), np.float32)
    kaug[0:8] = 1
    kaug[8] = 1
    kaug[9] = 1
    kaug[10] = ((s % 128) // 64) * 64
    kaug[11] = s % 64
    kaug[12] = 1
    kaug[13] = s // 128
    tq = np.arange(128)[:, None]
    uu = np.arange(S)[None, :]
    c["dtab"] = np.abs(tq - uu + 1920).astype(np.float32).astype(ml_dtypes.bfloat16)
    sct = np.zeros((128, 8), np.float32)
    sct[64:72] = np.diag(2.0 ** (-(np.arange(1, 9))))
    sct[0:8] = np.diag(2.0 ** (-(np.arange(1, 9))))
    c["sctab"] = sct
    c["kaug_c"] = kaug.astype(ml_dtypes.bfloat16)
    slopes = 2.0 ** (-(np.arange(1, 9)))
    tp = np.arange(128)
    qaug = np.zeros((16, 6, 8, 128), np.float32)
    for i in range(16):
        for h in range(8):
            qaug[i, 0, h] = -slopes[h] * ((tp // 64) * 64)
            qaug[i, 1, h] = -slopes[h] * (tp % 64)
            qaug[i, 2, h] = slopes[h]
            qaug[i, 3, h] = slopes[h]
            qaug[i, 4, h] = -slopes[h] * 128 * i
            qaug[i, 5, h] = slopes[h] * 128
    c["qaug_c"] = qaug.reshape(16, 6, 1024).astype(ml_dtypes.bfloat16)
    tt = tp[:, None]
    ss = tp[None, :]
    c["constU"] = np.maximum(ss - tt, 0).astype(np.float32).astype(ml_dtypes.bfloat16)
    dneg = np.zeros((128, 8, 128), np.float32)
    irep = np.zeros((128, 8, 128), np.float32)
    for h in range(8):
        dneg[:, h, :] = -2 * slopes[h] * np.eye(128)
        irep[:, h, :] = np.eye(128)
    c["constDneg"] = dneg.reshape(128, 1024).astype(ml_dtypes.bfloat16)
    c["constIrep"] = irep.reshape(128, 1024).astype(ml_dtypes.bfloat16)
    c["ident"] = np.eye(128, dtype=np.float32)
    dm = np.zeros((128, 128), np.float32)
    dm[:64, 64:] = NEG
    c["diagmask"] = dm
    c["iota512"] = np.broadcast_to(np.arange(512, dtype=np.float32), (128, 512)).copy()
    sel = np.zeros((65, 64), np.float32)
    sel[64, :] = 1.0
    c["sel65"] = sel
    return c


def _layout_common(inp):
    f = np.float32
    m = {}
    m["w_mod"] = np.ascontiguousarray(inp["w_mod"], f).reshape(L, 8, 128, 6 * D)
    nrm = np.stack([inp["norm1"][0], inp["norm2"][0], inp["norm1"][1], inp["norm2"][1]], 0)
    m["nrm"] = np.ascontiguousarray(nrm.reshape(2 * L, 8, 128).transpose(2, 0, 1), f)
    m["normf_b"] = np.ascontiguousarray(np.broadcast_to(inp["norm_f"][None, :], (128, D)), f)
    w_in = np.zeros((L, D, DIN_P), f)
    w_in[:, :, 0:1988] = inp["w_in"][:, :, 0:1988]
    w_in[:, :, 2048:5120] = inp["w_in"][:, :, 1988:5060]
    m["w_in"] = w_in
    m["b_gate_t"] = np.ascontiguousarray(inp["b_gate"].reshape(L, 24, 128).transpose(2, 0, 1), f)
    m["conv_w_t"] = np.ascontiguousarray(inp["conv_w"].reshape(L, 3, 2, 128).transpose(3, 0, 1, 2), f)
    for nme in ("w_conv_out", "w_glu", "w_attn_out", "w_o", "w_ffn_out"):
        m[nme] = np.ascontiguousarray(inp[nme], f)
    wf = inp["w_ffn_in"]
    wfr = np.zeros((L, D, 2 * DFF), f)
    for g in range(11):
        wfr[:, :, g * 512:g * 512 + 256] = wf[:, :, g * 256:(g + 1) * 256]
        wfr[:, :, g * 512 + 256:(g + 1) * 512] = wf[:, :, DFF + g * 256:DFF + (g + 1) * 256]
    m["w_ffn_in"] = wfr
    sc = np.zeros((128, L, 8, 3), f)
    for l in range(L):
        for g in range(16):
            kk, gl = g // 2, g % 2
            sc[gl * 64:(gl + 1) * 64, l, kk, 0] = inp["ssm_a_re"][l, g]
            sc[gl * 64:(gl + 1) * 64, l, kk, 1] = inp["ssm_a_im"][l, g]
            sc[gl * 64:(gl + 1) * 64, l, kk, 2] = inp["ssm_log_dt"][l, g]
    m["ssm_sc"] = sc
    bpad = np.zeros((128, L, 8, 2, 32), f)
    cpad = np.zeros((128, L, 8, 2, 128), f)
    for l in range(L):
        for g in range(16):
            kk, gl = g // 2, g % 2
            rows = slice(gl * 64, (gl + 1) * 64)
            bpad[rows, l, kk, 0, gl * 16:(gl + 1) * 16] = inp["ssm_b_re"][l, g]
            bpad[rows, l, kk, 1, gl * 16:(gl + 1) * 16] = inp["ssm_b_im"][l, g]
            c0 = 32 * (kk % 4) + gl * 16
            cpad[rows, l, kk, 0, c0:c0 + 16] = inp["ssm_c_re"][l, g].T
            cpad[rows, l, kk, 1, c0:c0 + 16] = inp["ssm_c_im"][l, g].T
    m["bpad"] = bpad
    m["cpad"] = cpad
    m["ssm_d_t"] = np.ascontiguousarray(inp["ssm_d"].reshape(L, 2, 128).transpose(2, 0, 1), f)
    m.update(_host_consts())
    return m


def _layout_core(inp, core):
    f = np.float32
    rows = slice(core * NB, (core + 1) * NB)
    m = {}
    m["x"] = np.ascontiguousarray(inp["x"][rows], f)
    m["cT"] = np.ascontiguousarray(inp["c"][rows].reshape(NB, 8, 128).transpose(2, 1, 0), f)
    m["b_mod2"] = np.ascontiguousarray(np.broadcast_to(inp["b_mod"][:, None, :], (L, NB, 6 * D)), f)
    return m


_CACHE = {}


def kernel(**inputs):
    inp = {k_: np.asarray(v) for k_, v in inputs.items()}
    if "nc" not in _CACHE:
        nc = bass.Bass("TRN2", target_bir_lowering=False)
        build(nc)
        _CACHE["nc"] = nc
    nc = _CACHE["nc"]
    common = _layout_common(inp)
    in_maps = []
    for core in range(NCORES):
        m = dict(common)
        m.update(_layout_core(inp, core))
        in_maps.append(m)
    res = run_bass_kernel_spmd(nc, in_maps, core_ids=list(range(NCORES)))
    out = np.concatenate([np.asarray(r["out"], np.float32) for r in res.results], axis=0)
    return out
```

```python
import numpy as np
import ml_dtypes
from contextlib import ExitStack
import concourse.bass as bass
import concourse.mybir as mybir
from concourse.bass_utils import run_bass_kernel_spmd

F32 = mybir.dt.float32
BF16 = mybir.dt.bfloat16
AF = mybir.ActivationFunctionType
ALU = mybir.AluOpType
AX = mybir.AxisListType

S = 2048
D = 1024
NB = 2
L = 2
DFF = 2816
DIN_P = 5120
NCORES = 8
NEG = -1e30
NDS = 24
BISECT_ITERS = 10
FUSE_NORM = True
SSM_PIPE = True
DBG_TILES = (3, 5, 8, 14)
TWO_PI = 6.283185307179586
MAGIC = 12582912.0


class Buf:
    __slots__ = ("t", "w", "r", "name")

    def __init__(self, t, name=""):
        self.t = t
        self.w = {}
        self.r = {}
        self.name = name

    def __getitem__(self, idx):
        return self.t[idx]


class KB:
    def __init__(self, nc, st):
        self.nc = nc
        self.E = {"pe": nc.tensor, "act": nc.scalar, "dve": nc.vector, "pool": nc.gpsimd, "sp": nc.sync}
        self.semobj = {}
        self.cnt = {e: 0 for e in self.E}
        self.seen = {e: {} for e in self.E}
        for e in self.E:
            self.semobj[e] = st.enter_context(nc.semaphore("s_" + e))
        self.dcnt = [0] * NDS
        self.dnext = 0
        for i in range(NDS):
            self.semobj["d%d" % i] = st.enter_context(nc.semaphore("dq%d" % i))
        self.ninstr = 0

    def _wait(self, eng, sid, val):
        if self.seen[eng].get(sid, 0) >= val:
            return
        self.E[eng].wait_ge(self.semobj[sid], val)
        self.seen[eng][sid] = val

    def _deps(self, eng, reads, writes, merge):
        for b in reads:
            for sid, val in b.w.items():
                if sid == eng and eng in ("pe", "sp"):
                    continue
                self._wait(eng, sid, val)
        strict = eng in ("act", "dve", "pool")
        for b in list(writes) + list(merge):
            if b not in merge:
                for sid, val in b.w.items():
                    if sid != eng or strict:
                        self._wait(eng, sid, val)
            for sid, val in b.r.items():
                if sid != eng or strict:
                    self._wait(eng, sid, val)

    def _reg(self, sid, val, reads, writes, merge):
        for b in reads:
            if b.r.get(sid, 0) < val:
                b.r[sid] = val
        for b in writes:
            b.w = {sid: val}
            b.r = {}
        for b in merge:
            b.w[sid] = val
            b.r = {}

    def op(self, eng, fn, reads=(), writes=(), inc=True, merge=()):
        self._deps(eng, reads, writes, merge)
        ins = fn()
        self.ninstr += 1
        if inc:
            self.cnt[eng] += 1
            ins.then_inc(self.semobj[eng], 1)
            val = self.cnt[eng]
        else:
            val = self.cnt[eng] + 1
        self._reg(eng, val, reads, writes, merge)
        return ins

    def dma(self, q, out, in_, reads=(), writes=(), merge=(), **kw):
        slot = self.dnext
        self.dnext = (self.dnext + 1) % NDS
        sid = "d%d" % slot
        if self.dcnt[slot] > 0:
            self._wait(q, sid, 16 * self.dcnt[slot])
        self._deps(q, reads, writes, merge)
        self.dcnt[slot] += 1
        self.E[q].dma_start(out=out, in_=in_, **kw).then_inc(self.semobj[sid], 16)
        self.ninstr += 1
        self._reg(sid, 16 * self.dcnt[slot], reads, writes, merge)

    def barrier(self, engines=None):
        engines = engines or list(self.E)
        for e in engines:
            for f in self.E:
                if f != e and self.cnt[f] > 0:
                    self._wait(e, f, self.cnt[f])
            for i in range(NDS):
                if self.dcnt[i] > 0:
                    self._wait(e, "d%d" % i, 16 * self.dcnt[i])


class Pool:
    def __init__(self, bufs):
        self.bufs = bufs
        self.i = 0

    def get(self):
        b = self.bufs[self.i]
        self.i = (self.i + 1) % len(self.bufs)
        return b


_UN = [0]


def un(name):
    _UN[0] += 1
    return "%s_u%d" % (name, _UN[0])


def build(nc, stage=99, dbg=None):
    st = ExitStack()
    k = KB(nc, st)
    V, A, P, T = nc.vector, nc.scalar, nc.gpsimd, nc.tensor

    def dram_in(name, shape, dt=F32):
        return nc.dram_tensor(name, list(shape), dt, kind="ExternalInput").ap()

    x_d = dram_in("x", [NB, S, D])
    out_d = nc.dram_tensor("out", [NB, S, D], F32, kind="ExternalOutput").ap()
    cT_d = dram_in("cT", [128, 8, NB])
    wmod_d = dram_in("w_mod", [L, 8, 128, 6 * D])
    bmod_d = dram_in("b_mod2", [L, NB, 6 * D])
    nrm_d = dram_in("nrm", [128, 2 * L, 8])
    normf_d = dram_in("normf_b", [128, D])
    bgate_d = dram_in("b_gate_t", [128, L, 24])
    convw_d = dram_in("conv_w_t", [128, L, 3, 2])
    ssmsc_d = dram_in("ssm_sc", [128, L, 8, 3])
    bpad_d = dram_in("bpad", [128, L, 8, 2, 32])
    cpad_d = dram_in("cpad", [128, L, 8, 2, 128])
    ssmd_d = dram_in("ssm_d_t", [128, L, 2])
    kaug_d = dram_in("kaug_c", [14, S], BF16)
    dtab_d = dram_in("dtab", [128, S], BF16)
    sctab_d = dram_in("sctab", [128, 8])
    qaug_d = dram_in("qaug_c", [16, 6, 1024], BF16)
    cU_d = dram_in("constU", [128, 128], BF16)
    cDneg_d = dram_in("constDneg", [128, 1024], BF16)
    cIrep_d = dram_in("constIrep", [128, 1024], BF16)
    ident_d = dram_in("ident", [128, 128])
    diagm_d = dram_in("diagmask", [128, 128])
    iota_d = dram_in("iota512", [128, 512])
    sel_d = dram_in("sel65", [65, 64])

    wspecs = {
        "w_in": (D, DIN_P),
        "w_conv_out": (256, D),
        "w_glu": (256, 2 * D),
        "w_attn_out": (512, D),
        "w_o": (D, D),
        "w_ffn_in": (D, 2 * DFF),
        "w_ffn_out": (DFF, D),
    }
    wf32 = {}
    wbf = {}
    wbuf = {}
    for name, (kk, nn) in wspecs.items():
        wf32[name] = dram_in(name, [L, kk, nn])
        wbf[name] = nc.dram_tensor(name + "_b", [L, kk, nn], BF16, kind="Internal").ap()
        for l in range(L):
            wbuf[(name, l)] = Buf(None, name + str(l))

    dbg_out = {}

    def dbg_tensor(name, shape, dt=F32):
        t = nc.dram_tensor("dbg_" + name, list(shape), dt, kind="ExternalOutput").ap()
        dbg_out[name] = t
        return t

    def sb(name, shape, dt=F32):
        return Buf(st.enter_context(nc.sbuf_tensor(un("S_" + name), list(shape), dt)), name)

    def ps(name, shape, dt=F32):
        return Buf(st.enter_context(nc.psum_tensor(un("P_" + name), list(shape), dt)), name)

    def prepass(l, name):
        kk, nn = wspecs[name]
        rows = kk * nn // 2048
        src = wf32[name][l].rearrange("k n -> (k n)").rearrange("(r c) -> r c", c=2048)
        dst = wbf[name][l].rearrange("k n -> (k n)").rearrange("(r c) -> r c", c=2048)
        sid = "pp_%s_%d" % (name, l)
        k.semobj[sid] = st.enter_context(nc.semaphore(sid))
        n_ = 0
        for r0 in range(0, rows, 1024):
            r1 = min(rows, r0 + 1024)
            P.dma_start(out=dst[r0:r1, :], in_=src[r0:r1, :]).then_inc(k.semobj[sid], 16)
            n_ += 1
        wbuf[(name, l)].w = {sid: 16 * n_}

    prepass(0, "w_in")

    xT = sb("xT", [128, 8, S])
    hT = sb("hT", [128, 8, S], BF16)
    hTb = [Buf(hT.t, "hT%d" % i) for i in range(4)]
    xTb = [Buf(xT.t, "xT%d" % i) for i in range(4)]

    def HB(sl):
        return hTb[sl.start // 512]

    def XB(sl):
        return xTb[sl.start // 512]
    ident = sb("ident", [128, 128])
    identb = sb("identb", [128, 128], BF16)
    onesb = sb("onesb", [128, 128], BF16)
    nrm = sb("nrm_s", [128, 2 * L, 8])
    modT = sb("modT", [128, L, 48, NB])
    esc = sb("esc", [128, L, 2, NB, 8])
    bgate = sb("bgate", [128, L, 24])
    convw = sb("convw", [128, L, 3, 2])
    ssmd = sb("ssmd", [128, L, 2])
    eps_t = sb("eps_t", [128, 1])
    halfpi = sb("halfpi", [128, 1])
    zero_t = sb("zero_t", [128, 1])

    for dst, src in ((ident, ident_d), (nrm, nrm_d), (bgate, bgate_d),
                     (convw, convw_d), (ssmd, ssmd_d)):
        k.dma("sp", dst[:], src, writes=[dst])
    k.op("dve", lambda: V.tensor_copy(out=identb[:], in_=ident[:]), [ident], [identb])
    k.op("dve", lambda: V.memset(onesb[:], 1.0 / D), [], [onesb])
    k.op("dve", lambda: V.memset(eps_t[:], 1e-6), [], [eps_t])
    k.op("dve", lambda: V.memset(halfpi[:], TWO_PI / 4), [], [halfpi])
    k.op("dve", lambda: V.memset(zero_t[:], 0.0), [], [zero_t])

    psb = [ps("psb%d" % i, [128, 512]) for i in range(8)]
    pp = Pool(psb)

    with ExitStack() as ph:
        def sbp(name, shape, dt=F32):
            return Buf(ph.enter_context(nc.sbuf_tensor(un(name), list(shape), dt)), name)
        cT = sbp("cT_s", [128, 8, NB])
        cact = sbp("cact", [128, 8, NB])
        modtok = sbp("modtok", [NB, 6 * D])
        bmod = sbp("bmod", [NB, 6 * D])
        wm = Pool([sbp("wm%d" % i, [128, 8, 512]) for i in range(2)])
        k.dma("sp", cT[:], cT_d, writes=[cT])
        k.op("act", lambda: A.activation(out=cact[:], in_=cT[:], func=AF.Silu), [cT], [cact])
        for l in range(L):
            k.dma("sp", bmod[:], bmod_d[l], writes=[bmod])
            for nb in range(12):
                w = wm.get()
                k.dma("sp", w[:], wmod_d[l, :, :, nb * 512:(nb + 1) * 512].rearrange("kc p n -> p kc n"),
                      writes=[w])
                pt = pp.get()
                for kc in range(8):
                    k.op("pe", lambda: T.matmul(pt[0:NB, :], lhsT=cact[:, kc, :], rhs=w[:, kc, :],
                                                start=(kc == 0), stop=(kc == 7)),
                         [cact, w], [pt], inc=(kc == 7))
                k.op("dve", lambda: V.tensor_tensor(out=modtok[:, nb * 512:(nb + 1) * 512], in0=pt[0:NB, :],
                                                    in1=bmod[:, nb * 512:(nb + 1) * 512], op=ALU.add),
                     [pt, bmod], [modtok])
            pt = pp.get()
            for c in range(48):
                k.op("pe", lambda: T.transpose(out=pt[:, c * NB:(c + 1) * NB], in_=modtok[:, c * 128:(c + 1) * 128],
                                               identity=ident[0:NB, 0:NB]),
                     [modtok, ident], [pt], inc=(c == 47))
            k.op("dve", lambda: V.tensor_copy(out=modT[:, l, :, :].rearrange("p c b -> p (c b)"),
                                              in_=pt[:, 0:48 * NB]), [pt], [modT])
            for j, (sci, ni) in enumerate(((1, 2 * l), (4, 2 * l + 1))):
                for b in range(NB):
                    k.op("dve", lambda: V.scalar_tensor_tensor(
                        out=esc[:, l, j, b, :], in0=modT[:, l, sci * 8:(sci + 1) * 8, b], scalar=1.0,
                        in1=nrm[:, ni, :], op0=ALU.add, op1=ALU.mult), [modT, nrm], [esc])
        k.barrier()

    for l in range(L):
        for name in wspecs:
            if not (l == 0 and name == "w_in"):
                prepass(l, name)
    if dbg is not None and "modT" in dbg:
        t = dbg_tensor("modT", [128, L * 48 * NB])
        k.dma("act", t, modT[:].rearrange("p l c b -> p (l c b)"), reads=[modT])
        t = dbg_tensor("esc", [128, L * 2 * NB * 8])
        k.dma("act", t, esc[:].rearrange("p l j b c -> p (l j b c)"), reads=[esc])

    def load_x(b):
        with ExitStack() as ph:
            xin = Pool([Buf(ph.enter_context(nc.sbuf_tensor(un("xin%d" % i), [128, D], F32))) for i in range(2)])
            for ti in range(16):
                xt = xin.get()
                k.dma("sp", xt[:], x_d[b, ti * 128:(ti + 1) * 128, :], writes=[xt])
                for half in range(2):
                    pt = pp.get()
                    for j in range(4):
                        kc = half * 4 + j
                        k.op("pe", lambda: T.transpose(out=pt[:, j * 128:(j + 1) * 128],
                                                       in_=xt[:, kc * 128:(kc + 1) * 128], identity=ident[:]),
                             [xt, ident], [pt], inc=(j == 3))
                    eng = "act" if half == 0 else "dve"
                    o = xT[:, half * 4:(half + 1) * 4, ti * 128:(ti + 1) * 128]
                    i_ = pt[:, :].rearrange("p (c t) -> p c t", c=4)
                    if eng == "act":
                        k.op("act", lambda: A.activation(out=o, in_=i_, func=AF.Copy), [pt], [xTb[ti // 4]])
                    else:
                        k.op("dve", lambda: V.tensor_copy(out=o, in_=i_), [pt], [xTb[ti // 4]])
            k.barrier()

    def norm_pools(ph):
        sqp = Pool([Buf(ph.enter_context(nc.sbuf_tensor(un("sq%d" % i), [128, 512], BF16))) for i in range(4)])
        rsp = Pool([Buf(ph.enter_context(nc.sbuf_tensor(un("rstd%d" % i), [128, 512], F32))) for i in range(1)])
        tmp = Pool([Buf(ph.enter_context(nc.sbuf_tensor(un("nmt%d" % i), [128, 512], F32))) for i in range(2)])
        return sqp, rsp, tmp

    def norm_tb(l, j, b, tb, pools):
        sqp, rsp, tmp = pools
        shi = 0 if j == 0 else 3
        cols = slice(tb * 512, (tb + 1) * 512)
        rstd = rsp.get()
        pt = pp.get()
        for kc in range(8):
            sq = sqp.get()
            k.op("act", lambda: A.activation(out=sq[:], in_=xT[:, kc, cols], func=AF.Square), [xTb[tb]], [sq])
            k.op("pe", lambda: T.matmul(pt[:, :], lhsT=onesb[:], rhs=sq[:], start=(kc == 0), stop=(kc == 7)),
                 [onesb, sq], [pt], inc=True)
        k.op("act", lambda: A.activation(out=rstd[:], in_=pt[:, :], func=AF.Sqrt, bias=eps_t[:], scale=1.0),
             [pt, eps_t], [rstd])
        k.op("dve", lambda: V.reciprocal(out=rstd[:], in_=rstd[:]), [rstd], [rstd])
        for kc in range(8):
            t_ = tmp.get()
            k.op("dve", lambda: V.tensor_tensor(out=t_[:], in0=xT[:, kc, cols], in1=rstd[:], op=ALU.mult),
                 [xTb[tb], rstd], [t_])
            k.op("act", lambda: A.activation(out=hT[:, kc, cols], in_=t_[:], func=AF.Identity,
                                             bias=modT[:, l, shi * 8 + kc, b:b + 1],
                                             scale=esc[:, l, j, b, kc:kc + 1]),
                 [t_, modT, esc], [hTb[tb]])

    def norm_mod(l, j, b, ph):
        pools = norm_pools(ph)
        for tb in range(4):
            norm_tb(l, j, b, tb, pools)

    def final_store(b):
        with ExitStack() as ph:
            normf_b = Buf(ph.enter_context(nc.sbuf_tensor(un("normf_bs"), [128, D], F32)))
            k.dma("sp", normf_b[:], normf_d, writes=[normf_b])
            ot = Pool([Buf(ph.enter_context(nc.sbuf_tensor(un("ot%d" % i), [128, D], F32))) for i in range(2)])
            junk = Buf(ph.enter_context(nc.sbuf_tensor(un("fjunk"), [128, D], BF16)))
            ss = Pool([Buf(ph.enter_context(nc.sbuf_tensor(un("ss%d" % i), [128, 2], F32))) for i in range(2)])
            rs = Pool([Buf(ph.enter_context(nc.sbuf_tensor(un("rs%d" % i), [128, 1], F32))) for i in range(2)])
            for ti in range(16):
                pts = []
                s_ = ss.get()
                for half in range(2):
                    pt = pp.get()
                    for j in range(4):
                        kc = half * 4 + j
                        k.op("pe", lambda: T.transpose(out=pt[:, j * 128:(j + 1) * 128],
                                                       in_=xT[:, kc, ti * 128:(ti + 1) * 128], identity=ident[:]),
                             [xTb[ti // 4], ident], [pt], inc=(j == 3))
                    k.op("act", lambda: A.activation(out=junk[:, half * 512:(half + 1) * 512], in_=pt[:, :],
                                                     func=AF.Square, accum_out=s_[:, half:half + 1]),
                         [pt], [junk, s_])
                    pts.append(pt)
                r_ = rs.get()
                k.op("dve", lambda: V.tensor_tensor(out=r_[:], in0=s_[:, 0:1], in1=s_[:, 1:2], op=ALU.add),
                     [s_], [r_])
                k.op("act", lambda: A.activation(out=r_[:], in_=r_[:], func=AF.Sqrt, bias=eps_t[:], scale=1.0 / D),
                     [r_, eps_t], [r_])
                k.op("dve", lambda: V.reciprocal(out=r_[:], in_=r_[:]), [r_], [r_])
                o = ot.get()
                for half in range(2):
                    k.op("dve", lambda: V.scalar_tensor_tensor(
                        out=o[:, half * 512:(half + 1) * 512], in0=pts[half][:, :], scalar=r_[:, 0:1],
                        in1=normf_b[:, half * 512:(half + 1) * 512], op0=ALU.mult, op1=ALU.mult),
                        [pts[half], r_, normf_b], [o])
                k.dma("act", out_d[b, ti * 128:(ti + 1) * 128, :], o[:], reads=[o])
            k.barrier()

    thrall = sb("thrall", [128, 1])
    k.op("dve", lambda: V.memset(thrall[:], -1e29), [], [thrall])

    theta = sb("theta", [128, L * 8])
    rdec = sb("rdec", [128, L * 8])
    coefr = sb("coefr", [128, L * 8])
    coefi = sb("coefi", [128, L * 8])
    with ExitStack() as ph:
        def sbp(name, shape, dt=F32):
            return Buf(ph.enter_context(nc.sbuf_tensor(un(name), list(shape), dt)), name)
        sc = sbp("ssc", [128, L * 8, 3])
        k.dma("sp", sc[:], ssmsc_d.rearrange("p l k c -> p (l k) c"), writes=[sc])
        ar, ai, ldt = sc[:, :, 0], sc[:, :, 1], sc[:, :, 2]
        tt = [sbp("sst%d" % i, [128, L * 8]) for i in range(10)]
        dt_, dtar, t1, kk, thr_, sn, ab, cs, abr, abi = tt
        k.op("act", lambda: A.activation(out=dt_[:], in_=ldt, func=AF.Exp), [sc], [dt_])
        k.op("dve", lambda: V.tensor_tensor(out=dtar[:], in0=dt_[:], in1=ar, op=ALU.mult), [dt_, sc], [dtar])
        k.op("dve", lambda: V.tensor_tensor(out=theta[:], in0=dt_[:], in1=ai, op=ALU.mult), [dt_, sc], [theta])
        k.op("act", lambda: A.activation(out=rdec[:], in_=dtar[:], func=AF.Exp), [dtar], [rdec])
        k.op("dve", lambda: V.tensor_scalar(out=t1[:], in0=theta[:], scalar1=1.0 / TWO_PI, scalar2=MAGIC,
                                            op0=ALU.mult, op1=ALU.add), [theta], [t1])
        k.op("dve", lambda: V.tensor_scalar(out=kk[:], in0=t1[:], scalar1=MAGIC, scalar2=-TWO_PI,
                                            op0=ALU.subtract, op1=ALU.mult), [t1], [kk])
        k.op("dve", lambda: V.tensor_tensor(out=thr_[:], in0=theta[:], in1=kk[:], op=ALU.add), [theta, kk], [thr_])
        k.op("dve", lambda: V.tensor_scalar(out=thr_[:], in0=thr_[:], scalar1=-3.141592, scalar2=3.141592,
                                            op0=ALU.max, op1=ALU.min), [thr_], [thr_])
        k.op("act", lambda: A.activation(out=sn[:], in_=thr_[:], func=AF.Sin), [thr_], [sn])
        k.op("dve", lambda: V.tensor_scalar(out=ab[:], in0=thr_[:], scalar1=TWO_PI / 4, scalar2=-TWO_PI,
                                            op0=ALU.is_gt, op1=ALU.mult), [thr_], [ab])
        k.op("dve", lambda: V.scalar_tensor_tensor(out=ab[:], in0=thr_[:], scalar=TWO_PI / 4, in1=ab[:],
                                                   op0=ALU.add, op1=ALU.add), [thr_, ab], [ab])
        k.op("dve", lambda: V.tensor_scalar(out=ab[:], in0=ab[:], scalar1=-3.141592, scalar2=3.141592,
                                            op0=ALU.max, op1=ALU.min), [ab], [ab])
        k.op("act", lambda: A.activation(out=cs[:], in_=ab[:], func=AF.Sin), [ab], [cs])
        k.op("dve", lambda: V.tensor_tensor(out=abr[:], in0=rdec[:], in1=cs[:], op=ALU.mult), [rdec, cs], [abr])
        k.op("dve", lambda: V.tensor_tensor(out=abi[:], in0=rdec[:], in1=sn[:], op=ALU.mult), [rdec, sn], [abi])
        u1, u2, den, nr = dt_, dtar, t1, kk
        k.op("dve", lambda: V.tensor_scalar(out=nr[:], in0=abr[:], scalar1=-1.0, scalar2=None, op0=ALU.add),
             [abr], [nr])
        k.op("dve", lambda: V.tensor_tensor(out=u1[:], in0=ar, in1=ar, op=ALU.mult), [sc], [u1])
        k.op("dve", lambda: V.tensor_tensor(out=u2[:], in0=ai, in1=ai, op=ALU.mult), [sc], [u2])
        k.op("dve", lambda: V.tensor_tensor(out=den[:], in0=u1[:], in1=u2[:], op=ALU.add), [u1, u2], [den])
        k.op("dve", lambda: V.reciprocal(out=den[:], in_=den[:]), [den], [den])
        k.op("dve", lambda: V.tensor_tensor(out=u1[:], in0=nr[:], in1=ar, op=ALU.mult), [nr, sc], [u1])
        k.op("dve", lambda: V.tensor_tensor(out=u2[:], in0=abi[:], in1=ai, op=ALU.mult), [abi, sc], [u2])
        k.op("dve", lambda: V.tensor_tensor(out=u1[:], in0=u1[:], in1=u2[:], op=ALU.add), [u1, u2], [u1])
        k.op("dve", lambda: V.tensor_tensor(out=coefr[:], in0=u1[:], in1=den[:], op=ALU.mult), [u1, den], [coefr])
        k.op("dve", lambda: V.tensor_tensor(out=u1[:], in0=abi[:], in1=ar, op=ALU.mult), [abi, sc], [u1])
        k.op("dve", lambda: V.tensor_tensor(out=u2[:], in0=nr[:], in1=ai, op=ALU.mult), [nr, sc], [u2])
        k.op("dve", lambda: V.tensor_tensor(out=u1[:], in0=u1[:], in1=u2[:], op=ALU.subtract), [u1, u2], [u1])
        k.op("dve", lambda: V.tensor_tensor(out=coefi[:], in0=u1[:], in1=den[:], op=ALU.mult), [u1, den], [coefi])
        k.barrier()
    if dbg is not None and "ssmsetup" in dbg:
        for nme, tl in (("theta", theta), ("rdec", rdec), ("coefr", coefr), ("coefi", coefi)):
            t = dbg_tensor(nme, [128, L * 8])
            k.dma("act", t, tl[:], reads=[tl])

    def wload(dst, name, l, c0, c1, kc=8, p=128, k0=0):
        src = wbf[name][l, k0:k0 + kc * p, c0:c1].rearrange("(kc p) n -> p kc n", p=p)
        k.dma("sp", dst[0:p, 0:kc, 0:c1 - c0], src, reads=[wbuf[(name, l)]], writes=[dst])

    def attn_phase(l, b, oT):
        with ExitStack() as ph:
            def sbp(name, shape, dt=F32):
                return Buf(ph.enter_context(nc.sbuf_tensor(un(name), list(shape), dt)), name)
            Lp = Pool(psb[0:4])
            Ob = [psb[4], psb[5]]
            Mp = Pool(psb[6:8])
            wQI = sbp("wQI", [128, 8, 256], BF16)
            wq = sbp("wq", [128, 8, 512], BF16)
            kaug = sbp("kaug", [128, S], BF16)
            kiT = sbp("kiT", [64, S], BF16)
            Vp = sbp("Vp", [128, 16, 65], BF16)
            wiT = sbp("wiT", [128, 16, 4])
            qps = Pool([sbp("qp%d" % i, [128, 1024], BF16) for i in range(3)])
            qis = Pool([sbp("qi%d" % i, [64, 512], BF16) for i in range(2)])
            Rts = Pool([sbp("Rt%d" % i, [128, 512]) for i in range(2)])
            pts = Pool([sbp("pt%d" % i, [128, 512], BF16) for i in range(3)])
            accs = Pool([sbp("accS%d" % i, [65, 512]) for i in range(1)])
            recs = Pool([sbp("rec%d" % i, [64, 512]) for i in range(1)])
            sm = Pool([sbp("sm%d" % i, [128, 8]) for i in range(8)])
            pw = sbp("pw", [128, 32])
            hss = Pool([sbp("hs%d" % i, [128, 64]) for i in range(2)])
            for it in range(BISECT_ITERS):
                k.op("dve", lambda: V.memset(pw[:, it:it + 1], 2.0 ** (-(it + 1))), [], [pw])
            cU = sbp("cU", [128, 128], BF16)
            cDneg = sbp("cDneg", [128, 1024], BF16)
            cIrep = sbp("cIrep", [128, 1024], BF16)
            diagm = sbp("diagm", [128, 128])
            sel65 = sbp("sel65", [65, 64])
            dtab = sbp("dtab", [128, S], BF16)
            sctab = sbp("sctab", [128, 8])
            dsh = sbp("dsh", [128, 128])
            ones8 = sbp("ones8", [128, 8])
            dm8s = Pool([sbp("dm8%d" % i, [128, 32]) for i in range(4)])
            ones32 = sbp("ones32", [128, 32])
            k.op("dve", lambda: V.memset(ones32[:], 1.0), [], [ones32])
            k.op("dve", lambda: V.memset(ones8[:], 1.0), [], [ones8])
            for dst, src in ((cU, cU_d), (cDneg, cDneg_d), (cIrep, cIrep_d), (diagm, diagm_d), (sel65, sel_d),
                             (dtab, dtab_d), (sctab, sctab_d)):
                k.dma("sp", dst[:], src, writes=[dst])
            wstg = ExitStack()
            wD = Buf(wstg.enter_context(nc.sbuf_tensor(un("wD"), [128, 8, 452], BF16)), "wD")
            wload(wD, "w_in", l, 1536, 1988)
            wload(wQI, "w_in", l, 1664, 1920)
            wload(wq, "w_in", l, 1024, 1536)
            k.dma("sp", kaug[64:78, :], kaug_d, merge=[kaug])
            k.op("dve", lambda: V.memset(Vp[:, :, 64:65], 1.0), [], [Vp])
            for tb in range(4):
                cols = slice(tb * 512, (tb + 1) * 512)
                for (c0, dst) in ((0, kaug), (384, kiT)):
                    pt = Mp.get()
                    for kc in range(8):
                        k.op("pe", lambda: T.matmul(pt[0:64, :], lhsT=wD[:, kc, c0:c0 + 64], rhs=hT[:, kc, cols],
                                                    start=(kc == 0), stop=(kc == 7)), [wD, HB(cols)], [pt], inc=(kc == 7))
                    k.op("act", lambda: A.activation(out=dst[0:64, cols], in_=pt[0:64, :], func=AF.Copy),
                         [pt], [], merge=[dst])
            for ti in range(16):
                tc_ = slice(ti * 128, (ti + 1) * 128)
                pt = Mp.get()
                for kc in range(8):
                    k.op("pe", lambda: T.matmul(pt[:, 0:64], lhsT=hT[:, kc, tc_], rhs=wD[:, kc, 64:128],
                                                start=(kc == 0), stop=(kc == 7)), [wD, HB(tc_)], [pt], inc=(kc == 7))
                k.op("dve", lambda: V.tensor_copy(out=Vp[:, ti, 0:64], in_=pt[:, 0:64]), [pt, Vp], [], merge=[Vp])
                pt2 = Mp.get()
                for kc in range(8):
                    k.op("pe", lambda: T.matmul(pt2[:, 0:4], lhsT=hT[:, kc, tc_], rhs=wD[:, kc, 448:452],
                                                start=(kc == 0), stop=(kc == 7)), [wD, HB(tc_)], [pt2], inc=(kc == 7))
                k.op("act", lambda: A.activation(out=wiT[:, ti, :], in_=pt2[:, 0:4], func=AF.Copy, scale=1.0 / 16),
                     [pt2], [wiT])

            k.barrier()
            wstg.close()
            idxs = Pool([sbp("idx%d" % i, [128, S]) for i in range(3)])
            mbs = Pool([sbp("mb%d" % i, [128, S], BF16) for i in range(2)])
            junk = sbp("junk", [128, S], mybir.dt.uint8)
            rkb = sbp("rkb", [128, S], mybir.dt.float16)
            if dbg is not None and "attn" in dbg and b == 0 and l == 0:
                t = dbg_tensor("wiT", [128, 64])
                k.dma("act", t, wiT[:].rearrange("p a c -> p (a c)"), reads=[wiT])
            F16 = mybir.dt.float16

            def stage_P(i):
                tc_ = slice(i * 128, (i + 1) * 128)
                nk = 128 * (i + 1)
                qp = qps.get()
                k.dma("sp", qp[72:78, :], qaug_d[i], merge=[qp])
                for half in range(2):
                    pt = Lp.get()
                    for hh in range(4):
                        h = half * 4 + hh
                        for kc in range(8):
                            k.op("pe", lambda: T.matmul(pt[0:64, hh * 128:(hh + 1) * 128],
                                                        lhsT=wq[:, kc, h * 64:(h + 1) * 64], rhs=hT[:, kc, tc_],
                                                        start=(kc == 0), stop=(kc == 7)),
                                 [wq, HB(tc_)], [pt], inc=(hh == 3 and kc == 7))
                    k.op("act", lambda: A.activation(out=qp[0:64, half * 512:(half + 1) * 512], in_=pt[0:64, :],
                                                     func=AF.Copy, scale=0.125), [pt], [], merge=[qp])
                qi = qis.get()
                pt = Lp.get()
                for hh in range(4):
                    for kc in range(8):
                        k.op("pe", lambda: T.matmul(pt[0:64, hh * 128:(hh + 1) * 128],
                                                    lhsT=wQI[:, kc, hh * 64:(hh + 1) * 64],
                                                    rhs=hT[:, kc, tc_], start=(kc == 0), stop=(kc == 7)),
                             [wQI, HB(tc_)], [pt], inc=(hh == 3 and kc == 7))
                k.op("act", lambda: A.activation(out=qi[:], in_=pt[0:64, :], func=AF.Copy), [pt], [qi])
                idx = idxs.get()
                for kb2 in range((nk + 511) // 512):
                    w_ = min(512, nk - kb2 * 512)
                    kc_ = slice(kb2 * 512, kb2 * 512 + w_)
                    for hh in range(4):
                        pt = Mp.get()
                        k.op("pe", lambda: T.matmul(pt[:, 0:w_], lhsT=qi[:, hh * 128:(hh + 1) * 128],
                                                    rhs=kiT[0:64, kc_], start=True, stop=True), [qi, kiT], [pt])
                        rt = Rts.get()
                        k.op("act", lambda: A.activation(out=rt[:, 0:w_], in_=pt[:, 0:w_], func=AF.Relu), [pt], [rt])
                        if hh == 0:
                            k.op("pool", lambda: P.tensor_scalar(out=idx[:, kc_], in0=rt[:, 0:w_],
                                                                 scalar1=wiT[:, i, 0:1], scalar2=0.0, op0=ALU.mult,
                                                                 op1=ALU.add), [rt, wiT], [idx])
                        else:
                            k.op("pool", lambda: P.tensor_scalar(out=rt[:, 0:w_], in0=rt[:, 0:w_],
                                                                 scalar1=wiT[:, i, hh:hh + 1], scalar2=0.0,
                                                                 op0=ALU.mult, op1=ALU.add), [rt, wiT], [rt])
                            k.op("pool", lambda: P.tensor_tensor(out=idx[:, kc_], in0=idx[:, kc_], in1=rt[:, 0:w_],
                                                                 op=ALU.add), [rt, idx], [idx])
                s_ = sm.get()
                return dict(i=i, nk=nk, qp=qp, idx=idx, s_=s_)

            def gen_A(c):
                i, nk, idx, s_ = c["i"], c["nk"], c["idx"], c["s_"]
                hi0, thr, cnt, e_ = s_[:, 0:1], s_[:, 1:2], s_[:, 2:3], s_[:, 3:4]
                if i >= 2:
                    k.op("dve", lambda: V.tensor_reduce(out=hi0, in_=idx[:, 0:nk], axis=AX.X, op=ALU.max,
                                                        apply_absolute_value=True), [idx], [s_])
                    yield
                    k.op("dve", lambda: V.tensor_scalar(out=hi0, in0=hi0, scalar1=1.01, scalar2=1e-6, op0=ALU.mult,
                                                        op1=ALU.add), [s_], [s_])
                    yield
                    k.op("dve", lambda: V.memset(thr, 0.0), [s_], [s_])
                    yield
                k.op("dve", lambda: V.tensor_tensor(out=idx[:, nk - 128:nk], in0=idx[:, nk - 128:nk], in1=diagm[:],
                                                    op=ALU.add), [idx, diagm], [idx])
                yield
                if i < 2:
                    return
                hs = hss.get()
                k.op("dve", lambda: V.tensor_scalar(out=hs[:, 0:BISECT_ITERS], in0=pw[:, 0:BISECT_ITERS],
                                                    scalar1=hi0, scalar2=None, op0=ALU.mult), [pw, s_], [hs])
                yield
                k.op("dve", lambda: V.tensor_scalar(out=hs[:, 32:32 + BISECT_ITERS], in0=pw[:, 0:BISECT_ITERS],
                                                    scalar1=hi0, scalar2=2.0, op0=ALU.mult, op1=ALU.mult),
                     [pw, s_], [hs])
                yield
                for it in range(BISECT_ITERS):
                    k.op("dve", lambda: V.tensor_scalar(out=junk[:, 0:nk], in0=idx[:, 0:nk], scalar1=thr,
                                                        scalar2=0.0, op0=ALU.is_ge, op1=ALU.add, accum_out=cnt),
                         [idx, s_], [junk, s_])
                    yield
                    k.op("dve", lambda: V.tensor_scalar(out=e_, in0=cnt, scalar1=255.5,
                                                        scalar2=hs[:, 32 + it:33 + it],
                                                        op0=ALU.is_ge, op1=ALU.mult), [s_, hs], [s_])
                    yield
                    k.op("dve", lambda: V.scalar_tensor_tensor(out=thr, in0=e_, scalar=hs[:, it:it + 1], in1=thr,
                                                               op0=ALU.subtract, op1=ALU.add), [s_, hs], [s_])
                    yield

            def gen_B(c):
                i, nk, idx, s_, qp = c["i"], c["nk"], c["idx"], c["s_"], c["qp"]
                mb = mbs.get()
                c["mb"] = mb
                if i >= 2:
                    hi0, thr = s_[:, 0:1], s_[:, 1:2]
                    lob, hib, chi, need = s_[:, 4:5], s_[:, 5:6], s_[:, 6:7], s_[:, 7:8]
                    cl = 2.0 ** (-BISECT_ITERS)
                    k.op("dve", lambda: V.scalar_tensor_tensor(out=lob, in0=hi0, scalar=-cl, in1=thr,
                                                               op0=ALU.mult, op1=ALU.add), [s_], [s_])
                    yield
                    k.op("dve", lambda: V.scalar_tensor_tensor(out=hib, in0=hi0, scalar=cl * 1.001, in1=thr,
                                                               op0=ALU.mult, op1=ALU.add), [s_], [s_])
                    yield
                    k.op("dve", lambda: V.tensor_scalar(out=hib, in0=hib, scalar1=1e-30, scalar2=None,
                                                        op0=ALU.add), [s_], [s_])
                    yield
                    k.op("dve", lambda: V.tensor_scalar(out=junk[:, 0:nk], in0=idx[:, 0:nk], scalar1=hib,
                                                        scalar2=0.0, op0=ALU.is_ge, op1=ALU.add, accum_out=chi),
                         [idx, s_], [junk, s_])
                    yield
                    k.op("dve", lambda: V.tensor_scalar(out=need, in0=chi, scalar1=-1.0, scalar2=256.0,
                                                        op0=ALU.mult, op1=ALU.add), [s_], [s_])
                    yield
                    k.op("dve", lambda: V.tensor_scalar(out=mb[:, 0:nk], in0=idx[:, 0:nk], scalar1=lob,
                                                        scalar2=None, op0=ALU.is_ge), [idx, s_], [mb])
                    yield
                    k.op("dve", lambda: V.scalar_tensor_tensor(out=mb[:, 0:nk], in0=idx[:, 0:nk], scalar=hib,
                                                               in1=mb[:, 0:nk], op0=ALU.is_lt, op1=ALU.mult),
                         [idx, s_, mb], [mb])
                    yield
                    k.op("dve", lambda: V.tensor_tensor_scan(out=rkb[:, 0:nk],
                                                             data0=ones8[:, 0:1].to_broadcast([128, nk]),
                                                             data1=mb[:, 0:nk], initial=0.0, op0=ALU.mult,
                                                             op1=ALU.add), [mb, ones8], [rkb])
                    yield
                    k.op("dve", lambda: V.scalar_tensor_tensor(out=rkb[:, 0:nk], in0=rkb[:, 0:nk], scalar=need,
                                                               in1=mb[:, 0:nk], op0=ALU.is_le, op1=ALU.mult),
                         [rkb, mb, s_], [rkb])
                    yield
                    k.op("dve", lambda: V.scalar_tensor_tensor(out=rkb[:, 0:nk], in0=idx[:, 0:nk], scalar=hib,
                                                               in1=rkb[:, 0:nk], op0=ALU.is_ge, op1=ALU.add),
                         [idx, s_, rkb], [rkb])
                    yield
                    k.op("dve", lambda: V.tensor_scalar(out=mb[:, 0:nk], in0=rkb[:, 0:nk], scalar1=-1.0,
                                                        scalar2=32768.0, op0=ALU.add, op1=ALU.mult), [rkb], [mb])
                    yield
                else:
                    k.op("dve", lambda: V.tensor_scalar(out=mb[:, 0:nk], in0=idx[:, 0:nk], scalar1=thrall[:, 0:1],
                                                        scalar2=-32768.0, op0=ALU.is_lt, op1=ALU.mult),
                         [idx, thrall], [mb])
                    yield
                s2 = sm.get()
                k.op("dve", lambda: V.scalar_tensor_tensor(out=rkb[:, 0:nk], in0=mb[:, 0:nk], scalar=-1.0,
                                                           in1=dtab[:, 1920 - 128 * i:2048], op0=ALU.mult,
                                                           op1=ALU.add), [mb, dtab], [rkb])
                yield
                k.op("dve", lambda: V.tensor_reduce(out=s2[:, 0:1], in_=rkb[:, 0:nk], axis=AX.X, op=ALU.min),
                     [rkb], [s2])
                yield
                dm32, tr32 = dm8s.get(), dm8s.get()
                k.op("dve", lambda: V.tensor_scalar(out=dm32[:], in0=ones32[:], scalar1=s2[:, 0:1], scalar2=None,
                                                    op0=ALU.mult), [ones32, s2], [dm32])
                yield
                k.op("dve", lambda: V.transpose(out=tr32[:], in_=dm32[:]), [dm32], [tr32])
                yield
                for bq in range(4):
                    k.op("dve", lambda: V.tensor_copy(out=dsh[64:72, bq * 32:(bq + 1) * 32],
                                                      in_=tr32[bq * 32:bq * 32 + 8, 0:32]), [tr32], [dsh])
                    yield
                for h in range(8):
                    k.op("dve", lambda: V.tensor_scalar(out=qp[64:72, h * 128:(h + 1) * 128], in0=dsh[64:72, :],
                                                        scalar1=sctab[64:72, h:h + 1], scalar2=None, op0=ALU.mult),
                         [dsh, sctab], [], merge=[qp])
                    yield
                if dbg is not None and "attn" in dbg and b == 0 and l == 0 and i in DBG_TILES:
                    t = dbg_tensor("idx%d" % i, [128, S])
                    k.dma("act", t, idx[:], reads=[idx])
                    t = dbg_tensor("mb%d" % i, [128, S], BF16)
                    k.dma("act", t, mb[:], reads=[mb])

            def stage_C(c):
                i, nk, qp, mb = c["i"], c["nk"], c["qp"], c["mb"]
                tc_ = slice(i * 128, (i + 1) * 128)
                pairs = [(j, half) for j in range(i + 1) for half in range(2)]
                Lts = {}

                def emit_L(n):
                    j, half = pairs[n]
                    kj = slice(j * 128, (j + 1) * 128)
                    hc = slice(half * 512, (half + 1) * 512)
                    Lt = Lp.get()
                    Lts[n] = Lt
                    k.op("pe", lambda: T.matmul(Lt[:, :], lhsT=kaug[0:78, kj], rhs=qp[0:78, hc],
                                                start=True, stop=False), [kaug, qp], [Lt], inc=False)
                    k.op("pe", lambda: T.matmul(Lt[:, :], lhsT=mb[:, kj], rhs=cIrep[:, hc],
                                                start=False, stop=(j != i)), [mb, cIrep], [Lt], inc=(j != i))
                    if j == i:
                        k.op("pe", lambda: T.matmul(Lt[:, :], lhsT=cU[:], rhs=cDneg[:, hc],
                                                    start=False, stop=True), [cU, cDneg], [Lt])

                def emit_exp_pv(n):
                    j, half = pairs[n]
                    Lt = Lts.pop(n)
                    p_ = pts.get()
                    k.op("act", lambda: A.activation(out=p_[:], in_=Lt[:, :], func=AF.Exp), [Lt], [p_])
                    k.op("pe", lambda: T.matmul(Ob[half][0:65, :], lhsT=Vp[:, j, 0:65], rhs=p_[:],
                                                start=(j == 0), stop=(j == i)), [Vp, p_], [Ob[half]],
                         inc=(j == i))

                for n in range(min(2, len(pairs))):
                    emit_L(n)
                for n in range(len(pairs)):
                    if n + 2 < len(pairs):
                        emit_L(n + 2)
                    emit_exp_pv(n)
                for half in range(2):
                    acc = accs.get()
                    k.op("act", lambda: A.activation(out=acc[0:65, :], in_=Ob[half][0:65, :], func=AF.Copy),
                         [Ob[half]], [acc])
                    pt = Mp.get()
                    k.op("pe", lambda: T.matmul(pt[0:64, :], lhsT=sel65[0:65, :], rhs=acc[0:65, :],
                                                start=True, stop=True), [sel65, acc], [pt])
                    rec = recs.get()
                    k.op("act", lambda: A.activation(out=rec[:], in_=pt[0:64, :], func=AF.Ln), [pt], [rec])
                    k.op("act", lambda: A.activation(out=rec[:], in_=rec[:], func=AF.Exp, scale=-1.0), [rec], [rec])
                    for par in range(2):
                        av = acc[0:64, :].rearrange("p (a q t) -> p a q t", a=2, q=2)[:, :, par, :]
                        rv = rec[:, :].rearrange("p (a q t) -> p a q t", a=2, q=2)[:, :, par, :]
                        k.op("pool", lambda: P.tensor_tensor(
                            out=oT[par * 64:(par + 1) * 64, half * 2:half * 2 + 2, tc_], in0=av, in1=rv,
                            op=ALU.mult), [acc, rec], [oT])

            def run(g):
                for _ in g:
                    pass

            def interleave(g1, g2):
                live = [g1, g2]
                while live:
                    for g in list(live):
                        try:
                            next(g)
                        except StopIteration:
                            live.remove(g)

            ctxs = {0: stage_P(0), 1: stage_P(1)}
            run(gen_A(ctxs[0]))
            for s_i in range(16):
                if s_i + 2 < 16:
                    ctxs[s_i + 2] = stage_P(s_i + 2)
                if s_i + 1 < 16:
                    interleave(gen_A(ctxs[s_i + 1]), gen_B(ctxs[s_i]))
                else:
                    run(gen_B(ctxs[s_i]))
                stage_C(ctxs[s_i])
            k.barrier()

    def conv_phase(l, b, uA):
        with ExitStack() as ph:
            def sbp(name, shape, dt=F32):
                return Buf(ph.enter_context(nc.sbuf_tensor(un(name), list(shape), dt)), name)
            wA = sbp("wA", [128, 8, 768], BF16)
            zt = sbp("zt", [128, S + 2])
            cgt = Pool([sbp("cgt%d" % i, [128, 512]) for i in range(2)])
            bgt = sbp("bgt", [128, S])
            yt = sbp("yt", [128, S])
            wload(wA, "w_in", l, 0, 768)
            k.op("dve", lambda: V.memset(zt[:, 0:2], 0.0), [], [zt])
            for j in range(2):
                for tb in range(4):
                    cols = slice(tb * 512, (tb + 1) * 512)
                    pc, pv, pb = pp.get(), pp.get(), pp.get()
                    for (pt_, c0) in ((pc, 256 + 128 * j), (pv, 512 + 128 * j), (pb, 128 * j)):
                        for kc in range(8):
                            k.op("pe", lambda: T.matmul(pt_[:, :], lhsT=wA[:, kc, c0:c0 + 128], rhs=hT[:, kc, cols],
                                                        start=(kc == 0), stop=(kc == 7)), [wA, HB(cols)], [pt_],
                                 inc=(kc == 7))
                    cg = cgt.get()
                    k.op("act", lambda: A.activation(out=cg[:], in_=pc[:, :], func=AF.Copy), [pc], [cg])
                    k.op("dve", lambda: V.tensor_tensor(out=zt[:, 2 + tb * 512:2 + (tb + 1) * 512], in0=pv[:, :],
                                                        in1=cg[:], op=ALU.mult), [pv, cg], [zt])
                    k.op("act", lambda: A.activation(out=bgt[:, cols], in_=pb[:, :], func=AF.Copy), [pb], [bgt])
                k.op("dve", lambda: V.tensor_scalar(out=yt[:], in0=zt[:, 2:S + 2], scalar1=convw[:, l, 2, j:j + 1],
                                                    scalar2=None, op0=ALU.mult), [zt, convw], [yt])
                k.op("dve", lambda: V.scalar_tensor_tensor(out=yt[:], in0=zt[:, 1:S + 1],
                                                           scalar=convw[:, l, 1, j:j + 1], in1=yt[:],
                                                           op0=ALU.mult, op1=ALU.add), [zt, convw, yt], [yt])
                k.op("dve", lambda: V.scalar_tensor_tensor(out=yt[:], in0=zt[:, 0:S],
                                                           scalar=convw[:, l, 0, j:j + 1], in1=yt[:],
                                                           op0=ALU.mult, op1=ALU.add), [zt, convw, yt], [yt])
                k.op("dve", lambda: V.tensor_tensor(out=uA[:, j, :], in0=bgt[:], in1=yt[:], op=ALU.mult),
                     [bgt, yt], [uA])
            k.barrier()

    def ssm_phase(l, b, zT):
        with ExitStack() as ph:
            def sbp(name, shape, dt=F32):
                return Buf(ph.enter_context(nc.sbuf_tensor(un(name), list(shape), dt)), name)
            vP = Pool(psb[0:4])
            yP = Pool(psb[4:6])
            mP = Pool(psb[6:8])
            wB = sbp("wB", [128, 8, 256], BF16)
            ubf = sbp("ubf", [128, 2, S], BF16)
            BbT = sbp("BbT", [128, 8, 2, 128], BF16)
            k.op("dve", lambda: V.memset(BbT[:], 0.0), [], [BbT])
            Cre = sbp("Cre", [128, 8, 128], BF16)
            nCre = sbp("nCre", [128, 8, 128], BF16)
            nCim = sbp("nCim", [128, 8, 128], BF16)
            gst = sbp("gst", [128, 8, 2])
            iota = sbp("iota", [128, 512])
            k.dma("sp", iota[:], iota_d, writes=[iota])
            stg = ExitStack()
            bp = Buf(stg.enter_context(nc.sbuf_tensor(un("bp"), [128, 8, 2, 32], F32)), "bp")
            cp = Buf(stg.enter_context(nc.sbuf_tensor(un("cp"), [128, 8, 2, 128], F32)), "cp")
            bbt = Pool([Buf(stg.enter_context(nc.sbuf_tensor(un("bbt"), [128, 32], F32)), "bbt") for i in range(3)])
            wload(wB, "w_in", l, 768, 1024)
            k.dma("sp", bp[:], bpad_d[:, l], writes=[bp])
            k.dma("sp", cp[:], cpad_d[:, l], writes=[cp])
            k.op("act", lambda: A.activation(out=Cre[:], in_=cp[:, :, 0, :], func=AF.Copy), [cp], [Cre])
            k.op("act", lambda: A.activation(out=nCre[:], in_=cp[:, :, 0, :], func=AF.Copy, scale=-1.0), [cp], [nCre])
            k.op("act", lambda: A.activation(out=nCim[:], in_=cp[:, :, 1, :], func=AF.Copy, scale=-1.0), [cp], [nCim])
            for kk_ in range(8):
                ci = l * 8 + kk_
                for ri in range(2):
                    t0, t1_ = bbt.get(), bbt.get()
                    if ri == 0:
                        k.op("dve", lambda: V.tensor_scalar(out=t0[:], in0=bp[:, kk_, 1, :], scalar1=coefi[:, ci:ci + 1],
                                                            scalar2=None, op0=ALU.mult), [bp, coefi], [t0])
                        k.op("dve", lambda: V.scalar_tensor_tensor(out=t1_[:], in0=bp[:, kk_, 0, :],
                                                                   scalar=coefr[:, ci:ci + 1], in1=t0[:],
                                                                   op0=ALU.mult, op1=ALU.subtract),
                             [bp, coefr, t0], [t1_])
                    else:
                        k.op("dve", lambda: V.tensor_scalar(out=t0[:], in0=bp[:, kk_, 0, :], scalar1=coefi[:, ci:ci + 1],
                                                            scalar2=None, op0=ALU.mult), [bp, coefi], [t0])
                        k.op("dve", lambda: V.scalar_tensor_tensor(out=t1_[:], in0=bp[:, kk_, 1, :],
                                                                   scalar=coefr[:, ci:ci + 1], in1=t0[:],
                                                                   op0=ALU.mult, op1=ALU.add),
                             [bp, coefr, t0], [t1_])
                    pt = mP.get()
                    k.op("pe", lambda: T.transpose(out=pt[0:32, 0:128], in_=t1_[:, :], identity=ident[:]),
                         [t1_, ident], [pt])
                    r0 = 32 * (kk_ % 4)
                    k.op("act", lambda: A.activation(out=BbT[r0:r0 + 32, kk_, ri, :], in_=pt[0:32, 0:128],
                                                     func=AF.Copy), [pt, BbT], [], merge=[BbT])
            k.barrier()
            stg.close()
            phs = Pool([sbp("phs%d" % i, [128, 512]) for i in range(2)])
            Ct = Pool([sbp("Ct%d" % i, [128, 512]) for i in range(2)])
            St = Pool([sbp("St%d" % i, [128, 512]) for i in range(2)])
            ms = Pool([sbp("ms%d" % i, [128, 512]) for i in range(8)])
            ws = Pool([sbp("ws%d" % i, [128, 512]) for i in range(4)])
            gs = Pool([sbp("gs%d" % i, [128, 512]) for i in range(2)])
            prs = Pool([sbp("pr%d" % i, [128, 512], BF16) for i in range(4)])
            yfs = Pool([sbp("yf%d" % i, [128, 512]) for i in range(1)])
            gt = Pool([sbp("gt%d" % i, [128, 512]) for i in range(2)])
            for kb in range(2):
                for tb in range(4):
                    cols = slice(tb * 512, (tb + 1) * 512)
                    pt = mP.get()
                    for kc in range(8):
                        k.op("pe", lambda: T.matmul(pt[:, :], lhsT=wB[:, kc, kb * 128:(kb + 1) * 128],
                                                    rhs=hT[:, kc, cols], start=(kc == 0), stop=(kc == 7)),
                             [wB, HB(cols)], [pt], inc=(kc == 7))
                    k.op("act", lambda: A.activation(out=ubf[:, kb, cols], in_=pt[:, :], func=AF.Copy), [pt], [ubf])
            ystate = {}

            def ssm_S1(tb, kk_):
                cols = slice(tb * 512, (tb + 1) * 512)
                ci = l * 8 + kk_
                kb = kk_ // 4
                p0, p1 = phs.get(), phs.get()
                k.op("dve", lambda: V.tensor_scalar(out=p0[:], in0=iota[:], scalar1=float(tb * 512),
                                                    scalar2=theta[:, ci:ci + 1], op0=ALU.add, op1=ALU.mult),
                     [iota, theta], [p0])
                k.op("dve", lambda: V.tensor_scalar(out=p1[:], in0=p0[:], scalar1=1.0 / TWO_PI, scalar2=MAGIC,
                                                    op0=ALU.mult, op1=ALU.add), [p0], [p1])
                k.op("dve", lambda: V.tensor_scalar(out=p1[:], in0=p1[:], scalar1=MAGIC, scalar2=-TWO_PI,
                                                    op0=ALU.subtract, op1=ALU.mult), [p1], [p1])
                k.op("dve", lambda: V.tensor_tensor(out=p0[:], in0=p0[:], in1=p1[:], op=ALU.add), [p0, p1], [p0])
                k.op("dve", lambda: V.tensor_scalar(out=p0[:], in0=p0[:], scalar1=-3.141592, scalar2=3.141592,
                                                    op0=ALU.max, op1=ALU.min), [p0], [p0])
                Sn, Cs = St.get(), Ct.get()
                k.op("act", lambda: A.activation(out=Sn[:], in_=p0[:], func=AF.Sin), [p0], [Sn])
                k.op("dve", lambda: V.tensor_scalar(out=p1[:], in0=p0[:], scalar1=TWO_PI / 4, scalar2=-TWO_PI,
                                                    op0=ALU.is_gt, op1=ALU.mult), [p0], [p1])
                k.op("dve", lambda: V.scalar_tensor_tensor(out=p1[:], in0=p0[:], scalar=TWO_PI / 4, in1=p1[:],
                                                           op0=ALU.add, op1=ALU.add), [p0, p1], [p1])
                k.op("dve", lambda: V.tensor_scalar(out=p1[:], in0=p1[:], scalar1=-3.141592, scalar2=3.141592,
                                                    op0=ALU.max, op1=ALU.min), [p1], [p1])
                k.op("act", lambda: A.activation(out=Cs[:], in_=p1[:], func=AF.Sin), [p1], [Cs])
                vr, vi = vP.get(), vP.get()
                for (vt, ri) in ((vr, 0), (vi, 1)):
                    k.op("pe", lambda: T.matmul(vt[:, :], lhsT=BbT[:, kk_, ri, :],
                                                rhs=ubf[:, kb, cols], start=True, stop=True),
                         [BbT, ubf], [vt])
                m1, m2, m3, m4 = ms.get(), ms.get(), ms.get(), ms.get()
                k.op("dve", lambda: V.tensor_tensor(out=m1[:], in0=vr[:, :], in1=Cs[:], op=ALU.mult), [vr, Cs], [m1])
                k.op("dve", lambda: V.tensor_tensor(out=m2[:], in0=vi[:, :], in1=Sn[:], op=ALU.mult), [vi, Sn], [m2])
                k.op("dve", lambda: V.tensor_tensor(out=m3[:], in0=vi[:, :], in1=Cs[:], op=ALU.mult), [vi, Cs], [m3])
                k.op("dve", lambda: V.tensor_tensor(out=m4[:], in0=vr[:, :], in1=Sn[:], op=ALU.mult), [vr, Sn], [m4])
                wr, wi_ = ws.get(), ws.get()
                k.op("pool", lambda: P.tensor_tensor(out=wr[:], in0=m1[:], in1=m2[:], op=ALU.add), [m1, m2], [wr])
                k.op("pool", lambda: P.tensor_tensor(out=wi_[:], in0=m3[:], in1=m4[:], op=ALU.subtract),
                     [m3, m4], [wi_])
                return dict(tb=tb, kk_=kk_, Sn=Sn, Cs=Cs, wr=wr, wi_=wi_)

            def ssm_S2(c):
                tb, kk_, Sn, Cs, wr, wi_ = c["tb"], c["kk_"], c["Sn"], c["Cs"], c["wr"], c["wi_"]
                cols = slice(tb * 512, (tb + 1) * 512)
                ci = l * 8 + kk_
                kb = kk_ // 4
                gr, gi = gs.get(), gs.get()
                dec = rdec[:, ci:ci + 1].to_broadcast([128, 512])
                for (g_, w__, ri) in ((gr, wr, 0), (gi, wi_, 1)):
                    init = 0.0 if tb == 0 else gst[:, kk_, ri:ri + 1]
                    k.op("dve", lambda: V.tensor_tensor_scan(out=g_[:], data0=dec, data1=w__[:], initial=init,
                                                             op0=ALU.mult, op1=ALU.add),
                         [rdec, w__, gst], [g_])
                    if tb < 3:
                        k.op("dve", lambda: V.tensor_copy(out=gst[:, kk_, ri:ri + 1], in_=g_[:, 511:512]),
                             [g_], [gst])
                if kk_ % 4 == 0:
                    ystate["ypt"] = yP.get()
                ypt = ystate["ypt"]
                combos = ((gr, Cs, Cre), (gi, Sn, nCre), (gi, Cs, nCim), (gr, Sn, nCim))
                for n_, (g_, tb_, cm) in enumerate(combos):
                    pr = prs.get()
                    k.op("pool", lambda: P.tensor_tensor(out=pr[:], in0=g_[:], in1=tb_[:], op=ALU.mult),
                         [g_, tb_], [pr])
                    k.op("pe", lambda: T.matmul(ypt[:, :], lhsT=cm[:, kk_, :], rhs=pr[:],
                                                start=(kk_ % 4 == 0 and n_ == 0), stop=(kk_ % 4 == 3 and n_ == 3)),
                         [cm, pr], [ypt], inc=(n_ == 3))
                if kk_ % 4 == 3:
                    yf = yfs.get()
                    k.op("dve", lambda: V.scalar_tensor_tensor(out=yf[:], in0=ubf[:, kb, cols],
                                                               scalar=ssmd[:, l, kb:kb + 1], in1=ypt[:, :],
                                                               op0=ALU.mult, op1=ALU.add), [ubf, ssmd, ypt], [yf])
                    if dbg is not None and "ssm" in dbg and b == 0 and l == 0:
                        t = dbg_tensor("y_%d_%d" % (tb, kb), [128, 512])
                        k.dma("act", t, yf[:], reads=[yf])
                    g1_, g2_ = gt.get(), gt.get()
                    k.op("dve", lambda: V.tensor_tensor(out=g1_[:], in0=yf[:], in1=yf[:], op=ALU.mult), [yf], [g1_])
                    k.op("dve", lambda: V.tensor_scalar(out=g1_[:], in0=g1_[:], scalar1=0.044715, scalar2=1.0,
                                                        op0=ALU.mult, op1=ALU.add), [g1_], [g1_])
                    k.op("dve", lambda: V.tensor_tensor(out=g1_[:], in0=g1_[:], in1=yf[:], op=ALU.mult),
                         [g1_, yf], [g1_])
                    k.op("act", lambda: A.activation(out=g2_[:], in_=g1_[:], func=AF.Sigmoid,
                                                     scale=1.5957691216057308), [g1_], [g2_])
                    k.op("dve", lambda: V.tensor_tensor(out=zT[:, kb, cols], in0=yf[:], in1=g2_[:], op=ALU.mult),
                         [yf, g2_], [zT])

            its = [(tb, kk_) for tb in range(4) for kk_ in range(8)]
            if SSM_PIPE:
                cur = ssm_S1(*its[0])
                for n_it in range(len(its)):
                    nxt = ssm_S1(*its[n_it + 1]) if n_it + 1 < len(its) else None
                    ssm_S2(cur)
                    cur = nxt
            else:
                for it_ in its:
                    ssm_S2(ssm_S1(*it_))
            k.barrier()

    def mix_phase(l, b, uA, zT, oT):
        with ExitStack() as ph:
            def sbp(name, shape, dt=F32):
                return Buf(ph.enter_context(nc.sbuf_tensor(un(name), list(shape), dt)), name)
            gseg = Pool([sbp("gseg%d" % i, [128, 8, 256], BF16) for i in range(6)])
            wcos = Pool([sbp("wco%d" % i, [128, 2, 256], BF16) for i in range(2)])
            wgas = Pool([sbp("wga%d" % i, [128, 2, 256], BF16) for i in range(2)])
            wgbs = Pool([sbp("wgb%d" % i, [128, 2, 256], BF16) for i in range(2)])
            waos = Pool([sbp("wao%d" % i, [128, 4, 256], BF16) for i in range(2)])
            wos = Pool([sbp("wo%d" % i, [128, 8, 512], BF16) for i in range(1)])
            mixs = Pool([sbp("mix%d" % i, [128, 8, 512], BF16) for i in range(1)])
            sg = Pool([sbp("sg%d" % i, [128, 512]) for i in range(4)])
            tm = Pool([sbp("tm%d" % i, [128, 512]) for i in range(3)])
            npools = norm_pools(ph) if FUSE_NORM else None
            for tb in range(4):
                cols = slice(tb * 512, (tb + 1) * 512)
                mix = mixs.get()
                for cg in range(4):
                    if cg == 2 and tb > 0 and npools is not None:
                        norm_tb(l, 1, b, tb - 1, npools)
                    segs = []
                    for br in range(3):
                        sgm = gseg.get()
                        c0 = 2048 + br * 1024 + cg * 256
                        wload(sgm, "w_in", l, c0, c0 + 256)
                        segs.append(sgm)
                    wco, wga, wgb, wao = wcos.get(), wgas.get(), wgbs.get(), waos.get()
                    wload(wco, "w_conv_out", l, cg * 256, (cg + 1) * 256, kc=2)
                    wload(wga, "w_glu", l, cg * 256, (cg + 1) * 256, kc=2)
                    wload(wgb, "w_glu", l, D + cg * 256, D + (cg + 1) * 256, kc=2)
                    wload(wao, "w_attn_out", l, cg * 256, (cg + 1) * 256, kc=4)
                    for cc in range(2):
                        c = cg * 2 + cc
                        mc = slice(cc * 128, (cc + 1) * 128)
                        gp = []
                        for br in range(3):
                            pt = pp.get()
                            for kc in range(8):
                                k.op("pe", lambda: T.matmul(pt[:, :], lhsT=segs[br][:, kc, mc], rhs=hT[:, kc, cols],
                                                            start=(kc == 0), stop=(kc == 7)), [segs[br], HB(cols)], [pt],
                                     inc=(kc == 7))
                            gp.append(pt)
                        sgs = []
                        for br in range(3):
                            s_ = sg.get()
                            k.op("act", lambda: A.activation(out=s_[:], in_=gp[br][:, :], func=AF.Sigmoid,
                                                             bias=bgate[:, l, br * 8 + c:br * 8 + c + 1], scale=1.0),
                                 [gp[br], bgate], [s_])
                            sgs.append(s_)
                        pya, pza, pzb, pyc = pp.get(), pp.get(), pp.get(), pp.get()
                        for kc in range(2):
                            k.op("pe", lambda: T.matmul(pya[:, :], lhsT=wco[:, kc, mc], rhs=uA[:, kc, cols],
                                                        start=(kc == 0), stop=(kc == 1)), [wco, uA], [pya], inc=(kc == 1))
                        for kc in range(2):
                            k.op("pe", lambda: T.matmul(pza[:, :], lhsT=wga[:, kc, mc], rhs=zT[:, kc, cols],
                                                        start=(kc == 0), stop=(kc == 1)), [wga, zT], [pza], inc=(kc == 1))
                        for kc in range(2):
                            k.op("pe", lambda: T.matmul(pzb[:, :], lhsT=wgb[:, kc, mc], rhs=zT[:, kc, cols],
                                                        start=(kc == 0), stop=(kc == 1)), [wgb, zT], [pzb], inc=(kc == 1))
                        for kc in range(4):
                            k.op("pe", lambda: T.matmul(pyc[:, :], lhsT=wao[:, kc, mc], rhs=oT[:, kc, cols],
                                                        start=(kc == 0), stop=(kc == 3)), [wao, oT], [pyc], inc=(kc == 3))
                        sz = sg.get()
                        k.op("act", lambda: A.activation(out=sz[:], in_=pzb[:, :], func=AF.Sigmoid), [pzb], [sz])
                        t1_, t2_, t3_ = tm.get(), tm.get(), tm.get()
                        k.op("dve", lambda: V.tensor_tensor(out=t1_[:], in0=pya[:, :], in1=sgs[0][:], op=ALU.mult),
                             [pya, sgs[0]], [t1_])
                        k.op("dve", lambda: V.tensor_tensor(out=t2_[:], in0=pza[:, :], in1=sz[:], op=ALU.mult),
                             [pza, sz], [t2_])
                        k.op("pool", lambda: P.tensor_tensor(out=t2_[:], in0=t2_[:], in1=sgs[1][:], op=ALU.mult),
                             [t2_, sgs[1]], [t2_])
                        k.op("dve", lambda: V.tensor_tensor(out=t3_[:], in0=pyc[:, :], in1=sgs[2][:], op=ALU.mult),
                             [pyc, sgs[2]], [t3_])
                        k.op("pool", lambda: P.tensor_tensor(out=t1_[:], in0=t1_[:], in1=t2_[:], op=ALU.add),
                             [t1_, t2_], [t1_])
                        k.op("pool", lambda: P.tensor_tensor(out=mix[:, c, :], in0=t1_[:], in1=t3_[:], op=ALU.add),
                             [t1_, t3_], [mix])
                for nh in range(2):
                    wo = wos.get()
                    wload(wo, "w_o", l, nh * 512, (nh + 1) * 512)
                    for nn in range(4):
                        n = nh * 4 + nn
                        pt = pp.get()
                        for c in range(8):
                            k.op("pe", lambda: T.matmul(pt[:, :], lhsT=wo[:, c, nn * 128:(nn + 1) * 128], rhs=mix[:, c, :],
                                                        start=(c == 0), stop=(c == 7)), [wo, mix], [pt], inc=(c == 7))
                        k.op("dve", lambda: V.scalar_tensor_tensor(out=xT[:, n, cols], in0=pt[:, :],
                                                                   scalar=modT[:, l, 2 * 8 + n, b:b + 1],
                                                                   in1=xT[:, n, cols],
                                                                   op0=ALU.mult, op1=ALU.add), [pt, modT, xTb[tb]], [xTb[tb]])
            if npools is not None:
                norm_tb(l, 1, b, 3, npools)
            k.barrier()

    def ffn_phase(l, b):
        with ExitStack() as ph:
            def sbp(name, shape, dt=F32):
                return Buf(ph.enter_context(nc.sbuf_tensor(un(name), list(shape), dt)), name)
            if not FUSE_NORM:
                with ExitStack() as ph2:
                    norm_mod(l, 1, b, ph2)
                    k.barrier()
            npools = norm_pools(ph) if FUSE_NORM else None
            fseg = Pool([sbp("fseg%d" % i, [128, 8, 512], BF16) for i in range(2)])
            wfo = sbp("wfo", [128, 22, D], BF16)
            acts = Pool([sbp("act%d" % i, [128, 22, 512], BF16) for i in range(1)])
            sg = Pool([sbp("fsg%d" % i, [128, 512]) for i in range(2)])
            wload(wfo, "w_ffn_out", l, 0, D, kc=22)
            for tb in range(4):
                cols = slice(tb * 512, (tb + 1) * 512)
                act = acts.get()
                for fg in range(11):
                    if fg == 5 and tb > 0 and npools is not None and l + 1 < L:
                        norm_tb(l + 1, 0, b, tb - 1, npools)
                    seg = fseg.get()
                    wload(seg, "w_ffn_in", l, fg * 512, (fg + 1) * 512)
                    for fc in range(2):
                        f = fg * 2 + fc
                        pg, pu = pp.get(), pp.get()
                        for (pt_, c0) in ((pg, fc * 128), (pu, 256 + fc * 128)):
                            for kc in range(8):
                                k.op("pe", lambda: T.matmul(pt_[:, :], lhsT=seg[:, kc, c0:c0 + 128], rhs=hT[:, kc, cols],
                                                            start=(kc == 0), stop=(kc == 7)), [seg, HB(cols)], [pt_],
                                     inc=(kc == 7))
                        s_ = sg.get()
                        k.op("act", lambda: A.activation(out=s_[:], in_=pg[:, :], func=AF.Silu), [pg], [s_])
                        k.op("dve", lambda: V.tensor_tensor(out=act[:, f, :], in0=pu[:, :], in1=s_[:], op=ALU.mult),
                             [pu, s_], [act])
                for n in range(8):
                    pt = pp.get()
                    for f in range(22):
                        k.op("pe", lambda: T.matmul(pt[:, :], lhsT=wfo[:, f, n * 128:(n + 1) * 128], rhs=act[:, f, :],
                                                    start=(f == 0), stop=(f == 21)), [wfo, act], [pt], inc=(f == 21))
                    k.op("dve", lambda: V.scalar_tensor_tensor(out=xT[:, n, cols], in0=pt[:, :],
                                                               scalar=modT[:, l, 5 * 8 + n, b:b + 1], in1=xT[:, n, cols],
                                                               op0=ALU.mult, op1=ALU.add), [pt, modT, xTb[tb]], [xTb[tb]])
            if npools is not None and l + 1 < L:
                norm_tb(l + 1, 0, b, 3, npools)
            k.barrier()

    def dump(name, buf, shape, dt, b, l):
        if dbg is not None and name in dbg and b == 0 and l == 0:
            t = dbg_tensor(name, shape, dt)
            pat = {2: "p a -> p a", 3: "p a s -> p (a s)"}[len(buf.t.shape)]
            k.dma("act", t, buf[:].rearrange(pat) if len(buf.t.shape) == 3 else buf[:], reads=[buf])

    for b in range(NB):
        load_x(b)
        for l in range(L if stage >= 2 else 0):
            if l == 0 or not FUSE_NORM or stage < 99:
                with ExitStack() as ph:
                    norm_mod(l, 0, b, ph)
                    k.barrier()
            dump("hT", hT, [128, 8 * S], BF16, b, l)
            if stage >= 3:
                with ExitStack() as lay:
                    def sbl(name, shape, dt=F32):
                        return Buf(lay.enter_context(nc.sbuf_tensor(un(name), list(shape), dt)), name)
                    oT = sbl("oT", [128, 4, S], BF16)
                    attn_phase(l, b, oT)
                    dump("oT", oT, [128, 4 * S], BF16, b, l)
                    if stage >= 4:
                        uA = sbl("uA", [128, 2, S], BF16)
                        conv_phase(l, b, uA)
                        dump("uA", uA, [128, 2 * S], BF16, b, l)
                    if stage >= 5:
                        zT = sbl("zT", [128, 2, S], BF16)
                        ssm_phase(l, b, zT)
                        dump("zT", zT, [128, 2 * S], BF16, b, l)
                    if stage >= 6:
                        mix_phase(l, b, uA, zT, oT)
                        dump("x1", xT, [128, 8 * S], F32, b, l)
                    k.barrier()
            if stage >= 7:
                ffn_phase(l, b)
                dump("x2", xT, [128, 8 * S], F32, b, l)
            if stage < 99 and l == 0:
                break
        final_store(b)
        if stage < 99:
            break

    k.barrier()
    st.close()
    return k, dbg_out


def _host_consts():
    c = {}
    s = np.arange(S)
    kaug = np.zeros((14, S), np.float32)
    kaug[0:8] = 1
    kaug[8] = 1
    kaug[9] = 1
    kaug[10] = ((s % 128) // 64) * 64
    kaug[11] = s % 64
    kaug[12] = 1
    kaug[13] = s // 128
    tq = np.arange(128)[:, None]
    uu = np.arange(S)[None, :]
    c["dtab"] = np.abs(tq - uu + 1920).astype(np.float32).astype(ml_dtypes.bfloat16)
    sct = np.zeros((128, 8), np.float32)
    sct[64:72] = np.diag(2.0 ** (-(np.arange(1, 9))))
    sct[0:8] = np.diag(2.0 ** (-(np.arange(1, 9))))
    c["sctab"] = sct
    c["kaug_c"] = kaug.astype(ml_dtypes.bfloat16)
    slopes = 2.0 ** (-(np.arange(1, 9)))
    tp = np.arange(128)
    qaug = np.zeros((16, 6, 8, 128), np.float32)
    for i in range(16):
        for h in range(8):
            qaug[i, 0, h] = -slopes[h] * ((tp // 64) * 64)
            qaug[i, 1, h] = -slopes[h] * (tp % 64)
            qaug[i, 2, h] = slopes[h]
            qaug[i, 3, h] = slopes[h]
            qaug[i, 4, h] = -slopes[h] * 128 * i
            qaug[i, 5, h] = slopes[h] * 128
    c["qaug_c"] = qaug.reshape(16, 6, 1024).astype(ml_dtypes.bfloat16)
    tt = tp[:, None]
    ss = tp[None, :]
    c["constU"] = np.maximum(ss - tt, 0).astype(np.float32).astype(ml_dtypes.bfloat16)
    dneg = np.zeros((128, 8, 128), np.float32)
    irep = np.zeros((128, 8, 128), np.float32)
    for h in range(8):
        dneg[:, h, :] = -2 * slopes[h] * np.eye(128)
        irep[:, h, :] = np.eye(128)
    c["constDneg"] = dneg.reshape(128, 1024).astype(ml_dtypes.bfloat16)
    c["constIrep"] = irep.reshape(128, 1024).astype(ml_dtypes.bfloat16)
    c["ident"] = np.eye(128, dtype=np.float32)
    dm = np.zeros((128, 128), np.float32)
    dm[:64, 64:] = NEG
    c["diagmask"] = dm
    c["iota512"] = np.broadcast_to(np.arange(512, dtype=np.float32), (128, 512)).copy()
    sel = np.zeros((65, 64), np.float32)
    sel[64, :] = 1.0
    c["sel65"] = sel
    return c


def _layout_common(inp):
    f = np.float32
    m = {}
    m["w_mod"] = np.ascontiguousarray(inp["w_mod"], f).reshape(L, 8, 128, 6 * D)
    nrm = np.stack([inp["norm1"][0], inp["norm2"][0], inp["norm1"][1], inp["norm2"][1]], 0)
    m["nrm"] = np.ascontiguousarray(nrm.reshape(2 * L, 8, 128).transpose(2, 0, 1), f)
    m["normf_b"] = np.ascontiguousarray(np.broadcast_to(inp["norm_f"][None, :], (128, D)), f)
    w_in = np.zeros((L, D, DIN_P), f)
    w_in[:, :, 0:1988] = inp["w_in"][:, :, 0:1988]
    w_in[:, :, 2048:5120] = inp["w_in"][:, :, 1988:5060]
    m["w_in"] = w_in
    m["b_gate_t"] = np.ascontiguousarray(inp["b_gate"].reshape(L, 24, 128).transpose(2, 0, 1), f)
    m["conv_w_t"] = np.ascontiguousarray(inp["conv_w"].reshape(L, 3, 2, 128).transpose(3, 0, 1, 2), f)
    for nme in ("w_conv_out", "w_glu", "w_attn_out", "w_o", "w_ffn_out"):
        m[nme] = np.ascontiguousarray(inp[nme], f)
    wf = inp["w_ffn_in"]
    wfr = np.zeros((L, D, 2 * DFF), f)
    for g in range(11):
        wfr[:, :, g * 512:g * 512 + 256] = wf[:, :, g * 256:(g + 1) * 256]
        wfr[:, :, g * 512 + 256:(g + 1) * 512] = wf[:, :, DFF + g * 256:DFF + (g + 1) * 256]
    m["w_ffn_in"] = wfr
    sc = np.zeros((128, L, 8, 3), f)
    for l in range(L):
        for g in range(16):
            kk, gl = g // 2, g % 2
            sc[gl * 64:(gl + 1) * 64, l, kk, 0] = inp["ssm_a_re"][l, g]
            sc[gl * 64:(gl + 1) * 64, l, kk, 1] = inp["ssm_a_im"][l, g]
            sc[gl * 64:(gl + 1) * 64, l, kk, 2] = inp["ssm_log_dt"][l, g]
    m["ssm_sc"] = sc
    bpad = np.zeros((128, L, 8, 2, 32), f)
    cpad = np.zeros((128, L, 8, 2, 128), f)
    for l in range(L):
        for g in range(16):
            kk, gl = g // 2, g % 2
            rows = slice(gl * 64, (gl + 1) * 64)
            bpad[rows, l, kk, 0, gl * 16:(gl + 1) * 16] = inp["ssm_b_re"][l, g]
            bpad[rows, l, kk, 1, gl * 16:(gl + 1) * 16] = inp["ssm_b_im"][l, g]
            c0 = 32 * (kk % 4) + gl * 16
            cpad[rows, l, kk, 0, c0:c0 + 16] = inp["ssm_c_re"][l, g].T
            cpad[rows, l, kk, 1, c0:c0 + 16] = inp["ssm_c_im"][l, g].T
    m["bpad"] = bpad
    m["cpad"] = cpad
    m["ssm_d_t"] = np.ascontiguousarray(inp["ssm_d"].reshape(L, 2, 128).transpose(2, 0, 1), f)
    m.update(_host_consts())
    return m


def _layout_core(inp, core):
    f = np.float32
    rows = slice(core * NB, (core + 1) * NB)
    m = {}
    m["x"] = np.ascontiguousarray(inp["x"][rows], f)
    m["cT"] = np.ascontiguousarray(inp["c"][rows].reshape(NB, 8, 128).transpose(2, 1, 0), f)
    m["b_mod2"] = np.ascontiguousarray(np.broadcast_to(inp["b_mod"][:, None, :], (L, NB, 6 * D)), f)
    return m


_CACHE = {}


def kernel(**inputs):
    inp = {k_: np.asarray(v) for k_, v in inputs.items()}
    if "nc" not in _CACHE:
        nc = bass.Bass("TRN2", target_bir_lowering=False)
        build(nc)
        _CACHE["nc"] = nc
    nc = _CACHE["nc"]
    common = _layout_common(inp)
    in_maps = []
    for core in range(NCORES):
        m = dict(common)
        m.update(_layout_core(inp, core))
        in_maps.append(m)
    res = run_bass_kernel_spmd(nc, in_maps, core_ids=list(range(NCORES)))
    out = np.concatenate([np.asarray(r["out"], np.float32) for r in res.results], axis=0)
    return out
```
